# Optimizing a Trainium2 kernel written in Bass

```python
import math
import jax, jax.numpy as jnp
from jax import lax
import numpy as np

D_MODEL = 1024
BATCH = 4
SEQ = 8192
DEPTH = 1

HEAD_DIM = 64
N_HEADS_A = 8
N_KV_A = 2
GROUP_A = N_HEADS_A // N_KV_A
N_HEADS_B = 8
WINDOW = 128
WIN_BLOCK = 128
T5_BUCKETS = 32
T5_MAX_DIST = 128
GRID_W = 64
NA_ROWS = 8
NA_COLS = 16
PEER_HEADS = 8
PEER_KEYS = 128
N_EXPERTS = PEER_KEYS * PEER_KEYS
PEER_QDIM = 256
PEER_HALF = PEER_QDIM // 2
PEER_TOPK = 16
PEER_CHUNK = 128
EPS = 1e-6
NEG = -1e30

W_QA = N_HEADS_A * HEAD_DIM
W_KVA = N_KV_A * HEAD_DIM
W_B = N_HEADS_B * HEAD_DIM
D_MIX = W_QA + W_B
D_IN = W_QA + 2 * W_KVA + 3 * W_B
IN_SPLITS = [W_QA, W_QA + W_KVA, W_QA + 2 * W_KVA, W_QA + 2 * W_KVA + W_B, W_QA + 2 * W_KVA + 2 * W_B]

kernel_name = "hymba_window_natten_peer_block"


def rms_norm(x, g):
    xf = x.astype(jnp.float32)
    y = xf * lax.rsqrt(jnp.mean(xf * xf, axis=-1, keepdims=True) + EPS)
    return (y * g.astype(jnp.float32)).astype(x.dtype)


def modulate(h, shift, scale):
    return h * (1.0 + scale[:, None, :]) + shift[:, None, :]


def t5_bucket(rel):
    half = T5_BUCKETS // 2
    max_exact = half // 2
    ret = jnp.where(rel > 0, half, 0)
    n = jnp.abs(rel)
    nf = jnp.maximum(n, 1).astype(jnp.float32)
    large = max_exact + (jnp.log(nf / max_exact) / math.log(T5_MAX_DIST / max_exact)
                         * (half - max_exact)).astype(jnp.int32)
    large = jnp.minimum(large, half - 1)
    return ret + jnp.where(n < max_exact, n, large)


def windowed_gqa(q, k, v, sink, t5_table):
    B, S = q.shape[0], q.shape[1]
    nb = S // WIN_BLOCK
    span = WIN_BLOCK + 2 * WINDOW
    pad = ((0, 0), (WINDOW, WINDOW), (0, 0), (0, 0))
    k_pad = jnp.pad(k, pad)
    v_pad = jnp.pad(v, pad)
    i = jnp.arange(WIN_BLOCK)[:, None]
    j = jnp.arange(span)[None, :]
    rel = j - WINDOW - i
    band = jnp.abs(rel) <= WINDOW
    bias = t5_table[t5_bucket(rel)].transpose(2, 0, 1).astype(jnp.float32)
    scale = HEAD_DIM ** -0.5
    sink_f = sink.astype(jnp.float32)

    def block(b):
        qb = lax.dynamic_slice_in_dim(q, b * WIN_BLOCK, WIN_BLOCK, axis=1)
        qb = qb.reshape(B, WIN_BLOCK, N_KV_A, GROUP_A, HEAD_DIM)
        kb = lax.dynamic_slice_in_dim(k_pad, b * WIN_BLOCK, span, axis=1)
        vb = lax.dynamic_slice_in_dim(v_pad, b * WIN_BLOCK, span, axis=1)
        s = jnp.einsum('bqgrd,bkgd->bgrqk', qb, kb).astype(jnp.float32)
        s = s.reshape(B, N_HEADS_A, WIN_BLOCK, span) * scale + bias
        kpos = b * WIN_BLOCK - WINDOW + jnp.arange(span)
        valid = band & ((kpos >= 0) & (kpos < S))[None, :]
        s = jnp.where(valid, s, NEG)
        sink_col = jnp.broadcast_to(sink_f[None, :, None, None], (B, N_HEADS_A, WIN_BLOCK, 1))
        p = jax.nn.softmax(jnp.concatenate([s, sink_col], axis=-1), axis=-1)[..., :span]
        p = p.astype(v.dtype).reshape(B, N_KV_A, GROUP_A, WIN_BLOCK, span)
        o = jnp.einsum('bgrqk,bkgd->bqgrd', p, vb)
        return o.reshape(B, WIN_BLOCK, N_HEADS_A * HEAD_DIM)

    out = lax.map(block, jnp.arange(nb))
    return out.transpose(1, 0, 2, 3).reshape(B, S, N_HEADS_A * HEAD_DIM)


def neighbourhood_attention(q, k, v, rpb):
    B, S = q.shape[0], q.shape[1]
    rows = S // GRID_W
    kr = min(NA_ROWS, rows)
    q_g = q.reshape(B, rows, GRID_W, N_HEADS_B, HEAD_DIM)
    k_g = k.reshape(B, rows, GRID_W, N_HEADS_B, HEAD_DIM)
    v_g = v.reshape(B, rows, GRID_W, N_HEADS_B, HEAD_DIM)
    cols = jnp.arange(GRID_W)
    col_start = jnp.clip(cols - NA_COLS // 2, 0, GRID_W - NA_COLS)
    col_idx = col_start[:, None] + jnp.arange(NA_COLS)[None, :]
    col_off = col_idx - cols[:, None] + (NA_COLS - 1)
    scale = HEAD_DIM ** -0.5
    rpb_f = rpb.astype(jnp.float32)

    def row_block(r):
        rs = jnp.clip(r - kr // 2, 0, rows - kr)
        q_r = lax.dynamic_index_in_dim(q_g, r, axis=1, keepdims=False)
        k_band = lax.dynamic_slice_in_dim(k_g, rs, kr, axis=1)
        v_band = lax.dynamic_slice_in_dim(v_g, rs, kr, axis=1)
        k_win = k_band[:, :, col_idx]
        v_win = v_band[:, :, col_idx]
        s = jnp.einsum('bchd,bkcwhd->bhckw', q_r, k_win).astype(jnp.float32) * scale
        row_off = rs + jnp.arange(kr) - r + (NA_ROWS - 1)
        bias = rpb_f[:, row_off[None, :, None], col_off[:, None, :]]
        s = (s + bias[None]).reshape(B, N_HEADS_B, GRID_W, kr * NA_COLS)
        p = jax.nn.softmax(s, axis=-1).reshape(B, N_HEADS_B, GRID_W, kr, NA_COLS).astype(v.dtype)
        return jnp.einsum('bhckw,bkcwhd->bchd', p, v_win)

    out = lax.map(row_block, jnp.arange(rows))
    return out.transpose(1, 0, 2, 3, 4).reshape(B, S, N_HEADS_B * HEAD_DIM)


def peer(h, w_query, sub_keys, u_experts, v_experts):
    B, S, D = h.shape
    tokens = h.reshape(B * S // PEER_CHUNK, PEER_CHUNK, D)

    def chunk(xc):
        qh = (xc @ w_query).reshape(PEER_CHUNK, PEER_HEADS, PEER_QDIM)
        s1 = jnp.einsum('thd,hnd->thn', qh[..., :PEER_HALF], sub_keys[:, 0]).astype(jnp.float32)
        s2 = jnp.einsum('thd,hnd->thn', qh[..., PEER_HALF:], sub_keys[:, 1]).astype(jnp.float32)
        v1, i1 = lax.top_k(s1, PEER_TOPK)
        v2, i2 = lax.top_k(s2, PEER_TOPK)
        cand = (v1[..., :, None] + v2[..., None, :]).reshape(PEER_CHUNK, PEER_HEADS, PEER_TOPK * PEER_TOPK)
        vs, ci = lax.top_k(cand, PEER_TOPK)
        e1 = jnp.take_along_axis(i1, ci // PEER_TOPK, axis=-1)
        e2 = jnp.take_along_axis(i2, ci % PEER_TOPK, axis=-1)
        idx = e1 * PEER_KEYS + e2
        g = jax.nn.softmax(vs, axis=-1)
        u_sel = u_experts[idx]
        v_sel = v_experts[idx]
        z = jnp.einsum('td,thkd->thk', xc, u_sel)
        a = (jax.nn.gelu(z.astype(jnp.float32)) * g).astype(xc.dtype)
        return jnp.einsum('thk,thkd->td', a, v_sel)

    out = lax.map(chunk, tokens)
    return out.reshape(B, S, D)


def setup_inputs(seed: int = 0) -> dict:
    key = jax.random.key(seed)
    ks = jax.random.split(key, 20)
    f32 = jnp.float32
    D = D_MODEL
    nrm = lambda k, shape, s: jax.random.normal(k, shape, f32) * s
    return {
        "x": nrm(ks[0], (BATCH, SEQ, D), 1.0),
        "c": nrm(ks[1], (BATCH, D), 1.0),
        "w_ada": nrm(ks[2], (DEPTH, D, 6 * D), 0.5 * D ** -0.5),
        "b_ada": nrm(ks[3], (DEPTH, 6 * D), 0.02),
        "norm1_g": 1.0 + nrm(ks[4], (DEPTH, D), 0.02),
        "w_in": nrm(ks[5], (DEPTH, D, D_IN), D ** -0.5),
        "sink_a": nrm(ks[6], (DEPTH, N_HEADS_A), 0.5),
        "t5_table": nrm(ks[7], (T5_BUCKETS, N_HEADS_A), 0.3),
        "rpb_b": nrm(ks[8], (DEPTH, N_HEADS_B, 2 * NA_ROWS - 1, 2 * NA_COLS - 1), 0.3),
        "out_norm_a": 1.0 + nrm(ks[9], (DEPTH, W_QA), 0.02),
        "out_norm_b": 1.0 + nrm(ks[10], (DEPTH, W_B), 0.02),
        "w_out": nrm(ks[11], (DEPTH, D_MIX, D), D_MIX ** -0.5),
        "norm2_g": 1.0 + nrm(ks[12], (DEPTH, D), 0.02),
        "w_query": nrm(ks[13], (DEPTH, D, PEER_HEADS * PEER_QDIM), D ** -0.5),
        "sub_keys": nrm(ks[14], (DEPTH, PEER_HEADS, 2, PEER_KEYS, PEER_HALF), PEER_HALF ** -0.5),
        "u_experts": nrm(ks[15], (DEPTH, N_EXPERTS, D), D ** -0.5),
        "v_experts": nrm(ks[16], (DEPTH, N_EXPERTS, D), 1.0),
        "final_g": 1.0 + nrm(ks[17], (D,), 0.02),
    }


def reference(x, c, w_ada, b_ada, norm1_g, w_in, sink_a, t5_table, rpb_b, out_norm_a, out_norm_b,
              w_out, norm2_g, w_query, sub_keys, u_experts, v_experts, final_g):
    B, S, D = x.shape
    c_act = jax.nn.silu(c)
    for layer in range(DEPTH):
        ada = c_act @ w_ada[layer] + b_ada[layer]
        shift1, scale1, gate1, shift2, scale2, gate2 = jnp.split(ada, 6, axis=-1)

        h = modulate(rms_norm(x, norm1_g[layer]), shift1, scale1)
        proj = h @ w_in[layer]
        qa, ka, va, qb, kb, vb = jnp.split(proj, IN_SPLITS, axis=-1)
        o_a = windowed_gqa(qa.reshape(B, S, N_HEADS_A, HEAD_DIM),
                           ka.reshape(B, S, N_KV_A, HEAD_DIM),
                           va.reshape(B, S, N_KV_A, HEAD_DIM),
                           sink_a[layer], t5_table)
        o_b = neighbourhood_attention(qb.reshape(B, S, N_HEADS_B, HEAD_DIM),
                                      kb.reshape(B, S, N_HEADS_B, HEAD_DIM),
                                      vb.reshape(B, S, N_HEADS_B, HEAD_DIM),
                                      rpb_b[layer])
        mixed = jnp.concatenate([rms_norm(o_a, out_norm_a[layer]),
                                 rms_norm(o_b, out_norm_b[layer])], axis=-1)
        x = x + gate1[:, None, :] * (mixed @ w_out[layer])

        h2 = modulate(rms_norm(x, norm2_g[layer]), shift2, scale2)
        x = x + gate2[:, None, :] * peer(h2, w_query[layer], sub_keys[layer],
                                          u_experts[layer], v_experts[layer])
    return rms_norm(x, final_g)
```

```python
import numpy as np
from contextlib import ExitStack
import concourse.bass as bass
import concourse.mybir as mybir
from concourse.bass_utils import run_bass_kernel_spmd

F32 = mybir.dt.float32
BF16 = mybir.dt.bfloat16
U32 = mybir.dt.uint32
AF = mybir.ActivationFunctionType
ALU = mybir.AluOpType
AX = mybir.AxisListType

SAME_ENGINE_SYNC = True
EPS = 1e-6
NEGM = -1.0e4
NCORES = 8
SEQ = 8192
TOK = 4096
HALO = 256
NEXT = 36
D = 1024


class Prog:
    EPOCH = 20000

    def __init__(self, nc, es):
        self.nc = nc
        self.es = es
        self.engs = {"pe": nc.tensor, "act": nc.scalar, "dve": nc.vector, "pool": nc.gpsimd, "sp": nc.sync}
        self.sems = {}
        self.cnt = {}
        self.waited = {}
        self.state = {}
        self.ops = []
        self.engcount = {k: 0 for k in self.engs}
        self.nwaits = 0
        self.stopped = False

    def _sem(self, k):
        if k not in self.sems:
            self.sems[k] = self.es.enter_context(self.nc.semaphore("s%d" % len(self.sems)))
        return self.sems[k]

    def _entries(self, name, key):
        d = self.state.setdefault(name, {})
        if key is None:
            if None not in d:
                d[None] = [None, {}, []]
            return list(d.values())
        if key not in d:
            if None in d:
                b = d[None]
                d[key] = [b[0], dict(b[1]), list(b[2])]
            else:
                d[key] = [None, {}, []]
        return [d[key]]

    def op(self, eng, fn, reads=(), writes=(), dma=False, semkey=None):
        if self.stopped:
            return None
        idx = len(self.ops)
        deps = set()
        psdeps = set()
        for (name, key) in reads:
            for ent in self._entries(name, key):
                if ent[0] is not None:
                    deps.add(ent[0])
        for (name, key) in writes:
            tgt = psdeps if name.startswith("ps") else deps
            for ent in self._entries(name, key):
                if ent[0] is not None:
                    tgt.add(ent[0])
                tgt.update(ent[1].values())
                tgt.update(ent[2])
        e = self.engs[eng]
        for di in sorted(deps | psdeps):
            deng, dk, dval, ddma = self.ops[di]
            if (not ddma) and deng == eng and (eng == "pe" or not SAME_ENGINE_SYNC or di not in deps):
                continue
            if self.waited.get((eng, dk), 0) >= dval:
                continue
            e.wait_ge(self._sem(dk), dval)
            self.waited[(eng, dk)] = dval
            self.nwaits += 1
        ins = fn(e)
        if dma:
            k = ("dma", semkey)
            inc = 16
        else:
            self.engcount[eng] += 1
            k = ("eng", eng, self.engcount[eng] // self.EPOCH)
            inc = 1
        self.cnt[k] = self.cnt.get(k, 0) + inc
        ins.then_inc(self._sem(k), inc)
        self.ops.append((eng, k, self.cnt[k], dma))
        for (name, key) in reads:
            for ent in self._entries(name, key):
                if dma:
                    ent[2].append(idx)
                else:
                    ent[1][eng] = idx
        for (name, key) in writes:
            d = self.state[name]
            if key is None:
                for kk in list(d.keys()):
                    d[kk] = [idx, {}, []]
            else:
                d[key] = [idx, {}, []]
        return idx

    def barrier(self):
        if self.stopped:
            return
        targets = list(self.cnt.items())
        for eng, e in self.engs.items():
            for k, v in targets:
                if self.waited.get((eng, k), 0) >= v:
                    continue
                e.wait_ge(self._sem(k), v)
                self.waited[(eng, k)] = v
                self.nwaits += 1

    def finish(self, eng="sp"):
        e = self.engs[eng]
        for k, v in self.cnt.items():
            if k[0] == "dma" and self.waited.get((eng, k), 0) < v:
                e.wait_ge(self._sem(k), v)


class _Stop(Exception):
    pass


STAGE = 99
DEBUG = False
OH1_ENG = "dve"
ACT_OH_EVERY = 1000000


def build_program():
    nc = bass.Bass("TRN2", target_bir_lowering=False)

    def stage(n):
        if STAGE <= n:
            P.stopped = True

    def din(name, shape, dt=F32):
        return nc.dram_tensor(name, shape, dt, kind="ExternalInput").ap()

    x_ext = din("x_ext", [NEXT * 128, D])
    c_col = din("c_col", [128, 8])
    w_ada = din("w_ada", [D, 6 * D])
    b_ada = din("b_ada", [1, 6 * D])
    g1c_d = din("g1c", [128, 8])
    g2c_d = din("g2c", [128, 8])
    onc_d = din("onc", [128, 8])
    w_in = din("w_in", [D, 2304])
    sink_d = din("sink", [1, 8])
    biasA_d = din("biasA", [3, 3, 128, 1024])
    biasBg_d = din("biasBg", [5, 128, 1024])
    biasBs_d = din("biasBs", [22, 128, 1024])
    w_out = din("w_out", [D, D])
    w_query = din("w_query", [D, 2048])
    skT_d = din("skT", [128, 2048])
    u_lay = din("u_lay", [128, 131072])
    v_lay = din("v_lay", [128, 131072])
    fg_d = din("final_g", [1, D])
    ident_d = din("ident", [128, 128])
    iota_d = din("iota", [128, 128])
    out = nc.dram_tensor("out", [TOK, D], F32, kind="ExternalOutput").ap()
    dbg = nc.dram_tensor("dbg", [128, 8192], F32, kind="ExternalOutput").ap() if DEBUG else None
    x1s = nc.dram_tensor("x1s", [TOK, D], F32, kind="Internal").ap()
    uTb = nc.dram_tensor("uTb", [128, 131072], BF16, kind="Internal").ap()
    vb = nc.dram_tensor("vb", [128, 131072], BF16, kind="Internal").ap()

    with ExitStack() as es:
      P = Prog(nc, es)
      try:

        def sbuf(stack, name, shape, dt):
            return stack.enter_context(nc.sbuf_tensor("sb_" + name, shape, dt))

        ps = [es.enter_context(nc.psum_tensor("ps%d" % b, [128, 512], F32)) for b in range(8)]

        def PS(b):
            return [("ps%d" % b, None)]

        ukey = [0]

        def dma(eng, out_ap, in_ap, reads, writes, semkey=None):
            if semkey is None:
                ukey[0] += 1
                semkey = "u%d" % ukey[0]
            return P.op(eng, lambda e: e.dma_start(out=out_ap, in_=in_ap), reads=reads, writes=writes, dma=True, semkey=semkey)

        def dump(ap, col0, width, res):
            if DEBUG:
                dma("pool", dbg[:, col0:col0 + width], ap, [res], [("dbg", col0)])

        def mm(o, lhsT, rhs, start, stop, reads, writes):
            return P.op("pe", lambda e: e.matmul(o, lhsT=lhsT, rhs=rhs, start=start, stop=stop), reads=reads, writes=writes)

        def tr(o, in_, ident, reads, writes):
            return P.op("pe", lambda e: e.transpose(out=o, in_=in_, identity=ident), reads=reads, writes=writes)

        def act(o, in_, func, reads, writes, bias=None, scale=None, accum=None):
            kw = {}
            if bias is not None:
                kw["bias"] = bias
            if scale is not None:
                kw["scale"] = scale
            if accum is not None:
                kw["accum_out"] = accum
            return P.op("act", lambda e: e.activation(out=o, in_=in_, func=func, **kw), reads=reads, writes=writes)

        def tt(eng, o, a, b, op, reads, writes):
            return P.op(eng, lambda e: e.tensor_tensor(out=o, in0=a, in1=b, op=op), reads=reads, writes=writes)

        def tsc(eng, o, a, s1, s2, op0, op1, reads, writes):
            if op1 is None:
                return P.op(eng, lambda e: e.tensor_scalar(out=o, in0=a, scalar1=s1, scalar2=None, op0=op0), reads=reads, writes=writes)
            return P.op(eng, lambda e: e.tensor_scalar(out=o, in0=a, scalar1=s1, scalar2=s2, op0=op0, op1=op1), reads=reads, writes=writes)

        def cp(eng, o, a, reads, writes):
            if eng == "act":
                return P.op("act", lambda e: e.copy(out=o, in_=a), reads=reads, writes=writes)
            return P.op(eng, lambda e: e.tensor_copy(out=o, in_=a), reads=reads, writes=writes)

        ident_f = sbuf(es, "ident_f", [128, 128], F32)
        ident_b = sbuf(es, "ident_b", [128, 128], BF16)
        iota_f = sbuf(es, "iota_f", [128, 128], F32)
        iota_b = sbuf(es, "iota_b", [128, 128], BF16)
        ones_f = sbuf(es, "ones_f", [128, 128], F32)
        g2c = sbuf(es, "g2c", [128, 8], F32)
        a2 = sbuf(es, "a2", [128, 8], F32)
        b2 = sbuf(es, "b2", [128, 8], F32)
        gate2_bc = sbuf(es, "gate2_bc", [128, D], F32)
        fg_bc = sbuf(es, "fg_bc", [128, D], F32)

        dma("sp", ident_f[:], ident_d, [], [("ident_f", None)])
        dma("sp", iota_f[:], iota_d, [], [("iota_f", None)])
        dma("sp", g2c[:], g2c_d, [], [("g2c", None)])
        dma("sp", fg_bc[:], fg_d[0].partition_broadcast(128), [], [("fg_bc", None)])
        cp("dve", ident_b[:], ident_f[:], [("ident_f", None)], [("ident_b", None)])
        cp("dve", iota_b[:], iota_f[:], [("iota_f", None)], [("iota_b", None)])
        P.op("dve", lambda e: e.memset(ones_f[:], 1.0), writes=[("ones_f", None)])

        def norm_transpose(stack_bufs, xt_ap, xt_res, acol, bcol, acol_res, dst_fn, dst_res, tag, bank=0):
            ssq, rstd, xn = stack_bufs
            act(xn[:], xt_ap, AF.Square, [xt_res], [("xn" + tag, None), ("ssq" + tag, None)], accum=ssq[:])
            tsc("dve", rstd[:], ssq[:], 1.0 / D, EPS, ALU.mult, ALU.add, [("ssq" + tag, None)], [("rstd" + tag, None)])
            act(rstd[:], rstd[:], AF.Sqrt, [("rstd" + tag, None)], [("rstd" + tag, None)])
            P.op("dve", lambda e: e.reciprocal(out=rstd[:], in_=rstd[:]), reads=[("rstd" + tag, None)], writes=[("rstd" + tag, None)])
            tsc("dve", xn[:], xt_ap, rstd[:, 0:1], None, ALU.mult, None, [xt_res, ("rstd" + tag, None)], [("xn" + tag, None)])
            psb = ps[bank][:, :].bitcast(BF16)
            for kc in range(8):
                tr(psb[:, kc * 128:(kc + 1) * 128], xn[:, kc * 128:(kc + 1) * 128], ident_b[:],
                   [("xn" + tag, None), ("ident_b", None)], PS(bank))
            for kc in range(8):
                eng = "dve" if kc % 2 == 0 else "act"
                if eng == "dve":
                    tsc("dve", dst_fn(kc), psb[:, kc * 128:(kc + 1) * 128], acol[:, kc:kc + 1], bcol[:, kc:kc + 1],
                        ALU.mult, ALU.add, [acol_res], PS(bank) + [dst_res])
                else:
                    act(dst_fn(kc), psb[:, kc * 128:(kc + 1) * 128], AF.Identity, [acol_res], PS(bank) + [dst_res],
                        bias=bcol[:, kc:kc + 1], scale=acol[:, kc:kc + 1])

        with ExitStack() as ea:
            c_sb = sbuf(ea, "c_sb", [128, 8], F32)
            c_act = sbuf(ea, "c_act", [128, 8], F32)
            g1c = sbuf(ea, "g1c", [128, 8], F32)
            onc = sbuf(ea, "onc", [128, 8], F32)
            a1 = sbuf(ea, "a1", [128, 8], F32)
            b1 = sbuf(ea, "b1", [128, 8], F32)
            adacol = sbuf(ea, "adacol", [128, 32], F32)
            sink_bc = sbuf(ea, "sink_bc", [128, 8], F32)
            expsink = sbuf(ea, "expsink", [128, 8], F32)
            zeros8 = sbuf(ea, "zeros8", [128, 8], F32)
            wo_b = sbuf(ea, "wo_b", [128, 8, D], BF16)

            dma("sp", c_sb[:], c_col, [], [("c_sb", None)])
            dma("sp", g1c[:], g1c_d, [], [("g1c", None)])
            dma("sp", onc[:], onc_d, [], [("onc", None)])
            dma("sp", sink_bc[:], sink_d[0].partition_broadcast(128), [], [("sink_bc", None)])
            P.op("dve", lambda e: e.memset(zeros8[:], 0.0), writes=[("zeros8", None)])
            act(expsink[:], sink_bc[:], AF.Exp, [("sink_bc", None)], [("expsink", None)])
            act(c_act[:], c_sb[:], AF.Silu, [("c_sb", None)], [("c_act", None)])

            with ExitStack() as e0:
                wa = [sbuf(e0, "wa%d" % i, [128, 8, 512], F32) for i in range(2)]
                ada_row = sbuf(e0, "ada_row", [1, 6 * D], F32)
                bada = sbuf(e0, "bada", [1, 6 * D], F32)
                wo_f = sbuf(e0, "wo_f", [128, 8, D], F32)
                gate1_bc = sbuf(e0, "gate1_bc", [128, D], F32)
                dma("sp", bada[:], b_ada, [], [("bada", None)])
                dma("sp", wo_f[:], w_out.rearrange("(kc p) n -> p kc n", p=128), [], [("wo_f", None)])
                w_ada_v = w_ada.rearrange("(kc p) n -> p kc n", p=128)
                for blk in range(12):
                    sl = blk % 2
                    dma("sp", wa[sl][:], w_ada_v[:, :, blk * 512:(blk + 1) * 512], [], [("wa%d" % sl, None)], "wa%d" % sl)
                    for kc in range(8):
                        mm(ps[7][0:1, 0:512], c_act[:, kc:kc + 1], wa[sl][:, kc, :], kc == 0, kc == 7,
                           [("c_act", None), ("wa%d" % sl, None)], PS(7))
                    tt("dve", ada_row[0:1, blk * 512:(blk + 1) * 512], ps[7][0:1, 0:512], bada[0:1, blk * 512:(blk + 1) * 512],
                       ALU.add, [("bada", None)], PS(7) + [("ada_row", blk)])
                for vi, v in enumerate([0, 1, 3, 4]):
                    for kc in range(8):
                        mm(ps[7][:, vi * 8 + kc:vi * 8 + kc + 1], ada_row[0:1, v * D + kc * 128:v * D + (kc + 1) * 128],
                           ident_f[0:1, 0:1], True, True, [("ada_row", None), ("ident_f", None)], PS(7))
                cp("dve", adacol[:], ps[7][:, 0:32], [], PS(7) + [("adacol", None)])
                tsc("dve", a1[:], adacol[:, 8:16], 1.0, None, ALU.add, None, [("adacol", None)], [("a1", None)])
                tt("dve", a1[:], a1[:], g1c[:], ALU.mult, [("g1c", None)], [("a1", None)])
                cp("dve", b1[:], adacol[:, 0:8], [("adacol", None)], [("b1", None)])
                tsc("dve", a2[:], adacol[:, 24:32], 1.0, None, ALU.add, None, [("adacol", None)], [("a2", None)])
                tt("dve", a2[:], a2[:], g2c[:], ALU.mult, [("g2c", None)], [("a2", None)])
                cp("dve", b2[:], adacol[:, 16:24], [("adacol", None)], [("b2", None)])
                for gi, (v, dst, dname) in enumerate([(2, gate1_bc, "gate1_bc"), (5, gate2_bc, "gate2_bc")]):
                    for hf in range(2):
                        mm(ps[1 + hf][:, 0:512], ones_f[0:1, 0:128], ada_row[0:1, v * D + hf * 512:v * D + (hf + 1) * 512],
                           True, True, [("ones_f", None), ("ada_row", None)], PS(1 + hf))
                        cp("dve", dst[:, hf * 512:(hf + 1) * 512], ps[1 + hf][:, 0:512], [], PS(1 + hf) + [(dname, None)])
                for kc in range(8):
                    P.op("dve", lambda e, kc=kc: e.scalar_tensor_tensor(out=wo_b[:, kc, :], in0=wo_f[:, kc, :], scalar=onc[:, kc:kc + 1],
                                                                          in1=gate1_bc[:], op0=ALU.mult, op1=ALU.mult),
                         reads=[("wo_f", None), ("onc", None), ("gate1_bc", None)], writes=[("wo_b", None)])

            P.barrier()
            dump(a1[:], 0, 8, ("a1", None)); dump(b1[:], 8, 8, ("b1", None)); dump(a2[:], 16, 8, ("a2", None)); dump(b2[:], 24, 8, ("b2", None))
            dump(gate2_bc[:], 32, 1024, ("gate2_bc", None)); dump(wo_b[:, 0, :], 1056, 1024, ("wo_b", None))
            stage(1)
            w_in_b = sbuf(ea, "w_in_b", [128, 8, 2304], BF16)
            wkdup = sbuf(ea, "wkdup", [128, 8, 2, 128], BF16)
            biasA = sbuf(ea, "biasA", [128, 3, 1024], BF16)
            biasBg = sbuf(ea, "biasBg", [128, 5, 1024], BF16)
            biasBs = sbuf(ea, "biasBs", [128, 6, 1024], BF16)
            w_in_v = w_in.rearrange("(kc p) n -> p kc n", p=128)
            for hf in range(2):
                dma("pool", w_in_b[:, :, hf * 1152:(hf + 1) * 1152], w_in_v[:, :, hf * 1152:(hf + 1) * 1152],
                    [], [("w_in_b", None)])
            dma("pool", biasA[:], biasA_d[0].rearrange("c k n -> k c n"), [], [("biasA", None)])
            dma("pool", biasBg[:], biasBg_d.rearrange("c k n -> k c n"), [], [("biasBg", None)])
            for g in range(2):
                for dup in range(2):
                    cp("pool", wkdup[:, :, g, dup * 64:(dup + 1) * 64], w_in_b[:, :, 512 + g * 64:512 + (g + 1) * 64],
                       [("w_in_b", None)], [("wkdup", None)])
            hT = sbuf(ea, "hT", [128, 8, 1024], BF16)
            KA = sbuf(ea, "KA", [128, 2, 1024], BF16)
            KB = sbuf(ea, "KB", [128, 4, 1024], BF16)
            QA = sbuf(ea, "QA", [128, 4, 512], BF16)
            QB = sbuf(ea, "QB", [128, 4, 512], BF16)
            VA = sbuf(ea, "VA", [128, 8, 2, 65], BF16)
            VB = sbuf(ea, "VB", [128, 8, 8, 65], BF16)
            NXT = 3
            xts = [sbuf(ea, "xt%d" % i, [128, D], F32) for i in range(NXT)]
            xrs = [sbuf(ea, "xr%d" % i, [128, D], F32) for i in range(2)]
            nbs = [(sbuf(ea, "ssqA%d" % i, [128, 1], F32), sbuf(ea, "rstdA%d" % i, [128, 1], F32), sbuf(ea, "xnA%d" % i, [128, D], BF16))
                   for i in range(2)]
            Pb = [sbuf(ea, "Pb0", [128, 3, 1024], BF16), sbuf(ea, "Pb1", [128, 6, 1024], BF16),
                  sbuf(ea, "Pb2", [128, 3, 1024], BF16), sbuf(ea, "Pb3", [128, 6, 1024], BF16)]
            den = sbuf(ea, "den", [128, 8], F32)
            rden = sbuf(ea, "rden", [128, 8], F32)
            o_t = sbuf(ea, "o_t", [128, 512], BF16)
            oss = sbuf(ea, "oss", [128, 1], F32)
            orr = sbuf(ea, "orr", [128, 1], F32)
            on_a = sbuf(ea, "on_a", [128, 512], BF16)
            on_b = sbuf(ea, "on_b", [128, 512], BF16)
            mixT = sbuf(ea, "mixT", [128, 8, 128], BF16)
            P.op("dve", lambda e: e.memset(VA[:], 1.0), writes=[("VA", None)])
            P.op("dve", lambda e: e.memset(VB[:], 1.0), writes=[("VB", None)])

            sstate = {"sb": 0, "pb": 0, "x": 0}

            def attn_tile(pb, nch, kT, q, bias, V, sinkt, dst, dst_name, rd, pvb):
                Pt = Pb[pb]
                pname = "Pb%d" % pb
                for c in range(nch):
                    for grp in range(2):
                        bk = 3 + sstate["sb"]
                        sstate["sb"] ^= 1
                        mm(ps[bk][:, 0:512], ident_b[:], bias(c)[:, grp * 512:(grp + 1) * 512], True, False,
                           [("ident_b", None)] + rd, PS(bk))
                        for r in range(4):
                            h = grp + 2 * r
                            mm(ps[bk][:, r * 128:(r + 1) * 128], kT(h, c), q(h), False, r == 3, rd, PS(bk))
                        act(Pt[:, c, grp * 512:(grp + 1) * 512], ps[bk][:, 0:512], AF.Exp, [], PS(bk) + [(pname, None)])

                def pv_phase():
                    for h in range(8):
                        bk = pvb[h // 4]
                        for c in range(nch):
                            pos = (h % 2) * 4 + h // 2
                            mm(ps[bk][:, (h % 4) * 65:(h % 4) * 65 + 65], Pt[:, c, pos * 128:(pos + 1) * 128], V(h, c), c == 0, c == nch - 1,
                               [(pname, None)] + rd, PS(bk))

                def norm_phase():
                    for bnk in range(2):
                        pv = ps[pvb[bnk]][:, 0:260].rearrange("p (h e) -> p h e", e=65)
                        tt("dve", den[:, bnk * 4:(bnk + 1) * 4], pv[:, :, 64], sinkt[:, bnk * 4:(bnk + 1) * 4], ALU.add,
                           [("expsink", None), ("zeros8", None)], PS(pvb[bnk]) + [("den", None)])
                    P.op("dve", lambda e: e.reciprocal(out=rden[:], in_=den[:]), reads=[("den", None)], writes=[("rden", None)])
                    for bnk in range(2):
                        pv = ps[pvb[bnk]][:, 0:260].rearrange("p (h e) -> p h e", e=65)
                        ov = o_t[:, bnk * 256:(bnk + 1) * 256].rearrange("p (h e) -> p h e", e=64)
                        rb = rden[:, bnk * 4:(bnk + 1) * 4].unsqueeze(2).broadcast_to([128, 4, 64])
                        tt("dve", ov, pv[:, :, 0:64], rb, ALU.mult, [("rden", None)], PS(pvb[bnk]) + [("o_t", None)])
                    act(dst[:], o_t[:], AF.Square, [("o_t", None)], [(dst_name, None), ("oss", None)], accum=oss[:])
                    tsc("dve", orr[:], oss[:], 1.0 / 512, EPS, ALU.mult, ALU.add, [("oss", None)], [("orr", None)])
                    act(orr[:], orr[:], AF.Sqrt, [("orr", None)], [("orr", None)])
                    P.op("dve", lambda e: e.reciprocal(out=orr[:], in_=orr[:]), reads=[("orr", None)], writes=[("orr", None)])
                    tsc("dve", dst[:], o_t[:], orr[:, 0:1], None, ALU.mult, None, [("o_t", None), ("orr", None)], [(dst_name, None)])

                return pv_phase, norm_phase

            sp_off = {0: 0, 1: 6, 30: 11, 31: 16}
            for s in range(8):
                if s == 1:
                    stage(6)
                def x_load(tl, s=s):
                    te = 4 * s + tl
                    xs = tl % NXT
                    dma("sp", xts[xs][:], x_ext[te * 128:(te + 1) * 128, :], [], [("xt%d" % xs, None)], "xt%d" % xs)

                def nt_stats(tl):
                    if tl + NXT - 1 < 8:
                        x_load(tl + NXT - 1)
                    xs = tl % NXT
                    xt = xts[xs]
                    ssq, rstd, xn = nbs[tl % 2]
                    tg = "A%d" % (tl % 2)
                    xres = ("xt%d" % xs, None)
                    act(xn[:], xt[:], AF.Square, [xres], [("xn" + tg, None), ("ssq" + tg, None)], accum=ssq[:])
                    tsc("dve", rstd[:], ssq[:], 1.0 / D, EPS, ALU.mult, ALU.add, [("ssq" + tg, None)], [("rstd" + tg, None)])
                    act(rstd[:], rstd[:], AF.Sqrt, [("rstd" + tg, None)], [("rstd" + tg, None)])
                    P.op("dve", lambda e: e.reciprocal(out=rstd[:], in_=rstd[:]), reads=[("rstd" + tg, None)], writes=[("rstd" + tg, None)])
                    tsc("dve", xn[:], xt[:], rstd[:, 0:1], None, ALU.mult, None, [xres, ("rstd" + tg, None)], [("xn" + tg, None)])

                def nt_rest(tl):
                    xs = tl % 2
                    xn = nbs[xs][2]
                    tg = "A%d" % xs
                    psb0 = ps[0][:, :].bitcast(BF16)
                    psb7 = ps[7][:, :].bitcast(BF16)
                    for kc in range(8):
                        pb_, bk_ = (psb0, 0) if kc < 4 else (psb7, 7)
                        tr(pb_[:, (kc % 4) * 128:(kc % 4 + 1) * 128], xn[:, kc * 128:(kc + 1) * 128], ident_b[:],
                           [("xn" + tg, None), ("ident_b", None)], PS(bk_))
                    for kk in range(4):
                        for kc in (kk, kk + 4):
                            dstk = hT[:, kc, tl * 128:(tl + 1) * 128]
                            if kc < 4:
                                tsc("dve", dstk, psb0[:, kc * 128:(kc + 1) * 128], a1[:, kc:kc + 1], b1[:, kc:kc + 1],
                                    ALU.mult, ALU.add, [("a1", None)], PS(0) + [("hT", (tl, kc))])
                            else:
                                act(dstk, psb7[:, (kc - 4) * 128:(kc - 3) * 128], AF.Identity, [("a1", None)], PS(7) + [("hT", (tl, kc))],
                                    bias=b1[:, kc:kc + 1], scale=a1[:, kc:kc + 1])
                    for kc in range(8):
                        mm(ps[1][:, 0:512], hT[:, kc, tl * 128:(tl + 1) * 128], w_in_b[:, kc, 1792:2304], kc == 0, kc == 7,
                           [("hT", (tl, kc)), ("w_in_b", None)], PS(1))
                        mm(ps[2][:, 0:128], hT[:, kc, tl * 128:(tl + 1) * 128], w_in_b[:, kc, 640:768], kc == 0, kc == 7,
                           [("hT", (tl, kc)), ("w_in_b", None)], PS(2))
                    cp("act", VB[:, tl, :, 0:64], ps[1][:, 0:512].rearrange("p (h e) -> p h e", e=64), [], PS(1) + [("VB", tl)])
                    cp("dve", VA[:, tl, :, 0:64], ps[2][:, 0:128].rearrange("p (h e) -> p h e", e=64), [], PS(2) + [("VA", tl)])

                if s == 0:
                    for tl0 in range(NXT - 1):
                        x_load(tl0)
                nt_stats(0)
                for tl in range(8):
                    if tl + 1 < 8:
                        nt_stats(tl + 1)
                    nt_rest(tl)
                if s + 1 < 8:
                    for tl0 in range(NXT - 1):
                        x_load(tl0, s + 1)
                stage(2)
                pj = 0
                for hf in range(2):
                    for ch in range(6):
                        bk = 1 + pj % 2
                        pj += 1
                        for kc in range(8):
                            lw = wkdup[:, kc, ch, :] if ch < 2 else w_in_b[:, kc, 1280 + (ch - 2) * 128:1280 + (ch - 1) * 128]
                            mm(ps[bk][:, 0:512], lw, hT[:, kc, hf * 512:(hf + 1) * 512], kc == 0, kc == 7,
                               [("hT", None), ("w_in_b", None), ("wkdup", None)], PS(bk))
                        if ch < 2:
                            cp("act" if pj % 2 else "dve", KA[:, ch, hf * 512:(hf + 1) * 512], ps[bk][:, 0:512], [], PS(bk) + [("KA", None)])
                        else:
                            cp("act" if pj % 2 else "dve", KB[:, ch - 2, hf * 512:(hf + 1) * 512], ps[bk][:, 0:512], [], PS(bk) + [("KB", None)])
                for ch in range(8):
                    bk = 1 + pj % 2
                    pj += 1
                    col0 = ch * 128 if ch < 4 else 768 + (ch - 4) * 128
                    for kc in range(8):
                        mm(ps[bk][:, 0:512], w_in_b[:, kc, col0:col0 + 128], hT[:, kc, 256:768], kc == 0, kc == 7,
                           [("hT", None), ("w_in_b", None)], PS(bk))
                    dq = QA[:, ch, :] if ch < 4 else QB[:, ch - 4, :]
                    dn = "QA" if ch < 4 else "QB"
                    act(dq, ps[bk][:, 0:512], AF.Identity, [], PS(bk) + [(dn, None)], scale=0.125)
                stage(3)
                def s_phase(j):
                    i = 4 * s + j
                    tl = j + 2
                    pbo = 2 * (j % 2)
                    if 2 <= i < 18:
                        pc = i - 2
                        for (src_d, dst_d, nm) in ((u_lay, uTb, "uTb"), (v_lay, vb, "vb")):
                            dma("pool", dst_d[:, pc * 8192:(pc + 1) * 8192].rearrange("p (a b) -> p a b", b=2048),
                                src_d[:, pc * 8192:(pc + 1) * 8192].rearrange("p (a b) -> p a b", b=2048), [], [(nm, pc)], "cv" + nm)
                    rdA = [("KA", None), ("QA", None), ("VA", None), ("biasA", None), ("biasBs", None)]
                    if i in (0, 31):
                        dma("pool", biasBs[:, 0:3, :], biasA_d[1 if i == 0 else 2].rearrange("c k n -> k c n"),
                            [], [("biasBs", None)], "bs")
                        bfa = lambda c: biasBs[:, c, :]
                    else:
                        bfa = lambda c: biasA[:, c, :]
                    wpv, wnorm = attn_tile(pbo, 3,
                              lambda h, c, tl=tl: KA[(h % 2) * 64:(h % 2) * 64 + 64, h // 4, (tl - 1 + c) * 128:(tl + c) * 128],
                              lambda h, j=j: QA[(h % 2) * 64:(h % 2) * 64 + 64, h // 2, j * 128:(j + 1) * 128],
                              bfa,
                              lambda h, c, tl=tl: VA[:, tl - 1 + c, h // 4, :],
                              expsink, on_a, "on_a", rdA, (5, 6))
                    stage(4)
                    rdB = [("KB", None), ("QB", None), ("VB", None), ("biasBg", None), ("biasBs", None)]
                    if i in sp_off:
                        nchb = 6 if i in (0, 31) else 5
                        dma("pool", biasBs[:, 0:nchb, :], biasBs_d[sp_off[i]:sp_off[i] + nchb].rearrange("c k n -> k c n"),
                            [], [("biasBs", None)], "bs")
                        c0 = tl - 3 if i == 31 else tl - 2
                        bfn = lambda c: biasBs[:, c, :]
                    else:
                        nchb = 5
                        c0 = tl - 2
                        bfn = lambda c: biasBg[:, c, :]
                    bpv, bnorm = attn_tile(pbo + 1, nchb,
                              lambda h, c, c0=c0: KB[(h % 2) * 64:(h % 2) * 64 + 64, h // 2, (c0 + c) * 128:(c0 + c + 1) * 128],
                              lambda h, j=j: QB[(h % 2) * 64:(h % 2) * 64 + 64, h // 2, j * 128:(j + 1) * 128],
                              bfn,
                              lambda h, c, c0=c0: VB[:, c0 + c, h, :],
                              zeros8, on_b, "on_b", rdB, (1, 2))

                    def pv():
                        wpv()
                        bpv()

                    def norms():
                        wnorm()
                        bnorm()
                        if i == 0:
                            dump(on_a[:], 2080, 512, ("on_a", None)); dump(on_b[:], 2592, 512, ("on_b", None))
                            dump(hT[:, 0, 256:384], 4128, 128, ("hT", None))

                    def tail():
                        stage(5)
                        psb = ps[0][:, :].bitcast(BF16)
                        for kc in range(8):
                            src_ = on_a if kc < 4 else on_b
                            tr(psb[:, kc * 128:(kc + 1) * 128], src_[:, (kc % 4) * 128:(kc % 4 + 1) * 128], ident_b[:],
                               [("on_a", None), ("on_b", None), ("ident_b", None)], PS(0))
                        cp("act", mixT[:].rearrange("p k t -> p (k t)"), psb[:, 0:1024], [], PS(0) + [("mixT", None)])
                        xr = xrs[i % 2]
                        dma("sp", xr[:], x_ext[(i + 2) * 128:(i + 3) * 128, :], [], [("xr%d" % (i % 2), None)], "xr%d" % (i % 2))
                        for hf in range(2):
                            for kc in range(8):
                                mm(ps[1 + hf][:, 0:512], mixT[:, kc, :], wo_b[:, kc, hf * 512:(hf + 1) * 512], kc == 0, kc == 7,
                                   [("mixT", None), ("wo_b", None)], PS(1 + hf))
                            tt("dve", xr[:, hf * 512:(hf + 1) * 512], ps[1 + hf][:, 0:512], xr[:, hf * 512:(hf + 1) * 512], ALU.add,
                               [("xr%d" % (i % 2), None)], PS(1 + hf) + [("xr%d" % (i % 2), None)])
                        if i == 0:
                            dump(xr[:], 3104, 1024, ("xr0", None))
                        dma("sp", x1s[i * 128:(i + 1) * 128, :], xr[:], [("xr%d" % (i % 2), None)], [("x1s", i)], "x1o%d" % (i % 2))

                    return pv, norms, tail

                prev = s_phase(0)
                prev[0]()
                for j in range(1, 4):
                    cur = s_phase(j)
                    prev[1]()
                    prev[2]()
                    cur[0]()
                    prev = cur
                prev[1]()
                prev[2]()

        P.barrier()
        stage(7)
        with ExitStack() as eb:
            wq = sbuf(eb, "wq", [128, 8, 2048], BF16)
            skT = sbuf(eb, "skT", [128, 2048], BF16)
            dma("pool", wq[:], w_query.rearrange("(kc p) n -> p kc n", p=128), [], [("wq", None)])
            dma("pool", skT[:], skT_d, [], [("skT", None)])
            stage(8)
            Gs = sbuf(eb, "Gs", [128, 256, 128], BF16)
            h2Ts = [sbuf(eb, "h2T%d" % i, [128, 8, 256], BF16) for i in range(2)]
            qhT = sbuf(eb, "qhT", [128, 16, 256], BF16)
            S = sbuf(eb, "S", [128, 16, 128], F32)
            V16 = sbuf(eb, "V16", [128, 16, 16], F32)
            I16 = sbuf(eb, "I16", [128, 16, 16], U32)
            I16f = sbuf(eb, "I16f", [128, 16, 16], F32)
            cand = sbuf(eb, "cand", [128, 8, 256], F32)
            eq = cand[:].rearrange("p h (a b) -> p h a b", b=16)
            VS = sbuf(eb, "VS", [128, 8, 16], F32)
            CI = sbuf(eb, "CI", [128, 8, 16], U32)
            CIf = sbuf(eb, "CIf", [128, 8, 16], F32)
            CAf = sbuf(eb, "CAf", [128, 8, 16], F32)
            CBf = sbuf(eb, "CBf", [128, 8, 16], F32)
            E1 = sbuf(eb, "E1", [128, 128], F32)
            E2 = sbuf(eb, "E2", [128, 128], F32)
            Gt = sbuf(eb, "Gt", [128, 128], F32)
            gsum = sbuf(eb, "gsum", [128, 8], F32)
            E1T = sbuf(eb, "E1T", [128, 256], F32)
            E2T = sbuf(eb, "E2T", [128, 256], F32)
            GT = sbuf(eb, "GT", [128, 256], F32)
            nE1T = sbuf(eb, "nE1T", [128, 256], F32)
            GTs = sbuf(eb, "GTs", [128, 256], F32)
            NOH = 6
            OH1 = [sbuf(eb, "OH1_%d" % i, [128, 128], BF16) for i in range(NOH)]
            OH2 = [sbuf(eb, "OH2_%d" % i, [128, 128], BF16) for i in range(NOH)]
            Uc = [sbuf(eb, "Uc%d" % i, [128, 2, 8, 128], BF16) for i in range(2)]
            Vc = [sbuf(eb, "Vc%d" % i, [128, 2, D], BF16) for i in range(4)]
            At = [sbuf(eb, "At%d" % i, [128, 256], BF16) for i in range(4)]
            x1g = [[sbuf(eb, "x1g%d_%d" % (p_, i), [128, D], F32) for i in range(2)] for p_ in range(2)]
            yt = sbuf(eb, "yt", [128, D], F32)
            nb2 = (sbuf(eb, "ssqB", [128, 1], F32), sbuf(eb, "rstdB", [128, 1], F32), sbuf(eb, "xnB", [128, D], BF16))
            fjunk = sbuf(eb, "fjunk", [128, D], BF16)
            fss = sbuf(eb, "fss", [128, 1], F32)
            frs = sbuf(eb, "frs", [128, 1], F32)
            PB = 7

            def top16_batch(items):
                for (s_, sr, vd, idd, vr, ir) in items:
                    P.op("dve", lambda e, s_=s_, vd=vd: e.max(out=vd[:, 0:8], in_=s_), reads=[sr], writes=[vr])
                yield
                for (s_, sr, vd, idd, vr, ir) in items:
                    P.op("dve", lambda e, s_=s_, vd=vd, idd=idd: e.max_index(out=idd[:, 0:8], in_max=vd[:, 0:8], in_values=s_),
                         reads=[sr, vr], writes=[ir])
                yield
                for (s_, sr, vd, idd, vr, ir) in items:
                    P.op("dve", lambda e, s_=s_, vd=vd: e.match_replace(out=s_, in_to_replace=vd[:, 0:8], in_values=s_, imm_value=-1e30),
                         reads=[vr], writes=[sr])
                yield
                for (s_, sr, vd, idd, vr, ir) in items:
                    P.op("dve", lambda e, s_=s_, vd=vd: e.max(out=vd[:, 8:16], in_=s_), reads=[sr], writes=[vr])
                yield
                for (s_, sr, vd, idd, vr, ir) in items:
                    P.op("dve", lambda e, s_=s_, vd=vd, idd=idd: e.max_index(out=idd[:, 8:16], in_max=vd[:, 8:16], in_values=s_),
                         reads=[sr, vr], writes=[ir])
                yield

            def prep(gi):
                par = gi % 2
                h2T = h2Ts[par]
                hn = "h2T%d" % par
                for tk in range(2):
                    ti = 2 * gi + tk
                    xn_ = "x1g%d_%d" % (par, tk)
                    dma("sp", x1g[par][tk][:], x1s[ti * 128:(ti + 1) * 128, :], [("x1s", ti)], [(xn_, None)], xn_)
                    norm_transpose(nb2, x1g[par][tk][:], (xn_, None), a2, b2, ("a2", None),
                                   lambda kc, tk=tk: h2T[:, kc, tk * 128:(tk + 1) * 128], (hn, None), "B", bank=PB)
                    yield
                stage(9)
                for c in range(16):
                    for kc in range(8):
                        mm(ps[PB][:, 0:256], wq[:, kc, c * 128:(c + 1) * 128], h2T[:, kc, :], kc == 0, kc == 7,
                           [("wq", None), (hn, None)], PS(PB))
                    cp("act", qhT[:, c, :], ps[PB][:, 0:256], [], PS(PB) + [("qhT", None)])
                    yield
                for tk in range(2):
                    for cg in range(4):
                        for cc in range(4):
                            c = cg * 4 + cc
                            mm(ps[PB][:, cc * 128:(cc + 1) * 128], qhT[:, c, tk * 128:(tk + 1) * 128], skT[:, c * 128:(c + 1) * 128],
                               True, True, [("qhT", None), ("skT", None)], PS(PB))
                        cp("act", S[:, cg * 4:(cg + 1) * 4, :].rearrange("p a b -> p (a b)"), ps[PB][:, 0:512],
                           [], PS(PB) + [("S", cg * 4 + q_) for q_ in range(4)])
                        yield
                    if gi == 0 and tk == 0:
                        dump(S[:, 0:2, :].rearrange("p a b -> p (a b)"), 4256, 256, ("S", None))
                        dump(h2T[:, 0, :], 5664, 256, (hn, None))
                    stage(10)
                    for half_ in range(2):
                        yield from top16_batch([(S[:, c, :], ("S", c), V16[:, c, :], I16[:, c, :], ("V16", c), ("I16", c))
                                                for c in range(half_ * 8, half_ * 8 + 8)])
                    V4 = V16[:].rearrange("p (h two) k -> p h two k", two=2)
                    c4 = cand[:].rearrange("p h (a b) -> p h a b", b=16)
                    for hq in range(4):
                        hs = slice(2 * hq, 2 * hq + 2)
                        tt("dve", c4[:, hs], V4[:, hs, 0, :].unsqueeze(3).broadcast_to([128, 2, 16, 16]),
                           V4[:, hs, 1, :].unsqueeze(2).broadcast_to([128, 2, 16, 16]), ALU.add,
                           [("V16", None)], [("cand", 2 * hq), ("cand", 2 * hq + 1)])
                        yield
                    yield from top16_batch([(cand[:, h, :], ("cand", h), VS[:, h, :], CI[:, h, :], ("VS", h), ("CI", h)) for h in range(8)])
                    stage(11)
                    MAGIC = 12582912.0
                    cp("dve", CIf[:], CI[:], [("CI", None)], [("CIf", None)])
                    tsc("dve", CAf[:], CIf[:], 0.0625, -0.46875, ALU.mult, ALU.add, [("CIf", None)], [("CAf", None)])
                    tsc("dve", CAf[:], CAf[:], MAGIC, None, ALU.add, None, [("CAf", None)], [("CAf", None)])
                    tsc("dve", CAf[:], CAf[:], -MAGIC, None, ALU.add, None, [("CAf", None)], [("CAf", None)])
                    P.op("dve", lambda e: e.scalar_tensor_tensor(out=CBf[:], in0=CAf[:], scalar=-16.0, in1=CIf[:], op0=ALU.mult, op1=ALU.add),
                         reads=[("CAf", None), ("CIf", None)], writes=[("CBf", None)])
                    cp("dve", I16f[:], I16[:], [("I16", None)], [("I16f", None)])
                    yield
                    I4 = I16f[:].rearrange("p (h two) k -> p h two k", two=2)
                    io16 = iota_f[:, 0:16].unsqueeze(1).unsqueeze(1).broadcast_to([128, 2, 16, 16])
                    for (cf, cfn, two, Ed, Edn) in ((CAf, "CAf", 0, E1, "E1"), (CBf, "CBf", 1, E2, "E2")):
                        for hq in range(4):
                            hs = slice(2 * hq, 2 * hq + 2)
                            cres = [("cand", 2 * hq), ("cand", 2 * hq + 1)]
                            tt("dve", eq[:, hs], io16, cf[:, hs].unsqueeze(3).broadcast_to([128, 2, 16, 16]), ALU.is_equal,
                               [("iota_f", None), (cfn, None)], cres)
                            yield
                            tt("dve", eq[:, hs], eq[:, hs], I4[:, hs, two, :].unsqueeze(2).broadcast_to([128, 2, 16, 16]), ALU.mult,
                               [("I16f", None)], cres)
                            yield
                            P.op("dve", lambda e, Ed=Ed, hs=hs: e.tensor_reduce(out=Ed[:].rearrange("p (h k) -> p h k", k=16)[:, hs],
                                                                                in_=eq[:, hs], axis=AX.X, op=ALU.add),
                                 reads=cres, writes=[(Edn, hq)])
                            yield
                    G3 = Gt[:].rearrange("p (h k) -> p h k", k=16)
                    tt("dve", G3, VS[:], VS[:, :, 0:1].broadcast_to([128, 8, 16]), ALU.subtract, [("VS", None)], [("Gt", None)])
                    act(Gt[:], Gt[:], AF.Exp, [("Gt", None)], [("Gt", None)])
                    P.op("dve", lambda e: e.tensor_reduce(out=gsum[:], in_=G3, axis=AX.X, op=ALU.add),
                         reads=[("Gt", None)], writes=[("gsum", None)])
                    P.op("dve", lambda e: e.reciprocal(out=gsum[:], in_=gsum[:]), reads=[("gsum", None)], writes=[("gsum", None)])
                    tt("dve", G3, G3, gsum[:].unsqueeze(2).broadcast_to([128, 8, 16]), ALU.mult, [("gsum", None)], [("Gt", None)])
                    yield
                    if gi == 0 and tk == 0:
                        dump(V16[:].rearrange("p a b -> p (a b)"), 4512, 256, ("V16", None))
                        dump(I16f[:].rearrange("p a b -> p (a b)"), 4768, 256, ("I16f", None))
                        dump(VS[:].rearrange("p a b -> p (a b)"), 5024, 128, ("VS", None))
                        dump(CIf[:].rearrange("p a b -> p (a b)"), 5152, 128, ("CIf", None))
                        dump(E1[:], 5280, 128, ("E1", None)); dump(E2[:], 5408, 128, ("E2", None)); dump(Gt[:], 5536, 128, ("Gt", None))
                    for qi, (src_, sn, dstT, dn, sc_) in enumerate(((E1, "E1", E1T, "E1T", 1.0), (E2, "E2", E2T, "E2T", 1.0),
                                                                    (Gt, "Gt", GT, "GT", 1.0))):
                        tr(ps[PB][:, 0:128], src_[:], ident_f[:], [(sn, None), ("ident_f", None)], PS(PB))
                        act(dstT[:, tk * 128:(tk + 1) * 128], ps[PB][:, 0:128], AF.Identity, [], PS(PB) + [(dn, None)], scale=sc_)
                        if dn == "E1T":
                            act(nE1T[:, tk * 128:(tk + 1) * 128], ps[PB][:, 0:128], AF.Identity, [], PS(PB) + [("nE1T", None)], scale=-4.0)
                        if dn == "GT":
                            act(GTs[:, tk * 128:(tk + 1) * 128], ps[PB][:, 0:128], AF.Identity, [], PS(PB) + [("GTs", None)], scale=1.0 / 1.125)
                        yield

            def gbuild(gi):
                for t in range(256):
                    sl = t % NOH
                    on_act = (t % ACT_OH_EVERY == ACT_OH_EVERY - 1)
                    gsrc, gname = (GTs, "GTs") if on_act else (GT, "GT")
                    tsc("dve", OH2[sl][:], iota_b[:], E2T[:, t:t + 1], gsrc[:, t:t + 1], ALU.is_equal, ALU.mult,
                        [("iota_b", None), ("E2T", None), (gname, None)], [("OH2_%d" % sl, None)])
                    if on_act:
                        act(OH1[sl][:], iota_b[:], AF.Derivative_Erf, [("iota_b", None), ("nE1T", None)], [("OH1_%d" % sl, None)],
                            bias=nE1T[:, t:t + 1], scale=4.0)
                    else:
                        tsc("dve", OH1[sl][:], iota_b[:], E1T[:, t:t + 1], None, ALU.is_equal, None,
                            [("iota_b", None), ("E1T", None)], [("OH1_%d" % sl, None)])
                    bk = 4 + (t // 4) % 4
                    mm(ps[bk][:, (t % 4) * 128:(t % 4 + 1) * 128], OH2[sl][:], OH1[sl][:], True, True,
                       [("OH2_%d" % sl, None), ("OH1_%d" % sl, None)], PS(bk))
                    if t % 4 == 3:
                        cp("act", Gs[:, t - 3:t + 1, :].rearrange("p a b -> p (a b)"), ps[bk][:, 0:512],
                           [], PS(bk) + [("Gs", None)])
                if gi == 0:
                    dump(Gs[:, 0, :], 6048, 128, ("Gs", None)); dump(Gs[:, 200, :], 6176, 128, ("Gs", None))

            LA = 2
            NZ = 3

            def sweep(gi, gen):
                par = gi % 2
                h2T = h2Ts[par]
                hn = "h2T%d" % par

                def zstage(c):
                    sc, cl = c // 2, c % 2
                    us = sc % 2
                    if cl == 0:
                        dma("sp", Uc[us][:].rearrange("p a b c -> p (a b c)"), uTb[:, sc * 2048:(sc + 1) * 2048],
                            [("uTb", None)], [("Uc%d" % us, None)], "Uc%d" % us)
                        vs_ = sc % 4
                        dma("sp", Vc[vs_][:].rearrange("p a b -> p (a b)"), vb[:, sc * 2048:(sc + 1) * 2048],
                            [("vb", None)], [("Vc%d" % vs_, None)], "Vc%d" % vs_)
                    bk = 4 + c % NZ
                    for kc in range(8):
                        mm(ps[bk][:, 0:256], Uc[us][:, cl, kc, :], h2T[:, kc, :], kc == 0, kc == 7,
                           [("Uc%d" % us, None), (hn, None)], PS(bk))
                    act(ps[bk][:, 0:256], ps[bk][:, 0:256], AF.Gelu_apprx_tanh, [], PS(bk))
                    tt("dve", At[c % 4][:], ps[bk][:, 0:256], Gs[:, :, c], ALU.mult, [("Gs", None)], PS(bk) + [("At%d" % (c % 4), None)])

                def ostage(c):
                    sc, cl = c // 2, c % 2
                    vs_ = sc % 4
                    for tk in range(2):
                        for hf in range(2):
                            mm(ps[tk * 2 + hf][:, 0:512], At[c % 4][:, tk * 128:(tk + 1) * 128], Vc[vs_][:, cl, hf * 512:(hf + 1) * 512],
                               c == 0, c == 127, [("At%d" % (c % 4), None), ("Vc%d" % vs_, None)], PS(tk * 2 + hf))

                for step in range(128 + LA):
                    if step < 128:
                        zstage(step)
                    if step >= LA:
                        ostage(step - LA)
                    if gen is not None and step >= 4:
                        next(gen, None)

            def finalize(gi):
                par = gi % 2
                for tk in range(2):
                    ti = 2 * gi + tk
                    xn_ = "x1g%d_%d" % (par, tk)
                    for hf in range(2):
                        tt("dve", yt[:, hf * 512:(hf + 1) * 512], ps[tk * 2 + hf][:, 0:512], gate2_bc[:, hf * 512:(hf + 1) * 512], ALU.mult,
                           [("gate2_bc", None)], PS(tk * 2 + hf) + [("yt", None)])
                    if ti == 0:
                        dump(yt[:], 6304, 1024, ("yt", None))
                    tt("pool", yt[:], yt[:], x1g[par][tk][:], ALU.add, [(xn_, None)], [("yt", None)])
                    act(fjunk[:], yt[:], AF.Square, [("yt", None)], [("fjunk", None), ("fss", None)], accum=fss[:])
                    tsc("dve", frs[:], fss[:], 1.0 / D, EPS, ALU.mult, ALU.add, [("fss", None)], [("frs", None)])
                    act(frs[:], frs[:], AF.Sqrt, [("frs", None)], [("frs", None)])
                    P.op("dve", lambda e: e.reciprocal(out=frs[:], in_=frs[:]), reads=[("frs", None)], writes=[("frs", None)])
                    o_ = x1g[par][tk]
                    P.op("dve", lambda e, o_=o_: e.scalar_tensor_tensor(out=o_[:], in0=yt[:], scalar=frs[:, 0:1], in1=fg_bc[:],
                                                                          op0=ALU.mult, op1=ALU.mult),
                         reads=[("yt", None), ("frs", None), ("fg_bc", None)], writes=[(xn_, None)])
                    dma("sp", out[ti * 128:(ti + 1) * 128, :], o_[:], [(xn_, None)], [("out", ti)], "oo" + xn_)

            for _ in prep(0):
                pass
            for gi in range(16):
                stage(12)
                gbuild(gi)
                stage(13)
                gen = prep(gi + 1) if gi + 1 < 16 else None
                sweep(gi, gen)
                if gen is not None:
                    for _ in gen:
                        pass
                stage(14)
                finalize(gi)
      except _Stop:
        pass
      P.finish("sp")
      build_program.stats = dict(nops=len(P.ops), nwaits=P.nwaits, nsems=len(P.sems))
    return nc


def _t5_bucket_np(rel):
    import jax
    import jax.numpy as jnp
    import math
    cpu = jax.devices("cpu")[0]
    with jax.default_device(cpu):
        rel = jnp.asarray(rel, dtype=jnp.int32)
        half = 16
        max_exact = 8
        ret = jnp.where(rel > 0, half, 0)
        n = jnp.abs(rel)
        nf = jnp.maximum(n, 1).astype(jnp.float32)
        large = max_exact + (jnp.log(nf / max_exact) / math.log(128 / max_exact) * (half - max_exact)).astype(jnp.int32)
        large = jnp.minimum(large, half - 1)
        return np.asarray(ret + jnp.where(n < max_exact, n, large))


def _col(v):
    return np.ascontiguousarray(np.asarray(v, np.float32).reshape(8, 128).T)


def _bias_a(t5_table):
    k = np.arange(128)[:, None]
    q = np.arange(128)[None, :]
    outp = np.full((3, 3, 128, 8, 128), NEGM, np.float32)
    for c in range(3):
        rel = (c * 128 + k - 128) - q
        band = np.abs(rel) <= 128
        bk = _t5_bucket_np(rel)
        vals = t5_table[bk]
        vals = np.transpose(vals, (0, 2, 1))[:, _HORDER, :]
        m = np.broadcast_to(band[:, None, :], vals.shape)
        blk = np.where(m, vals, np.float32(NEGM)).astype(np.float32)
        outp[0, c] = blk
        outp[1, c] = blk if c != 0 else NEGM
        outp[2, c] = blk if c != 2 else NEGM
    return outp.reshape(3, 3, 128, 1024)


def _bias_b_chunk(rpb, r0, kp):
    outp = np.full((128, 8, 128), NEGM, np.float32)
    kk = np.arange(128)
    kr, kcol = kk // 64, kk % 64
    y = 2 * kp + kr
    qq = np.arange(128)
    qr, c = qq // 64, qq % 64
    r = r0 + qr
    rs = np.clip(r - 4, 0, 120)
    cs = np.clip(c - 8, 0, 48)
    Y = y[:, None]
    KC = kcol[:, None]
    valid = (Y >= rs[None, :]) & (Y < rs[None, :] + 8) & (KC >= cs[None, :]) & (KC < cs[None, :] + 16) & (Y >= 0) & (Y < 128)
    ro = np.clip(Y - r[None, :] + 7, 0, 14)
    co = np.clip(KC - c[None, :] + 15, 0, 30)
    vals = rpb[:, ro, co]
    vals = np.transpose(vals, (1, 0, 2))[:, _HORDER, :]
    m = np.broadcast_to(valid[:, None, :], vals.shape)
    return np.where(m, vals, np.float32(NEGM)).astype(np.float32)


_PREP_ONLY = False
_HORDER = [0, 2, 4, 6, 1, 3, 5, 7]
_SP_LIST = [(0, list(range(-2, 4))), (1, list(range(-1, 4))), (30, list(range(28, 33))), (31, list(range(28, 34)))]


def kernel(x, c, w_ada, b_ada, norm1_g, w_in, sink_a, t5_table, rpb_b, out_norm_a, out_norm_b,
           w_out, norm2_g, w_query, sub_keys, u_experts, v_experts, final_g):
    f = lambda a: np.ascontiguousarray(np.asarray(a, dtype=np.float32))
    x = f(x); c = f(c); w_ada = f(w_ada)[0]; b_ada = f(b_ada); w_in = f(w_in)[0]
    t5 = f(t5_table); rpb = f(rpb_b)[0]; w_out = f(w_out)[0]; w_query = f(w_query)[0]
    sk = f(sub_keys)[0]; u = f(u_experts)[0]; v = f(v_experts)[0]
    shared = {
        "w_ada": w_ada, "b_ada": b_ada.reshape(1, 6 * D), "g1c": _col(f(norm1_g)[0]), "g2c": _col(f(norm2_g)[0]),
        "onc": _col(np.concatenate([f(out_norm_a)[0], f(out_norm_b)[0]])), "w_in": w_in, "sink": f(sink_a).reshape(1, 8),
        "biasA": _bias_a(t5), "w_out": w_out, "w_query": w_query,
        "skT": np.ascontiguousarray(np.transpose(sk, (3, 0, 1, 2)).reshape(128, 2048)),
        "u_lay": np.ascontiguousarray(np.transpose(u.reshape(128, 128, 8, 128), (3, 0, 2, 1)).reshape(128, 131072)),
        "v_lay": np.ascontiguousarray(np.transpose(v.reshape(128, 128, D), (1, 0, 2)).reshape(128, 131072)),
        "final_g": f(final_g).reshape(1, D), "ident": np.eye(128, dtype=np.float32),
        "iota": np.ascontiguousarray(np.broadcast_to(np.arange(128, dtype=np.float32)[None, :], (128, 128))),
        "biasBg": np.stack([_bias_b_chunk(rpb, 20, 10 - 2 + j) for j in range(5)]).reshape(5, 128, 1024),
    }
    in_maps = []
    for core in range(NCORES):
        b, half = core // 2, core % 2
        start = half * TOK - HALO
        xe = np.zeros((NEXT * 128, D), np.float32)
        lo, hi = max(start, 0), min(start + NEXT * 128, SEQ)
        xe[lo - start:hi - start] = x[b, lo:hi]
        sp = []
        for (li, kps) in _SP_LIST:
            gp = half * 32 + li
            for kp in kps:
                sp.append(_bias_b_chunk(rpb, 2 * gp, half * 32 + kp))
        bA = shared["biasA"].copy()
        if half != 0:
            bA[1] = bA[0]
        if half != 1:
            bA[2] = bA[0]
        m = dict(shared)
        m.update({"x_ext": xe, "c_col": _col(c[b]), "biasA": bA, "biasBs": np.stack(sp).reshape(22, 128, 1024)})
        in_maps.append(m)
    if _PREP_ONLY:
        return in_maps
    nc = build_program()
    res = run_bass_kernel_spmd(nc, in_maps, core_ids=list(range(NCORES)))
    outp = np.zeros((4, SEQ, D), np.float32)
    for core in range(NCORES):
        b, half = core // 2, core % 2
        outp[b, half * TOK:(half + 1) * TOK] = res.results[core]["out"]
    return outp


if __name__ == "__main__":
    import time
    t0 = time.time()
    build_program()
    print("built", time.time() - t0, build_program.stats)
```

```python
import numpy as np
from contextlib import ExitStack
import concourse.bass as bass
import concourse.mybir as mybir
from concourse.bass_utils import run_bass_kernel_spmd

F32 = mybir.dt.float32
BF16 = mybir.dt.bfloat16
U32 = mybir.dt.uint32
AF = mybir.ActivationFunctionType
ALU = mybir.AluOpType
AX = mybir.AxisListType

SAME_ENGINE_SYNC = True
EPS = 1e-6
NEGM = -1.0e4
NCORES = 8
SEQ = 8192
TOK = 4096
HALO = 256
NEXT = 36
D = 1024


class Prog:
    EPOCH = 20000

    def __init__(self, nc, es):
        self.nc = nc
        self.es = es
        self.engs = {"pe": nc.tensor, "act": nc.scalar, "dve": nc.vector, "pool": nc.gpsimd, "sp": nc.sync}
        self.sems = {}
        self.cnt = {}
        self.waited = {}
        self.state = {}
        self.ops = []
        self.engcount = {k: 0 for k in self.engs}
        self.nwaits = 0
        self.stopped = False

    def _sem(self, k):
        if k not in self.sems:
            self.sems[k] = self.es.enter_context(self.nc.semaphore("s%d" % len(self.sems)))
        return self.sems[k]

    def _entries(self, name, key):
        d = self.state.setdefault(name, {})
        if key is None:
            if None not in d:
                d[None] = [None, {}, []]
            return list(d.values())
        if key not in d:
            if None in d:
                b = d[None]
                d[key] = [b[0], dict(b[1]), list(b[2])]
            else:
                d[key] = [None, {}, []]
        return [d[key]]

    def op(self, eng, fn, reads=(), writes=(), dma=False, semkey=None):
        if self.stopped:
            return None
        idx = len(self.ops)
        deps = set()
        psdeps = set()
        for (name, key) in reads:
            for ent in self._entries(name, key):
                if ent[0] is not None:
                    deps.add(ent[0])
        for (name, key) in writes:
            tgt = psdeps if name.startswith("ps") else deps
            for ent in self._entries(name, key):
                if ent[0] is not None:
                    tgt.add(ent[0])
                tgt.update(ent[1].values())
                tgt.update(ent[2])
        e = self.engs[eng]
        for di in sorted(deps | psdeps):
            deng, dk, dval, ddma = self.ops[di]
            if (not ddma) and deng == eng and (eng == "pe" or not SAME_ENGINE_SYNC or di not in deps):
                continue
            if self.waited.get((eng, dk), 0) >= dval:
                continue
            e.wait_ge(self._sem(dk), dval)
            self.waited[(eng, dk)] = dval
            self.nwaits += 1
        ins = fn(e)
        if dma:
            k = ("dma", semkey)
            inc = 16
        else:
            self.engcount[eng] += 1
            k = ("eng", eng, self.engcount[eng] // self.EPOCH)
            inc = 1
        self.cnt[k] = self.cnt.get(k, 0) + inc
        ins.then_inc(self._sem(k), inc)
        self.ops.append((eng, k, self.cnt[k], dma))
        for (name, key) in reads:
            for ent in self._entries(name, key):
                if dma:
                    ent[2].append(idx)
                else:
                    ent[1][eng] = idx
        for (name, key) in writes:
            d = self.state[name]
            if key is None:
                for kk in list(d.keys()):
                    d[kk] = [idx, {}, []]
            else:
                d[key] = [idx, {}, []]
        return idx

    def barrier(self):
        if self.stopped:
            return
        targets = list(self.cnt.items())
        for eng, e in self.engs.items():
            for k, v in targets:
                if self.waited.get((eng, k), 0) >= v:
                    continue
                e.wait_ge(self._sem(k), v)
                self.waited[(eng, k)] = v
                self.nwaits += 1

    def finish(self, eng="sp"):
        e = self.engs[eng]
        for k, v in self.cnt.items():
            if k[0] == "dma" and self.waited.get((eng, k), 0) < v:
                e.wait_ge(self._sem(k), v)


class _Stop(Exception):
    pass


STAGE = 99
DEBUG = False
OH1_ENG = "dve"
ACT_OH_EVERY = 1000000


def build_program():
    nc = bass.Bass("TRN2", target_bir_lowering=False)

    def stage(n):
        if STAGE <= n:
            P.stopped = True

    def din(name, shape, dt=F32):
        return nc.dram_tensor(name, shape, dt, kind="ExternalInput").ap()

    x_ext = din("x_ext", [NEXT * 128, D])
    c_col = din("c_col", [128, 8])
    w_ada = din("w_ada", [D, 6 * D])
    b_ada = din("b_ada", [1, 6 * D])
    g1c_d = din("g1c", [128, 8])
    g2c_d = din("g2c", [128, 8])
    onc_d = din("onc", [128, 8])
    w_in = din("w_in", [D, 2304])
    sink_d = din("sink", [1, 8])
    biasA_d = din("biasA", [3, 3, 128, 1024])
    biasBg_d = din("biasBg", [5, 128, 1024])
    biasBs_d = din("biasBs", [22, 128, 1024])
    w_out = din("w_out", [D, D])
    w_query = din("w_query", [D, 2048])
    skT_d = din("skT", [128, 2048])
    u_lay = din("u_lay", [128, 131072])
    v_lay = din("v_lay", [128, 131072])
    fg_d = din("final_g", [1, D])
    ident_d = din("ident", [128, 128])
    iota_d = din("iota", [128, 128])
    out = nc.dram_tensor("out", [TOK, D], F32, kind="ExternalOutput").ap()
    dbg = nc.dram_tensor("dbg", [128, 8192], F32, kind="ExternalOutput").ap() if DEBUG else None
    x1s = nc.dram_tensor("x1s", [TOK, D], F32, kind="Internal").ap()
    uTb = nc.dram_tensor("uTb", [128, 131072], BF16, kind="Internal").ap()
    vb = nc.dram_tensor("vb", [128, 131072], BF16, kind="Internal").ap()

    with ExitStack() as es:
      P = Prog(nc, es)
      try:

        def sbuf(stack, name, shape, dt):
            return stack.enter_context(nc.sbuf_tensor("sb_" + name, shape, dt))

        ps = [es.enter_context(nc.psum_tensor("ps%d" % b, [128, 512], F32)) for b in range(8)]

        def PS(b):
            return [("ps%d" % b, None)]

        ukey = [0]

        def dma(eng, out_ap, in_ap, reads, writes, semkey=None):
            if semkey is None:
                ukey[0] += 1
                semkey = "u%d" % ukey[0]
            return P.op(eng, lambda e: e.dma_start(out=out_ap, in_=in_ap), reads=reads, writes=writes, dma=True, semkey=semkey)

        def dump(ap, col0, width, res):
            if DEBUG:
                dma("pool", dbg[:, col0:col0 + width], ap, [res], [("dbg", col0)])

        def mm(o, lhsT, rhs, start, stop, reads, writes):
            return P.op("pe", lambda e: e.matmul(o, lhsT=lhsT, rhs=rhs, start=start, stop=stop), reads=reads, writes=writes)

        def tr(o, in_, ident, reads, writes):
            return P.op("pe", lambda e: e.transpose(out=o, in_=in_, identity=ident), reads=reads, writes=writes)

        def act(o, in_, func, reads, writes, bias=None, scale=None, accum=None):
            kw = {}
            if bias is not None:
                kw["bias"] = bias
            if scale is not None:
                kw["scale"] = scale
            if accum is not None:
                kw["accum_out"] = accum
            return P.op("act", lambda e: e.activation(out=o, in_=in_, func=func, **kw), reads=reads, writes=writes)

        def tt(eng, o, a, b, op, reads, writes):
            return P.op(eng, lambda e: e.tensor_tensor(out=o, in0=a, in1=b, op=op), reads=reads, writes=writes)

        def tsc(eng, o, a, s1, s2, op0, op1, reads, writes):
            if op1 is None:
                return P.op(eng, lambda e: e.tensor_scalar(out=o, in0=a, scalar1=s1, scalar2=None, op0=op0), reads=reads, writes=writes)
            return P.op(eng, lambda e: e.tensor_scalar(out=o, in0=a, scalar1=s1, scalar2=s2, op0=op0, op1=op1), reads=reads, writes=writes)

        def cp(eng, o, a, reads, writes):
            if eng == "act":
                return P.op("act", lambda e: e.copy(out=o, in_=a), reads=reads, writes=writes)
            return P.op(eng, lambda e: e.tensor_copy(out=o, in_=a), reads=reads, writes=writes)

        ident_f = sbuf(es, "ident_f", [128, 128], F32)
        ident_b = sbuf(es, "ident_b", [128, 128], BF16)
        iota_f = sbuf(es, "iota_f", [128, 128], F32)
        iota_b = sbuf(es, "iota_b", [128, 128], BF16)
        ones_f = sbuf(es, "ones_f", [128, 128], F32)
        g2c = sbuf(es, "g2c", [128, 8], F32)
        a2 = sbuf(es, "a2", [128, 8], F32)
        b2 = sbuf(es, "b2", [128, 8], F32)
        gate2_bc = sbuf(es, "gate2_bc", [128, D], F32)
        fg_bc = sbuf(es, "fg_bc", [128, D], F32)

        dma("sp", ident_f[:], ident_d, [], [("ident_f", None)])
        dma("sp", iota_f[:], iota_d, [], [("iota_f", None)])
        dma("sp", g2c[:], g2c_d, [], [("g2c", None)])
        dma("sp", fg_bc[:], fg_d[0].partition_broadcast(128), [], [("fg_bc", None)])
        cp("dve", ident_b[:], ident_f[:], [("ident_f", None)], [("ident_b", None)])
        cp("dve", iota_b[:], iota_f[:], [("iota_f", None)], [("iota_b", None)])
        P.op("dve", lambda e: e.memset(ones_f[:], 1.0), writes=[("ones_f", None)])

        def norm_transpose(stack_bufs, xt_ap, xt_res, acol, bcol, acol_res, dst_fn, dst_res, tag, bank=0):
            ssq, rstd, xn = stack_bufs
            act(xn[:], xt_ap, AF.Square, [xt_res], [("xn" + tag, None), ("ssq" + tag, None)], accum=ssq[:])
            tsc("dve", rstd[:], ssq[:], 1.0 / D, EPS, ALU.mult, ALU.add, [("ssq" + tag, None)], [("rstd" + tag, None)])
            act(rstd[:], rstd[:], AF.Sqrt, [("rstd" + tag, None)], [("rstd" + tag, None)])
            P.op("dve", lambda e: e.reciprocal(out=rstd[:], in_=rstd[:]), reads=[("rstd" + tag, None)], writes=[("rstd" + tag, None)])
            tsc("dve", xn[:], xt_ap, rstd[:, 0:1], None, ALU.mult, None, [xt_res, ("rstd" + tag, None)], [("xn" + tag, None)])
            psb = ps[bank][:, :].bitcast(BF16)
            for kc in range(8):
                tr(psb[:, kc * 128:(kc + 1) * 128], xn[:, kc * 128:(kc + 1) * 128], ident_b[:],
                   [("xn" + tag, None), ("ident_b", None)], PS(bank))
            for kc in range(8):
                eng = "dve" if kc % 2 == 0 else "act"
                if eng == "dve":
                    tsc("dve", dst_fn(kc), psb[:, kc * 128:(kc + 1) * 128], acol[:, kc:kc + 1], bcol[:, kc:kc + 1],
                        ALU.mult, ALU.add, [acol_res], PS(bank) + [dst_res])
                else:
                    act(dst_fn(kc), psb[:, kc * 128:(kc + 1) * 128], AF.Identity, [acol_res], PS(bank) + [dst_res],
                        bias=bcol[:, kc:kc + 1], scale=acol[:, kc:kc + 1])

        with ExitStack() as ea:
            c_sb = sbuf(ea, "c_sb", [128, 8], F32)
            c_act = sbuf(ea, "c_act", [128, 8], F32)
            g1c = sbuf(ea, "g1c", [128, 8], F32)
            onc = sbuf(ea, "onc", [128, 8], F32)
            a1 = sbuf(ea, "a1", [128, 8], F32)
            b1 = sbuf(ea, "b1", [128, 8], F32)
            adacol = sbuf(ea, "adacol", [128, 32], F32)
            sink_bc = sbuf(ea, "sink_bc", [128, 8], F32)
            expsink = sbuf(ea, "expsink", [128, 8], F32)
            zeros8 = sbuf(ea, "zeros8", [128, 8], F32)
            wo_b = sbuf(ea, "wo_b", [128, 8, D], BF16)

            dma("sp", c_sb[:], c_col, [], [("c_sb", None)])
            dma("sp", g1c[:], g1c_d, [], [("g1c", None)])
            dma("sp", onc[:], onc_d, [], [("onc", None)])
            dma("sp", sink_bc[:], sink_d[0].partition_broadcast(128), [], [("sink_bc", None)])
            P.op("dve", lambda e: e.memset(zeros8[:], 0.0), writes=[("zeros8", None)])
            act(expsink[:], sink_bc[:], AF.Exp, [("sink_bc", None)], [("expsink", None)])
            act(c_act[:], c_sb[:], AF.Silu, [("c_sb", None)], [("c_act", None)])

            with ExitStack() as e0:
                wa = [sbuf(e0, "wa%d" % i, [128, 8, 512], F32) for i in range(2)]
                ada_row = sbuf(e0, "ada_row", [1, 6 * D], F32)
                bada = sbuf(e0, "bada", [1, 6 * D], F32)
                wo_f = sbuf(e0, "wo_f", [128, 8, D], F32)
                gate1_bc = sbuf(e0, "gate1_bc", [128, D], F32)
                dma("sp", bada[:], b_ada, [], [("bada", None)])
                dma("sp", wo_f[:], w_out.rearrange("(kc p) n -> p kc n", p=128), [], [("wo_f", None)])
                w_ada_v = w_ada.rearrange("(kc p) n -> p kc n", p=128)
                for blk in range(12):
                    sl = blk % 2
                    dma("sp", wa[sl][:], w_ada_v[:, :, blk * 512:(blk + 1) * 512], [], [("wa%d" % sl, None)], "wa%d" % sl)
                    for kc in range(8):
                        mm(ps[7][0:1, 0:512], c_act[:, kc:kc + 1], wa[sl][:, kc, :], kc == 0, kc == 7,
                           [("c_act", None), ("wa%d" % sl, None)], PS(7))
                    tt("dve", ada_row[0:1, blk * 512:(blk + 1) * 512], ps[7][0:1, 0:512], bada[0:1, blk * 512:(blk + 1) * 512],
                       ALU.add, [("bada", None)], PS(7) + [("ada_row", blk)])
                for vi, v in enumerate([0, 1, 3, 4]):
                    for kc in range(8):
                        mm(ps[7][:, vi * 8 + kc:vi * 8 + kc + 1], ada_row[0:1, v * D + kc * 128:v * D + (kc + 1) * 128],
                           ident_f[0:1, 0:1], True, True, [("ada_row", None), ("ident_f", None)], PS(7))
                cp("dve", adacol[:], ps[7][:, 0:32], [], PS(7) + [("adacol", None)])
                tsc("dve", a1[:], adacol[:, 8:16], 1.0, None, ALU.add, None, [("adacol", None)], [("a1", None)])
                tt("dve", a1[:], a1[:], g1c[:], ALU.mult, [("g1c", None)], [("a1", None)])
                cp("dve", b1[:], adacol[:, 0:8], [("adacol", None)], [("b1", None)])
                tsc("dve", a2[:], adacol[:, 24:32], 1.0, None, ALU.add, None, [("adacol", None)], [("a2", None)])
                tt("dve", a2[:], a2[:], g2c[:], ALU.mult, [("g2c", None)], [("a2", None)])
                cp("dve", b2[:], adacol[:, 16:24], [("adacol", None)], [("b2", None)])
                for gi, (v, dst, dname) in enumerate([(2, gate1_bc, "gate1_bc"), (5, gate2_bc, "gate2_bc")]):
                    for hf in range(2):
                        mm(ps[1 + hf][:, 0:512], ones_f[0:1, 0:128], ada_row[0:1, v * D + hf * 512:v * D + (hf + 1) * 512],
                           True, True, [("ones_f", None), ("ada_row", None)], PS(1 + hf))
                        cp("dve", dst[:, hf * 512:(hf + 1) * 512], ps[1 + hf][:, 0:512], [], PS(1 + hf) + [(dname, None)])
                for kc in range(8):
                    P.op("dve", lambda e, kc=kc: e.scalar_tensor_tensor(out=wo_b[:, kc, :], in0=wo_f[:, kc, :], scalar=onc[:, kc:kc + 1],
                                                                          in1=gate1_bc[:], op0=ALU.mult, op1=ALU.mult),
                         reads=[("wo_f", None), ("onc", None), ("gate1_bc", None)], writes=[("wo_b", None)])

            P.barrier()
            dump(a1[:], 0, 8, ("a1", None)); dump(b1[:], 8, 8, ("b1", None)); dump(a2[:], 16, 8, ("a2", None)); dump(b2[:], 24, 8, ("b2", None))
            dump(gate2_bc[:], 32, 1024, ("gate2_bc", None)); dump(wo_b[:, 0, :], 1056, 1024, ("wo_b", None))
            stage(1)
            w_in_b = sbuf(ea, "w_in_b", [128, 8, 2304], BF16)
            wkdup = sbuf(ea, "wkdup", [128, 8, 2, 128], BF16)
            biasA = sbuf(ea, "biasA", [128, 3, 1024], BF16)
            biasBg = sbuf(ea, "biasBg", [128, 5, 1024], BF16)
            biasBs = sbuf(ea, "biasBs", [128, 6, 1024], BF16)
            w_in_v = w_in.rearrange("(kc p) n -> p kc n", p=128)
            for hf in range(2):
                dma("pool", w_in_b[:, :, hf * 1152:(hf + 1) * 1152], w_in_v[:, :, hf * 1152:(hf + 1) * 1152],
                    [], [("w_in_b", None)])
            dma("pool", biasA[:], biasA_d[0].rearrange("c k n -> k c n"), [], [("biasA", None)])
            dma("pool", biasBg[:], biasBg_d.rearrange("c k n -> k c n"), [], [("biasBg", None)])
            for g in range(2):
                for dup in range(2):
                    cp("pool", wkdup[:, :, g, dup * 64:(dup + 1) * 64], w_in_b[:, :, 512 + g * 64:512 + (g + 1) * 64],
                       [("w_in_b", None)], [("wkdup", None)])
            hT = sbuf(ea, "hT", [128, 8, 1024], BF16)
            KA = sbuf(ea, "KA", [128, 2, 1024], BF16)
            KB = sbuf(ea, "KB", [128, 4, 1024], BF16)
            QA = sbuf(ea, "QA", [128, 4, 512], BF16)
            QB = sbuf(ea, "QB", [128, 4, 512], BF16)
            VA = sbuf(ea, "VA", [128, 8, 2, 65], BF16)
            VB = sbuf(ea, "VB", [128, 8, 8, 65], BF16)
            NXT = 3
            xts = [sbuf(ea, "xt%d" % i, [128, D], F32) for i in range(NXT)]
            xrs = [sbuf(ea, "xr%d" % i, [128, D], F32) for i in range(2)]
            nbs = [(sbuf(ea, "ssqA%d" % i, [128, 1], F32), sbuf(ea, "rstdA%d" % i, [128, 1], F32), sbuf(ea, "xnA%d" % i, [128, D], BF16))
                   for i in range(2)]
            Pb = [sbuf(ea, "Pb0", [128, 3, 1024], BF16), sbuf(ea, "Pb1", [128, 6, 1024], BF16),
                  sbuf(ea, "Pb2", [128, 3, 1024], BF16), sbuf(ea, "Pb3", [128, 6, 1024], BF16)]
            den = sbuf(ea, "den", [128, 8], F32)
            rden = sbuf(ea, "rden", [128, 8], F32)
            o_t = sbuf(ea, "o_t", [128, 512], BF16)
            oss = sbuf(ea, "oss", [128, 1], F32)
            orr = sbuf(ea, "orr", [128, 1], F32)
            on_a = sbuf(ea, "on_a", [128, 512], BF16)
            on_b = sbuf(ea, "on_b", [128, 512], BF16)
            mixT = sbuf(ea, "mixT", [128, 8, 128], BF16)
            P.op("dve", lambda e: e.memset(VA[:], 1.0), writes=[("VA", None)])
            P.op("dve", lambda e: e.memset(VB[:], 1.0), writes=[("VB", None)])

            sstate = {"sb": 0, "pb": 0, "x": 0}

            def attn_tile(pb, nch, kT, q, bias, V, sinkt, dst, dst_name, rd, pvb):
                Pt = Pb[pb]
                pname = "Pb%d" % pb
                for c in range(nch):
                    for grp in range(2):
                        bk = 3 + sstate["sb"]
                        sstate["sb"] ^= 1
                        mm(ps[bk][:, 0:512], ident_b[:], bias(c)[:, grp * 512:(grp + 1) * 512], True, False,
                           [("ident_b", None)] + rd, PS(bk))
                        for r in range(4):
                            h = grp + 2 * r
                            mm(ps[bk][:, r * 128:(r + 1) * 128], kT(h, c), q(h), False, r == 3, rd, PS(bk))
                        act(Pt[:, c, grp * 512:(grp + 1) * 512], ps[bk][:, 0:512], AF.Exp, [], PS(bk) + [(pname, None)])

                def pv_phase():
                    for h in range(8):
                        bk = pvb[h // 4]
                        for c in range(nch):
                            pos = (h % 2) * 4 + h // 2
                            mm(ps[bk][:, (h % 4) * 65:(h % 4) * 65 + 65], Pt[:, c, pos * 128:(pos + 1) * 128], V(h, c), c == 0, c == nch - 1,
                               [(pname, None)] + rd, PS(bk))

                def norm_phase():
                    for bnk in range(2):
                        pv = ps[pvb[bnk]][:, 0:260].rearrange("p (h e) -> p h e", e=65)
                        tt("dve", den[:, bnk * 4:(bnk + 1) * 4], pv[:, :, 64], sinkt[:, bnk * 4:(bnk + 1) * 4], ALU.add,
                           [("expsink", None), ("zeros8", None)], PS(pvb[bnk]) + [("den", None)])
                    P.op("dve", lambda e: e.reciprocal(out=rden[:], in_=den[:]), reads=[("den", None)], writes=[("rden", None)])
                    for bnk in range(2):
                        pv = ps[pvb[bnk]][:, 0:260].rearrange("p (h e) -> p h e", e=65)
                        ov = o_t[:, bnk * 256:(bnk + 1) * 256].rearrange("p (h e) -> p h e", e=64)
                        rb = rden[:, bnk * 4:(bnk + 1) * 4].unsqueeze(2).broadcast_to([128, 4, 64])
                        tt("dve", ov, pv[:, :, 0:64], rb, ALU.mult, [("rden", None)], PS(pvb[bnk]) + [("o_t", None)])
                    act(dst[:], o_t[:], AF.Square, [("o_t", None)], [(dst_name, None), ("oss", None)], accum=oss[:])
                    tsc("dve", orr[:], oss[:], 1.0 / 512, EPS, ALU.mult, ALU.add, [("oss", None)], [("orr", None)])
                    act(orr[:], orr[:], AF.Sqrt, [("orr", None)], [("orr", None)])
                    P.op("dve", lambda e: e.reciprocal(out=orr[:], in_=orr[:]), reads=[("orr", None)], writes=[("orr", None)])
                    tsc("dve", dst[:], o_t[:], orr[:, 0:1], None, ALU.mult, None, [("o_t", None), ("orr", None)], [(dst_name, None)])

                return pv_phase, norm_phase

            sp_off = {0: 0, 1: 6, 30: 11, 31: 16}
            for s in range(8):
                if s == 1:
                    stage(6)
                def x_load(te):
                    xs = te % NXT
                    dma("sp", xts[xs][:], x_ext[te * 128:(te + 1) * 128, :], [], [("xt%d" % xs, None)], "xt%d" % xs)

                def nt_stats(te):
                    if te in nstate["stats"]:
                        return
                    nstate["stats"].add(te)
                    if te + NXT - 1 < NEXT:
                        x_load(te + NXT - 1)
                    xs = te % NXT
                    xt = xts[xs]
                    ssq, rstd, xn = nbs[te % 2]
                    tg = "A%d" % (te % 2)
                    xres = ("xt%d" % xs, None)
                    act(xn[:], xt[:], AF.Square, [xres], [("xn" + tg, None), ("ssq" + tg, None)], accum=ssq[:])
                    tsc("dve", rstd[:], ssq[:], 1.0 / D, EPS, ALU.mult, ALU.add, [("ssq" + tg, None)], [("rstd" + tg, None)])
                    act(rstd[:], rstd[:], AF.Sqrt, [("rstd" + tg, None)], [("rstd" + tg, None)])
                    P.op("dve", lambda e: e.reciprocal(out=rstd[:], in_=rstd[:]), reads=[("rstd" + tg, None)], writes=[("rstd" + tg, None)])
                    tsc("dve", xn[:], xt[:], rstd[:, 0:1], None, ALU.mult, None, [xres, ("rstd" + tg, None)], [("xn" + tg, None)])

                def nt_rest(te):
                    tl = te % 8
                    xn = nbs[te % 2][2]
                    tg = "A%d" % (te % 2)
                    psb0 = ps[0][:, :].bitcast(BF16)
                    psb7 = ps[7][:, :].bitcast(BF16)
                    for kc in range(8):
                        pb_, bk_ = (psb0, 0) if kc < 4 else (psb7, 7)
                        tr(pb_[:, (kc % 4) * 128:(kc % 4 + 1) * 128], xn[:, kc * 128:(kc + 1) * 128], ident_b[:],
                           [("xn" + tg, None), ("ident_b", None)], PS(bk_))
                    for kk in range(4):
                        for kc in (kk, kk + 4):
                            dstk = hT[:, kc, tl * 128:(tl + 1) * 128]
                            if kc < 4:
                                tsc("dve", dstk, psb0[:, kc * 128:(kc + 1) * 128], a1[:, kc:kc + 1], b1[:, kc:kc + 1],
                                    ALU.mult, ALU.add, [("a1", None)], PS(0) + [("hT", (tl, kc))])
                            else:
                                act(dstk, psb7[:, (kc - 4) * 128:(kc - 3) * 128], AF.Identity, [("a1", None)], PS(7) + [("hT", (tl, kc))],
                                    bias=b1[:, kc:kc + 1], scale=a1[:, kc:kc + 1])
                    for kc in range(8):
                        mm(ps[1][:, 0:512], hT[:, kc, tl * 128:(tl + 1) * 128], w_in_b[:, kc, 1792:2304], kc == 0, kc == 7,
                           [("hT", (tl, kc)), ("w_in_b", None)], PS(1))
                        mm(ps[2][:, 0:128], hT[:, kc, tl * 128:(tl + 1) * 128], w_in_b[:, kc, 640:768], kc == 0, kc == 7,
                           [("hT", (tl, kc)), ("w_in_b", None)], PS(2))
                    cp("act", VB[:, tl, :, 0:64], ps[1][:, 0:512].rearrange("p (h e) -> p h e", e=64), [], PS(1) + [("VB", tl)])
                    cp("dve", VA[:, tl, :, 0:64], ps[2][:, 0:128].rearrange("p (h e) -> p h e", e=64), [], PS(2) + [("VA", tl)])

                if s == 0:
                    nstate = {"stats": set()}
                    for te0 in range(NXT - 1):
                        x_load(te0)
                newt = list(range(0, 8)) if s == 0 else list(range(4 * s + 4, 4 * s + 8))
                for te in newt:
                    nt_stats(te)
                    if te + 1 < NEXT:
                        nt_stats(te + 1)
                    nt_rest(te)
                stage(2)
                pj = 0
                halves = [0, 1] if s == 0 else [((4 * s + 4) % 8) // 4]
                for hf in halves:
                    for ch in range(6):
                        bk = 1 + pj % 2
                        pj += 1
                        for kc in range(8):
                            lw = wkdup[:, kc, ch, :] if ch < 2 else w_in_b[:, kc, 1280 + (ch - 2) * 128:1280 + (ch - 1) * 128]
                            mm(ps[bk][:, 0:512], lw, hT[:, kc, hf * 512:(hf + 1) * 512], kc == 0, kc == 7,
                               [("hT", None), ("w_in_b", None), ("wkdup", None)], PS(bk))
                        if ch < 2:
                            cp("act" if pj % 2 else "dve", KA[:, ch, hf * 512:(hf + 1) * 512], ps[bk][:, 0:512], [], PS(bk) + [("KA", None)])
                        else:
                            cp("act" if pj % 2 else "dve", KB[:, ch - 2, hf * 512:(hf + 1) * 512], ps[bk][:, 0:512], [], PS(bk) + [("KB", None)])
                q0 = (4 * s + 2) % 8
                qparts = [(q0 * 128, 512, 0)] if q0 == 2 else [(768, 256, 0), (0, 256, 256)]
                for ch in range(8):
                    bk = 1 + pj % 2
                    pj += 1
                    col0 = ch * 128 if ch < 4 else 768 + (ch - 4) * 128
                    first = True
                    for kc in range(8):
                        for (hc, hn_, pc_) in qparts:
                            mm(ps[bk][:, pc_:pc_ + hn_], w_in_b[:, kc, col0:col0 + 128], hT[:, kc, hc:hc + hn_], first,
                               kc == 7 and (hc, hn_, pc_) == qparts[-1], [("hT", None), ("w_in_b", None)], PS(bk))
                            first = False
                    dq = QA[:, ch, :] if ch < 4 else QB[:, ch - 4, :]
                    dn = "QA" if ch < 4 else "QB"
                    act(dq, ps[bk][:, 0:512], AF.Identity, [], PS(bk) + [(dn, None)], scale=0.125)
                stage(3)
                def s_phase(j):
                    i = 4 * s + j
                    tl = j + 2
                    sl_ = lambda p_: (4 * s + p_) % 8
                    pbo = 2 * (j % 2)
                    if 2 <= i < 18:
                        pc = i - 2
                        for (src_d, dst_d, nm) in ((u_lay, uTb, "uTb"), (v_lay, vb, "vb")):
                            dma("pool", dst_d[:, pc * 8192:(pc + 1) * 8192].rearrange("p (a b) -> p a b", b=2048),
                                src_d[:, pc * 8192:(pc + 1) * 8192].rearrange("p (a b) -> p a b", b=2048), [], [(nm, pc)], "cv" + nm)
                    rdA = [("KA", None), ("QA", None), ("VA", None), ("biasA", None), ("biasBs", None)]
                    if i in (0, 31):
                        dma("pool", biasBs[:, 0:3, :], biasA_d[1 if i == 0 else 2].rearrange("c k n -> k c n"),
                            [], [("biasBs", None)], "bs")
                        bfa = lambda c: biasBs[:, c, :]
                    else:
                        bfa = lambda c: biasA[:, c, :]
                    wpv, wnorm = attn_tile(pbo, 3,
                              lambda h, c, tl=tl: KA[(h % 2) * 64:(h % 2) * 64 + 64, h // 4, sl_(tl - 1 + c) * 128:sl_(tl - 1 + c) * 128 + 128],
                              lambda h, j=j: QA[(h % 2) * 64:(h % 2) * 64 + 64, h // 2, j * 128:(j + 1) * 128],
                              bfa,
                              lambda h, c, tl=tl: VA[:, sl_(tl - 1 + c), h // 4, :],
                              expsink, on_a, "on_a", rdA, (5, 6))
                    stage(4)
                    rdB = [("KB", None), ("QB", None), ("VB", None), ("biasBg", None), ("biasBs", None)]
                    if i in sp_off:
                        nchb = 6 if i in (0, 31) else 5
                        dma("pool", biasBs[:, 0:nchb, :], biasBs_d[sp_off[i]:sp_off[i] + nchb].rearrange("c k n -> k c n"),
                            [], [("biasBs", None)], "bs")
                        c0 = tl - 3 if i == 31 else tl - 2
                        bfn = lambda c: biasBs[:, c, :]
                    else:
                        nchb = 5
                        c0 = tl - 2
                        bfn = lambda c: biasBg[:, c, :]
                    bpv, bnorm = attn_tile(pbo + 1, nchb,
                              lambda h, c, c0=c0: KB[(h % 2) * 64:(h % 2) * 64 + 64, h // 2, sl_(c0 + c) * 128:sl_(c0 + c) * 128 + 128],
                              lambda h, j=j: QB[(h % 2) * 64:(h % 2) * 64 + 64, h // 2, j * 128:(j + 1) * 128],
                              bfn,
                              lambda h, c, c0=c0: VB[:, sl_(c0 + c), h, :],
                              zeros8, on_b, "on_b", rdB, (1, 2))

                    def pv():
                        wpv()
                        bpv()

                    def norms():
                        wnorm()
                        bnorm()
                        if i == 0:
                            dump(on_a[:], 2080, 512, ("on_a", None)); dump(on_b[:], 2592, 512, ("on_b", None))
                            dump(hT[:, 0, 256:384], 4128, 128, ("hT", None))

                    def tail():
                        stage(5)
                        psb = ps[0][:, :].bitcast(BF16)
                        for kc in range(8):
                            src_ = on_a if kc < 4 else on_b
                            tr(psb[:, kc * 128:(kc + 1) * 128], src_[:, (kc % 4) * 128:(kc % 4 + 1) * 128], ident_b[:],
                               [("on_a", None), ("on_b", None), ("ident_b", None)], PS(0))
                        cp("act", mixT[:].rearrange("p k t -> p (k t)"), psb[:, 0:1024], [], PS(0) + [("mixT", None)])
                        xr = xrs[i % 2]
                        dma("sp", xr[:], x_ext[(i + 2) * 128:(i + 3) * 128, :], [], [("xr%d" % (i % 2), None)], "xr%d" % (i % 2))
                        for hf in range(2):
                            for kc in range(8):
                                mm(ps[1 + hf][:, 0:512], mixT[:, kc, :], wo_b[:, kc, hf * 512:(hf + 1) * 512], kc == 0, kc == 7,
                                   [("mixT", None), ("wo_b", None)], PS(1 + hf))
                            tt("dve", xr[:, hf * 512:(hf + 1) * 512], ps[1 + hf][:, 0:512], xr[:, hf * 512:(hf + 1) * 512], ALU.add,
                               [("xr%d" % (i % 2), None)], PS(1 + hf) + [("xr%d" % (i % 2), None)])
                        if i == 0:
                            dump(xr[:], 3104, 1024, ("xr0", None))
                        dma("sp", x1s[i * 128:(i + 1) * 128, :], xr[:], [("xr%d" % (i % 2), None)], [("x1s", i)], "x1o%d" % (i % 2))

                    return pv, norms, tail

                prev = s_phase(0)
                prev[0]()
                for j in range(1, 4):
                    cur = s_phase(j)
                    prev[1]()
                    prev[2]()
                    cur[0]()
                    prev = cur
                prev[1]()
                prev[2]()

        P.barrier()
        stage(7)
        with ExitStack() as eb:
            wq = sbuf(eb, "wq", [128, 8, 2048], BF16)
            skT = sbuf(eb, "skT", [128, 2048], BF16)
            dma("pool", wq[:], w_query.rearrange("(kc p) n -> p kc n", p=128), [], [("wq", None)])
            dma("pool", skT[:], skT_d, [], [("skT", None)])
            stage(8)
            Gs = sbuf(eb, "Gs", [128, 256, 128], BF16)
            h2Ts = [sbuf(eb, "h2T%d" % i, [128, 8, 256], BF16) for i in range(2)]
            qhT = sbuf(eb, "qhT", [128, 16, 256], BF16)
            S = sbuf(eb, "S", [128, 16, 128], F32)
            V16 = sbuf(eb, "V16", [128, 16, 16], F32)
            I16 = sbuf(eb, "I16", [128, 16, 16], U32)
            I16f = sbuf(eb, "I16f", [128, 16, 16], F32)
            cand = sbuf(eb, "cand", [128, 8, 256], F32)
            eq = cand[:].rearrange("p h (a b) -> p h a b", b=16)
            VS = sbuf(eb, "VS", [128, 8, 16], F32)
            CI = sbuf(eb, "CI", [128, 8, 16], U32)
            CIf = sbuf(eb, "CIf", [128, 8, 16], F32)
            CAf = sbuf(eb, "CAf", [128, 8, 16], F32)
            CBf = sbuf(eb, "CBf", [128, 8, 16], F32)
            E1 = sbuf(eb, "E1", [128, 128], F32)
            E2 = sbuf(eb, "E2", [128, 128], F32)
            Gt = sbuf(eb, "Gt", [128, 128], F32)
            gsum = sbuf(eb, "gsum", [128, 8], F32)
            E1T = sbuf(eb, "E1T", [128, 256], F32)
            E2T = sbuf(eb, "E2T", [128, 256], F32)
            GT = sbuf(eb, "GT", [128, 256], F32)
            nE1T = sbuf(eb, "nE1T", [128, 256], F32)
            GTs = sbuf(eb, "GTs", [128, 256], F32)
            NOH = 6
            OH1 = [sbuf(eb, "OH1_%d" % i, [128, 128], BF16) for i in range(NOH)]
            OH2 = [sbuf(eb, "OH2_%d" % i, [128, 128], BF16) for i in range(NOH)]
            Uc = [sbuf(eb, "Uc%d" % i, [128, 2, 8, 128], BF16) for i in range(2)]
            Vc = [sbuf(eb, "Vc%d" % i, [128, 2, D], BF16) for i in range(4)]
            At = [sbuf(eb, "At%d" % i, [128, 256], BF16) for i in range(4)]
            x1g = [[sbuf(eb, "x1g%d_%d" % (p_, i), [128, D], F32) for i in range(2)] for p_ in range(2)]
            yt = sbuf(eb, "yt", [128, D], F32)
            nb2 = (sbuf(eb, "ssqB", [128, 1], F32), sbuf(eb, "rstdB", [128, 1], F32), sbuf(eb, "xnB", [128, D], BF16))
            fjunk = sbuf(eb, "fjunk", [128, D], BF16)
            fss = sbuf(eb, "fss", [128, 1], F32)
            frs = sbuf(eb, "frs", [128, 1], F32)
            PB = 7

            def top16_batch(items):
                for (s_, sr, vd, idd, vr, ir) in items:
                    P.op("dve", lambda e, s_=s_, vd=vd: e.max(out=vd[:, 0:8], in_=s_), reads=[sr], writes=[vr])
                yield
                for (s_, sr, vd, idd, vr, ir) in items:
                    P.op("dve", lambda e, s_=s_, vd=vd, idd=idd: e.max_index(out=idd[:, 0:8], in_max=vd[:, 0:8], in_values=s_),
                         reads=[sr, vr], writes=[ir])
                yield
                for (s_, sr, vd, idd, vr, ir) in items:
                    P.op("dve", lambda e, s_=s_, vd=vd: e.match_replace(out=s_, in_to_replace=vd[:, 0:8], in_values=s_, imm_value=-1e30),
                         reads=[vr], writes=[sr])
                yield
                for (s_, sr, vd, idd, vr, ir) in items:
                    P.op("dve", lambda e, s_=s_, vd=vd: e.max(out=vd[:, 8:16], in_=s_), reads=[sr], writes=[vr])
                yield
                for (s_, sr, vd, idd, vr, ir) in items:
                    P.op("dve", lambda e, s_=s_, vd=vd, idd=idd: e.max_index(out=idd[:, 8:16], in_max=vd[:, 8:16], in_values=s_),
                         reads=[sr, vr], writes=[ir])
                yield

            def prep(gi):
                par = gi % 2
                h2T = h2Ts[par]
                hn = "h2T%d" % par
                for tk in range(2):
                    ti = 2 * gi + tk
                    xn_ = "x1g%d_%d" % (par, tk)
                    dma("sp", x1g[par][tk][:], x1s[ti * 128:(ti + 1) * 128, :], [("x1s", ti)], [(xn_, None)], xn_)
                    norm_transpose(nb2, x1g[par][tk][:], (xn_, None), a2, b2, ("a2", None),
                                   lambda kc, tk=tk: h2T[:, kc, tk * 128:(tk + 1) * 128], (hn, None), "B", bank=PB)
                    yield
                stage(9)
                for c in range(16):
                    for kc in range(8):
                        mm(ps[PB][:, 0:256], wq[:, kc, c * 128:(c + 1) * 128], h2T[:, kc, :], kc == 0, kc == 7,
                           [("wq", None), (hn, None)], PS(PB))
                    cp("act", qhT[:, c, :], ps[PB][:, 0:256], [], PS(PB) + [("qhT", None)])
                    yield
                for tk in range(2):
                    for cg in range(4):
                        for cc in range(4):
                            c = cg * 4 + cc
                            mm(ps[PB][:, cc * 128:(cc + 1) * 128], qhT[:, c, tk * 128:(tk + 1) * 128], skT[:, c * 128:(c + 1) * 128],
                               True, True, [("qhT", None), ("skT", None)], PS(PB))
                        cp("act", S[:, cg * 4:(cg + 1) * 4, :].rearrange("p a b -> p (a b)"), ps[PB][:, 0:512],
                           [], PS(PB) + [("S", cg * 4 + q_) for q_ in range(4)])
                        yield
                    if gi == 0 and tk == 0:
                        dump(S[:, 0:2, :].rearrange("p a b -> p (a b)"), 4256, 256, ("S", None))
                        dump(h2T[:, 0, :], 5664, 256, (hn, None))
                    stage(10)
                    for half_ in range(2):
                        yield from top16_batch([(S[:, c, :], ("S", c), V16[:, c, :], I16[:, c, :], ("V16", c), ("I16", c))
                                                for c in range(half_ * 8, half_ * 8 + 8)])
                    V4 = V16[:].rearrange("p (h two) k -> p h two k", two=2)
                    c4 = cand[:].rearrange("p h (a b) -> p h a b", b=16)
                    for hq in range(4):
                        hs = slice(2 * hq, 2 * hq + 2)
                        tt("dve", c4[:, hs], V4[:, hs, 0, :].unsqueeze(3).broadcast_to([128, 2, 16, 16]),
                           V4[:, hs, 1, :].unsqueeze(2).broadcast_to([128, 2, 16, 16]), ALU.add,
                           [("V16", None)], [("cand", 2 * hq), ("cand", 2 * hq + 1)])
                        yield
                    yield from top16_batch([(cand[:, h, :], ("cand", h), VS[:, h, :], CI[:, h, :], ("VS", h), ("CI", h)) for h in range(8)])
                    stage(11)
                    MAGIC = 12582912.0
                    cp("dve", CIf[:], CI[:], [("CI", None)], [("CIf", None)])
                    tsc("dve", CAf[:], CIf[:], 0.0625, -0.46875, ALU.mult, ALU.add, [("CIf", None)], [("CAf", None)])
                    tsc("dve", CAf[:], CAf[:], MAGIC, None, ALU.add, None, [("CAf", None)], [("CAf", None)])
                    tsc("dve", CAf[:], CAf[:], -MAGIC, None, ALU.add, None, [("CAf", None)], [("CAf", None)])
                    P.op("dve", lambda e: e.scalar_tensor_tensor(out=CBf[:], in0=CAf[:], scalar=-16.0, in1=CIf[:], op0=ALU.mult, op1=ALU.add),
                         reads=[("CAf", None), ("CIf", None)], writes=[("CBf", None)])
                    cp("dve", I16f[:], I16[:], [("I16", None)], [("I16f", None)])
                    yield
                    I4 = I16f[:].rearrange("p (h two) k -> p h two k", two=2)
                    io16 = iota_f[:, 0:16].unsqueeze(1).unsqueeze(1).broadcast_to([128, 2, 16, 16])
                    for (cf, cfn, two, Ed, Edn) in ((CAf, "CAf", 0, E1, "E1"), (CBf, "CBf", 1, E2, "E2")):
                        for hq in range(4):
                            hs = slice(2 * hq, 2 * hq + 2)
                            cres = [("cand", 2 * hq), ("cand", 2 * hq + 1)]
                            tt("dve", eq[:, hs], io16, cf[:, hs].unsqueeze(3).broadcast_to([128, 2, 16, 16]), ALU.is_equal,
                               [("iota_f", None), (cfn, None)], cres)
                            yield
                            tt("dve", eq[:, hs], eq[:, hs], I4[:, hs, two, :].unsqueeze(2).broadcast_to([128, 2, 16, 16]), ALU.mult,
                               [("I16f", None)], cres)
                            yield
                            P.op("dve", lambda e, Ed=Ed, hs=hs: e.tensor_reduce(out=Ed[:].rearrange("p (h k) -> p h k", k=16)[:, hs],
                                                                                in_=eq[:, hs], axis=AX.X, op=ALU.add),
                                 reads=cres, writes=[(Edn, hq)])
                            yield
                    G3 = Gt[:].rearrange("p (h k) -> p h k", k=16)
                    tt("dve", G3, VS[:], VS[:, :, 0:1].broadcast_to([128, 8, 16]), ALU.subtract, [("VS", None)], [("Gt", None)])
                    act(Gt[:], Gt[:], AF.Exp, [("Gt", None)], [("Gt", None)])
                    P.op("dve", lambda e: e.tensor_reduce(out=gsum[:], in_=G3, axis=AX.X, op=ALU.add),
                         reads=[("Gt", None)], writes=[("gsum", None)])
                    P.op("dve", lambda e: e.reciprocal(out=gsum[:], in_=gsum[:]), reads=[("gsum", None)], writes=[("gsum", None)])
                    tt("dve", G3, G3, gsum[:].unsqueeze(2).broadcast_to([128, 8, 16]), ALU.mult, [("gsum", None)], [("Gt", None)])
                    yield
                    if gi == 0 and tk == 0:
                        dump(V16[:].rearrange("p a b -> p (a b)"), 4512, 256, ("V16", None))
                        dump(I16f[:].rearrange("p a b -> p (a b)"), 4768, 256, ("I16f", None))
                        dump(VS[:].rearrange("p a b -> p (a b)"), 5024, 128, ("VS", None))
                        dump(CIf[:].rearrange("p a b -> p (a b)"), 5152, 128, ("CIf", None))
                        dump(E1[:], 5280, 128, ("E1", None)); dump(E2[:], 5408, 128, ("E2", None)); dump(Gt[:], 5536, 128, ("Gt", None))
                    for qi, (src_, sn, dstT, dn, sc_) in enumerate(((E1, "E1", E1T, "E1T", 1.0), (E2, "E2", E2T, "E2T", 1.0),
                                                                    (Gt, "Gt", GT, "GT", 1.0))):
                        tr(ps[PB][:, 0:128], src_[:], ident_f[:], [(sn, None), ("ident_f", None)], PS(PB))
                        act(dstT[:, tk * 128:(tk + 1) * 128], ps[PB][:, 0:128], AF.Identity, [], PS(PB) + [(dn, None)], scale=sc_)
                        if dn == "E1T":
                            act(nE1T[:, tk * 128:(tk + 1) * 128], ps[PB][:, 0:128], AF.Identity, [], PS(PB) + [("nE1T", None)], scale=-4.0)
                        if dn == "GT":
                            act(GTs[:, tk * 128:(tk + 1) * 128], ps[PB][:, 0:128], AF.Identity, [], PS(PB) + [("GTs", None)], scale=1.0 / 1.125)
                        yield

            def gbuild(gi):
                for t in range(256):
                    sl = t % NOH
                    on_act = (t % ACT_OH_EVERY == ACT_OH_EVERY - 1)
                    gsrc, gname = (GTs, "GTs") if on_act else (GT, "GT")
                    tsc("dve", OH2[sl][:], iota_b[:], E2T[:, t:t + 1], gsrc[:, t:t + 1], ALU.is_equal, ALU.mult,
                        [("iota_b", None), ("E2T", None), (gname, None)], [("OH2_%d" % sl, None)])
                    if on_act:
                        act(OH1[sl][:], iota_b[:], AF.Derivative_Erf, [("iota_b", None), ("nE1T", None)], [("OH1_%d" % sl, None)],
                            bias=nE1T[:, t:t + 1], scale=4.0)
                    else:
                        tsc("dve", OH1[sl][:], iota_b[:], E1T[:, t:t + 1], None, ALU.is_equal, None,
                            [("iota_b", None), ("E1T", None)], [("OH1_%d" % sl, None)])
                    bk = 4 + (t // 4) % 4
                    mm(ps[bk][:, (t % 4) * 128:(t % 4 + 1) * 128], OH2[sl][:], OH1[sl][:], True, True,
                       [("OH2_%d" % sl, None), ("OH1_%d" % sl, None)], PS(bk))
                    if t % 4 == 3:
                        cp("act", Gs[:, t - 3:t + 1, :].rearrange("p a b -> p (a b)"), ps[bk][:, 0:512],
                           [], PS(bk) + [("Gs", None)])
                if gi == 0:
                    dump(Gs[:, 0, :], 6048, 128, ("Gs", None)); dump(Gs[:, 200, :], 6176, 128, ("Gs", None))

            LA = 2
            NZ = 3

            def sweep(gi, gen):
                par = gi % 2
                h2T = h2Ts[par]
                hn = "h2T%d" % par

                def zstage(c):
                    sc, cl = c // 2, c % 2
                    us = sc % 2
                    if cl == 0:
                        dma("sp", Uc[us][:].rearrange("p a b c -> p (a b c)"), uTb[:, sc * 2048:(sc + 1) * 2048],
                            [("uTb", None)], [("Uc%d" % us, None)], "Uc%d" % us)
                        vs_ = sc % 4
                        dma("sp", Vc[vs_][:].rearrange("p a b -> p (a b)"), vb[:, sc * 2048:(sc + 1) * 2048],
                            [("vb", None)], [("Vc%d" % vs_, None)], "Vc%d" % vs_)
                    bk = 4 + c % NZ
                    for kc in range(8):
                        mm(ps[bk][:, 0:256], Uc[us][:, cl, kc, :], h2T[:, kc, :], kc == 0, kc == 7,
                           [("Uc%d" % us, None), (hn, None)], PS(bk))
                    act(ps[bk][:, 0:256], ps[bk][:, 0:256], AF.Gelu_apprx_tanh, [], PS(bk))
                    tt("dve", At[c % 4][:], ps[bk][:, 0:256], Gs[:, :, c], ALU.mult, [("Gs", None)], PS(bk) + [("At%d" % (c % 4), None)])

                def ostage(c):
                    sc, cl = c // 2, c % 2
                    vs_ = sc % 4
                    for tk in range(2):
                        for hf in range(2):
                            mm(ps[tk * 2 + hf][:, 0:512], At[c % 4][:, tk * 128:(tk + 1) * 128], Vc[vs_][:, cl, hf * 512:(hf + 1) * 512],
                               c == 0, c == 127, [("At%d" % (c % 4), None), ("Vc%d" % vs_, None)], PS(tk * 2 + hf))

                for step in range(128 + LA):
                    if step < 128:
                        zstage(step)
                    if step >= LA:
                        ostage(step - LA)
                    if gen is not None and step >= 4:
                        next(gen, None)

            def finalize(gi):
                par = gi % 2
                for tk in range(2):
                    ti = 2 * gi + tk
                    xn_ = "x1g%d_%d" % (par, tk)
                    for hf in range(2):
                        tt("dve", yt[:, hf * 512:(hf + 1) * 512], ps[tk * 2 + hf][:, 0:512], gate2_bc[:, hf * 512:(hf + 1) * 512], ALU.mult,
                           [("gate2_bc", None)], PS(tk * 2 + hf) + [("yt", None)])
                    if ti == 0:
                        dump(yt[:], 6304, 1024, ("yt", None))
                    tt("pool", yt[:], yt[:], x1g[par][tk][:], ALU.add, [(xn_, None)], [("yt", None)])
                    act(fjunk[:], yt[:], AF.Square, [("yt", None)], [("fjunk", None), ("fss", None)], accum=fss[:])
                    tsc("dve", frs[:], fss[:], 1.0 / D, EPS, ALU.mult, ALU.add, [("fss", None)], [("frs", None)])
                    act(frs[:], frs[:], AF.Sqrt, [("frs", None)], [("frs", None)])
                    P.op("dve", lambda e: e.reciprocal(out=frs[:], in_=frs[:]), reads=[("frs", None)], writes=[("frs", None)])
                    o_ = x1g[par][tk]
                    P.op("dve", lambda e, o_=o_: e.scalar_tensor_tensor(out=o_[:], in0=yt[:], scalar=frs[:, 0:1], in1=fg_bc[:],
                                                                          op0=ALU.mult, op1=ALU.mult),
                         reads=[("yt", None), ("frs", None), ("fg_bc", None)], writes=[(xn_, None)])
                    dma("sp", out[ti * 128:(ti + 1) * 128, :], o_[:], [(xn_, None)], [("out", ti)], "oo" + xn_)

            for _ in prep(0):
                pass
            for gi in range(16):
                stage(12)
                gbuild(gi)
                stage(13)
                gen = prep(gi + 1) if gi + 1 < 16 else None
                sweep(gi, gen)
                if gen is not None:
                    for _ in gen:
                        pass
                stage(14)
                finalize(gi)
      except _Stop:
        pass
      P.finish("sp")
      build_program.stats = dict(nops=len(P.ops), nwaits=P.nwaits, nsems=len(P.sems))
    return nc


def _t5_bucket_np(rel):
    import jax
    import jax.numpy as jnp
    import math
    cpu = jax.devices("cpu")[0]
    with jax.default_device(cpu):
        rel = jnp.asarray(rel, dtype=jnp.int32)
        half = 16
        max_exact = 8
        ret = jnp.where(rel > 0, half, 0)
        n = jnp.abs(rel)
        nf = jnp.maximum(n, 1).astype(jnp.float32)
        large = max_exact + (jnp.log(nf / max_exact) / math.log(128 / max_exact) * (half - max_exact)).astype(jnp.int32)
        large = jnp.minimum(large, half - 1)
        return np.asarray(ret + jnp.where(n < max_exact, n, large))


def _col(v):
    return np.ascontiguousarray(np.asarray(v, np.float32).reshape(8, 128).T)


def _bias_a(t5_table):
    k = np.arange(128)[:, None]
    q = np.arange(128)[None, :]
    outp = np.full((3, 3, 128, 8, 128), NEGM, np.float32)
    for c in range(3):
        rel = (c * 128 + k - 128) - q
        band = np.abs(rel) <= 128
        bk = _t5_bucket_np(rel)
        vals = t5_table[bk]
        vals = np.transpose(vals, (0, 2, 1))[:, _HORDER, :]
        m = np.broadcast_to(band[:, None, :], vals.shape)
        blk = np.where(m, vals, np.float32(NEGM)).astype(np.float32)
        outp[0, c] = blk
        outp[1, c] = blk if c != 0 else NEGM
        outp[2, c] = blk if c != 2 else NEGM
    return outp.reshape(3, 3, 128, 1024)


def _bias_b_chunk(rpb, r0, kp):
    outp = np.full((128, 8, 128), NEGM, np.float32)
    kk = np.arange(128)
    kr, kcol = kk // 64, kk % 64
    y = 2 * kp + kr
    qq = np.arange(128)
    qr, c = qq // 64, qq % 64
    r = r0 + qr
    rs = np.clip(r - 4, 0, 120)
    cs = np.clip(c - 8, 0, 48)
    Y = y[:, None]
    KC = kcol[:, None]
    valid = (Y >= rs[None, :]) & (Y < rs[None, :] + 8) & (KC >= cs[None, :]) & (KC < cs[None, :] + 16) & (Y >= 0) & (Y < 128)
    ro = np.clip(Y - r[None, :] + 7, 0, 14)
    co = np.clip(KC - c[None, :] + 15, 0, 30)
    vals = rpb[:, ro, co]
    vals = np.transpose(vals, (1, 0, 2))[:, _HORDER, :]
    m = np.broadcast_to(valid[:, None, :], vals.shape)
    return np.where(m, vals, np.float32(NEGM)).astype(np.float32)


_PREP_ONLY = False
_HORDER = [0, 2, 4, 6, 1, 3, 5, 7]
_SP_LIST = [(0, list(range(-2, 4))), (1, list(range(-1, 4))), (30, list(range(28, 33))), (31, list(range(28, 34)))]


def kernel(x, c, w_ada, b_ada, norm1_g, w_in, sink_a, t5_table, rpb_b, out_norm_a, out_norm_b,
           w_out, norm2_g, w_query, sub_keys, u_experts, v_experts, final_g):
    f = lambda a: np.ascontiguousarray(np.asarray(a, dtype=np.float32))
    x = f(x); c = f(c); w_ada = f(w_ada)[0]; b_ada = f(b_ada); w_in = f(w_in)[0]
    t5 = f(t5_table); rpb = f(rpb_b)[0]; w_out = f(w_out)[0]; w_query = f(w_query)[0]
    sk = f(sub_keys)[0]; u = f(u_experts)[0]; v = f(v_experts)[0]
    shared = {
        "w_ada": w_ada, "b_ada": b_ada.reshape(1, 6 * D), "g1c": _col(f(norm1_g)[0]), "g2c": _col(f(norm2_g)[0]),
        "onc": _col(np.concatenate([f(out_norm_a)[0], f(out_norm_b)[0]])), "w_in": w_in, "sink": f(sink_a).reshape(1, 8),
        "biasA": _bias_a(t5), "w_out": w_out, "w_query": w_query,
        "skT": np.ascontiguousarray(np.transpose(sk, (3, 0, 1, 2)).reshape(128, 2048)),
        "u_lay": np.ascontiguousarray(np.transpose(u.reshape(128, 128, 8, 128), (3, 0, 2, 1)).reshape(128, 131072)),
        "v_lay": np.ascontiguousarray(np.transpose(v.reshape(128, 128, D), (1, 0, 2)).reshape(128, 131072)),
        "final_g": f(final_g).reshape(1, D), "ident": np.eye(128, dtype=np.float32),
        "iota": np.ascontiguousarray(np.broadcast_to(np.arange(128, dtype=np.float32)[None, :], (128, 128))),
        "biasBg": np.stack([_bias_b_chunk(rpb, 20, 10 - 2 + j) for j in range(5)]).reshape(5, 128, 1024),
    }
    in_maps = []
    for core in range(NCORES):
        b, half = core // 2, core % 2
        start = half * TOK - HALO
        xe = np.zeros((NEXT * 128, D), np.float32)
        lo, hi = max(start, 0), min(start + NEXT * 128, SEQ)
        xe[lo - start:hi - start] = x[b, lo:hi]
        sp = []
        for (li, kps) in _SP_LIST:
            gp = half * 32 + li
            for kp in kps:
                sp.append(_bias_b_chunk(rpb, 2 * gp, half * 32 + kp))
        bA = shared["biasA"].copy()
        if half != 0:
            bA[1] = bA[0]
        if half != 1:
            bA[2] = bA[0]
        m = dict(shared)
        m.update({"x_ext": xe, "c_col": _col(c[b]), "biasA": bA, "biasBs": np.stack(sp).reshape(22, 128, 1024)})
        in_maps.append(m)
    if _PREP_ONLY:
        return in_maps
    nc = build_program()
    res = run_bass_kernel_spmd(nc, in_maps, core_ids=list(range(NCORES)))
    outp = np.zeros((4, SEQ, D), np.float32)
    for core in range(NCORES):
        b, half = core // 2, core % 2
        outp[b, half * TOK:(half + 1) * TOK] = res.results[core]["out"]
    return outp


if __name__ == "__main__":
    import time
    t0 = time.time()
    build_program()
    print("built", time.time() - t0, build_program.stats)
```

```python
import numpy as np
from contextlib import ExitStack
import concourse.bass as bass
import concourse.mybir as mybir
from concourse.bass_utils import run_bass_kernel_spmd

F32 = mybir.dt.float32
BF16 = mybir.dt.bfloat16
U32 = mybir.dt.uint32
AF = mybir.ActivationFunctionType
ALU = mybir.AluOpType
AX = mybir.AxisListType

SAME_ENGINE_SYNC = True
EPS = 1e-6
NEGM = -1.0e4
NCORES = 8
SEQ = 8192
TOK = 4096
HALO = 256
NEXT = 36
D = 1024


class Prog:
    EPOCH = 20000

    def __init__(self, nc, es):
        self.nc = nc
        self.es = es
        self.engs = {"pe": nc.tensor, "act": nc.scalar, "dve": nc.vector, "pool": nc.gpsimd, "sp": nc.sync}
        self.sems = {}
        self.cnt = {}
        self.waited = {}
        self.state = {}
        self.ops = []
        self.engcount = {k: 0 for k in self.engs}
        self.nwaits = 0
        self.stopped = False

    def _sem(self, k):
        if k not in self.sems:
            self.sems[k] = self.es.enter_context(self.nc.semaphore("s%d" % len(self.sems)))
        return self.sems[k]

    def _entries(self, name, key):
        d = self.state.setdefault(name, {})
        if key is None:
            if None not in d:
                d[None] = [None, {}, []]
            return list(d.values())
        if key not in d:
            if None in d:
                b = d[None]
                d[key] = [b[0], dict(b[1]), list(b[2])]
            else:
                d[key] = [None, {}, []]
        return [d[key]]

    def op(self, eng, fn, reads=(), writes=(), dma=False, semkey=None):
        if self.stopped:
            return None
        idx = len(self.ops)
        deps = set()
        psdeps = set()
        for (name, key) in reads:
            for ent in self._entries(name, key):
                if ent[0] is not None:
                    deps.add(ent[0])
        for (name, key) in writes:
            tgt = psdeps if name.startswith("ps") else deps
            for ent in self._entries(name, key):
                if ent[0] is not None:
                    tgt.add(ent[0])
                tgt.update(ent[1].values())
                tgt.update(ent[2])
        e = self.engs[eng]
        for di in sorted(deps | psdeps):
            deng, dk, dval, ddma = self.ops[di]
            if (not ddma) and deng == eng and (eng == "pe" or not SAME_ENGINE_SYNC or di not in deps):
                continue
            if self.waited.get((eng, dk), 0) >= dval:
                continue
            e.wait_ge(self._sem(dk), dval)
            self.waited[(eng, dk)] = dval
            self.nwaits += 1
        ins = fn(e)
        if dma:
            k = ("dma", semkey)
            inc = 16
        else:
            self.engcount[eng] += 1
            k = ("eng", eng, self.engcount[eng] // self.EPOCH)
            inc = 1
        self.cnt[k] = self.cnt.get(k, 0) + inc
        ins.then_inc(self._sem(k), inc)
        self.ops.append((eng, k, self.cnt[k], dma))
        for (name, key) in reads:
            for ent in self._entries(name, key):
                if dma:
                    ent[2].append(idx)
                else:
                    ent[1][eng] = idx
        for (name, key) in writes:
            d = self.state[name]
            if key is None:
                for kk in list(d.keys()):
                    d[kk] = [idx, {}, []]
            else:
                d[key] = [idx, {}, []]
        return idx

    def barrier(self):
        if self.stopped:
            return
        targets = list(self.cnt.items())
        for eng, e in self.engs.items():
            for k, v in targets:
                if self.waited.get((eng, k), 0) >= v:
                    continue
                e.wait_ge(self._sem(k), v)
                self.waited[(eng, k)] = v
                self.nwaits += 1

    def finish(self, eng="sp"):
        e = self.engs[eng]
        for k, v in self.cnt.items():
            if k[0] == "dma" and self.waited.get((eng, k), 0) < v:
                e.wait_ge(self._sem(k), v)


class _Stop(Exception):
    pass


STAGE = 99
DEBUG = False
OH1_ENG = "dve"
SWEEP_MULT_ENG = "pool"
ACT_OH_EVERY = 1000000


def build_program():
    nc = bass.Bass("TRN2", target_bir_lowering=False)

    def stage(n):
        if STAGE <= n:
            P.stopped = True

    def din(name, shape, dt=F32):
        return nc.dram_tensor(name, shape, dt, kind="ExternalInput").ap()

    x_ext = din("x_ext", [NEXT * 128, D])
    c_col = din("c_col", [128, 8])
    w_ada = din("w_ada", [D, 6 * D])
    b_ada = din("b_ada", [1, 6 * D])
    g1c_d = din("g1c", [128, 8])
    g2c_d = din("g2c", [128, 8])
    onc_d = din("onc", [128, 8])
    w_in = din("w_in", [D, 2304])
    sink_d = din("sink", [1, 8])
    biasA_d = din("biasA", [3, 3, 128, 1024])
    biasBg_d = din("biasBg", [5, 128, 1024])
    biasBs_d = din("biasBs", [22, 128, 1024])
    w_out = din("w_out", [D, D])
    w_query = din("w_query", [D, 2048])
    skT_d = din("skT", [128, 2048])
    u_lay = din("u_lay", [128, 131072])
    v_lay = din("v_lay", [128, 131072])
    fg_d = din("final_g", [1, D])
    ident_d = din("ident", [128, 128])
    iota_d = din("iota", [128, 128])
    out = nc.dram_tensor("out", [TOK, D], F32, kind="ExternalOutput").ap()
    dbg = nc.dram_tensor("dbg", [128, 8192], F32, kind="ExternalOutput").ap() if DEBUG else None
    x1s = nc.dram_tensor("x1s", [TOK, D], F32, kind="Internal").ap()
    uTb = nc.dram_tensor("uTb", [128, 131072], BF16, kind="Internal").ap()
    vb = nc.dram_tensor("vb", [128, 131072], BF16, kind="Internal").ap()

    with ExitStack() as es:
      P = Prog(nc, es)
      try:

        def sbuf(stack, name, shape, dt):
            return stack.enter_context(nc.sbuf_tensor("sb_" + name, shape, dt))

        ps = [es.enter_context(nc.psum_tensor("ps%d" % b, [128, 512], F32)) for b in range(8)]

        def PS(b):
            return [("ps%d" % b, None)]

        ukey = [0]

        def dma(eng, out_ap, in_ap, reads, writes, semkey=None):
            if semkey is None:
                ukey[0] += 1
                semkey = "u%d" % ukey[0]
            return P.op(eng, lambda e: e.dma_start(out=out_ap, in_=in_ap), reads=reads, writes=writes, dma=True, semkey=semkey)

        def dump(ap, col0, width, res):
            if DEBUG:
                dma("pool", dbg[:, col0:col0 + width], ap, [res], [("dbg", col0)])

        def mm(o, lhsT, rhs, start, stop, reads, writes):
            return P.op("pe", lambda e: e.matmul(o, lhsT=lhsT, rhs=rhs, start=start, stop=stop), reads=reads, writes=writes)

        def tr(o, in_, ident, reads, writes):
            return P.op("pe", lambda e: e.transpose(out=o, in_=in_, identity=ident), reads=reads, writes=writes)

        def act(o, in_, func, reads, writes, bias=None, scale=None, accum=None):
            kw = {}
            if bias is not None:
                kw["bias"] = bias
            if scale is not None:
                kw["scale"] = scale
            if accum is not None:
                kw["accum_out"] = accum
            return P.op("act", lambda e: e.activation(out=o, in_=in_, func=func, **kw), reads=reads, writes=writes)

        def tt(eng, o, a, b, op, reads, writes):
            return P.op(eng, lambda e: e.tensor_tensor(out=o, in0=a, in1=b, op=op), reads=reads, writes=writes)

        def tsc(eng, o, a, s1, s2, op0, op1, reads, writes):
            if op1 is None:
                return P.op(eng, lambda e: e.tensor_scalar(out=o, in0=a, scalar1=s1, scalar2=None, op0=op0), reads=reads, writes=writes)
            return P.op(eng, lambda e: e.tensor_scalar(out=o, in0=a, scalar1=s1, scalar2=s2, op0=op0, op1=op1), reads=reads, writes=writes)

        def cp(eng, o, a, reads, writes):
            if eng == "act":
                return P.op("act", lambda e: e.copy(out=o, in_=a), reads=reads, writes=writes)
            return P.op(eng, lambda e: e.tensor_copy(out=o, in_=a), reads=reads, writes=writes)

        ident_f = sbuf(es, "ident_f", [128, 128], F32)
        ident_b = sbuf(es, "ident_b", [128, 128], BF16)
        iota_f = sbuf(es, "iota_f", [128, 128], F32)
        iota_b = sbuf(es, "iota_b", [128, 128], BF16)
        ones_f = sbuf(es, "ones_f", [128, 128], F32)
        g2c = sbuf(es, "g2c", [128, 8], F32)
        a2 = sbuf(es, "a2", [128, 8], F32)
        b2 = sbuf(es, "b2", [128, 8], F32)
        gate2_bc = sbuf(es, "gate2_bc", [128, D], F32)
        fg_bc = sbuf(es, "fg_bc", [128, D], F32)

        dma("sp", ident_f[:], ident_d, [], [("ident_f", None)])
        dma("sp", iota_f[:], iota_d, [], [("iota_f", None)])
        dma("sp", g2c[:], g2c_d, [], [("g2c", None)])
        dma("sp", fg_bc[:], fg_d[0].partition_broadcast(128), [], [("fg_bc", None)])
        cp("dve", ident_b[:], ident_f[:], [("ident_f", None)], [("ident_b", None)])
        cp("dve", iota_b[:], iota_f[:], [("iota_f", None)], [("iota_b", None)])
        P.op("dve", lambda e: e.memset(ones_f[:], 1.0), writes=[("ones_f", None)])

        def norm_transpose(stack_bufs, xt_ap, xt_res, acol, bcol, acol_res, dst_fn, dst_res, tag, bank=0):
            ssq, rstd, xn = stack_bufs
            act(xn[:], xt_ap, AF.Square, [xt_res], [("xn" + tag, None), ("ssq" + tag, None)], accum=ssq[:])
            tsc("dve", rstd[:], ssq[:], 1.0 / D, EPS, ALU.mult, ALU.add, [("ssq" + tag, None)], [("rstd" + tag, None)])
            act(rstd[:], rstd[:], AF.Sqrt, [("rstd" + tag, None)], [("rstd" + tag, None)])
            P.op("dve", lambda e: e.reciprocal(out=rstd[:], in_=rstd[:]), reads=[("rstd" + tag, None)], writes=[("rstd" + tag, None)])
            tsc("dve", xn[:], xt_ap, rstd[:, 0:1], None, ALU.mult, None, [xt_res, ("rstd" + tag, None)], [("xn" + tag, None)])
            psb = ps[bank][:, :].bitcast(BF16)
            for kc in range(8):
                tr(psb[:, kc * 128:(kc + 1) * 128], xn[:, kc * 128:(kc + 1) * 128], ident_b[:],
                   [("xn" + tag, None), ("ident_b", None)], PS(bank))
            for kc in range(8):
                eng = "dve" if kc % 2 == 0 else "act"
                if eng == "dve":
                    tsc("dve", dst_fn(kc), psb[:, kc * 128:(kc + 1) * 128], acol[:, kc:kc + 1], bcol[:, kc:kc + 1],
                        ALU.mult, ALU.add, [acol_res], PS(bank) + [dst_res])
                else:
                    act(dst_fn(kc), psb[:, kc * 128:(kc + 1) * 128], AF.Identity, [acol_res], PS(bank) + [dst_res],
                        bias=bcol[:, kc:kc + 1], scale=acol[:, kc:kc + 1])

        with ExitStack() as ea:
            c_sb = sbuf(ea, "c_sb", [128, 8], F32)
            c_act = sbuf(ea, "c_act", [128, 8], F32)
            g1c = sbuf(ea, "g1c", [128, 8], F32)
            onc = sbuf(ea, "onc", [128, 8], F32)
            a1 = sbuf(ea, "a1", [128, 8], F32)
            b1 = sbuf(ea, "b1", [128, 8], F32)
            adacol = sbuf(ea, "adacol", [128, 32], F32)
            sink_bc = sbuf(ea, "sink_bc", [128, 8], F32)
            expsink = sbuf(ea, "expsink", [128, 8], F32)
            zeros8 = sbuf(ea, "zeros8", [128, 8], F32)
            wo_b = sbuf(ea, "wo_b", [128, 8, D], BF16)

            dma("sp", c_sb[:], c_col, [], [("c_sb", None)])
            dma("sp", g1c[:], g1c_d, [], [("g1c", None)])
            dma("sp", onc[:], onc_d, [], [("onc", None)])
            dma("sp", sink_bc[:], sink_d[0].partition_broadcast(128), [], [("sink_bc", None)])
            P.op("dve", lambda e: e.memset(zeros8[:], 0.0), writes=[("zeros8", None)])
            act(expsink[:], sink_bc[:], AF.Exp, [("sink_bc", None)], [("expsink", None)])
            act(c_act[:], c_sb[:], AF.Silu, [("c_sb", None)], [("c_act", None)])

            with ExitStack() as e0:
                wa = [sbuf(e0, "wa%d" % i, [128, 8, 512], F32) for i in range(2)]
                ada_row = sbuf(e0, "ada_row", [1, 6 * D], F32)
                bada = sbuf(e0, "bada", [1, 6 * D], F32)
                wo_f = sbuf(e0, "wo_f", [128, 8, D], F32)
                gate1_bc = sbuf(e0, "gate1_bc", [128, D], F32)
                dma("sp", bada[:], b_ada, [], [("bada", None)])
                dma("sp", wo_f[:], w_out.rearrange("(kc p) n -> p kc n", p=128), [], [("wo_f", None)])
                w_ada_v = w_ada.rearrange("(kc p) n -> p kc n", p=128)
                for blk in range(12):
                    sl = blk % 2
                    dma("sp", wa[sl][:], w_ada_v[:, :, blk * 512:(blk + 1) * 512], [], [("wa%d" % sl, None)], "wa%d" % sl)
                    for kc in range(8):
                        mm(ps[7][0:1, 0:512], c_act[:, kc:kc + 1], wa[sl][:, kc, :], kc == 0, kc == 7,
                           [("c_act", None), ("wa%d" % sl, None)], PS(7))
                    tt("dve", ada_row[0:1, blk * 512:(blk + 1) * 512], ps[7][0:1, 0:512], bada[0:1, blk * 512:(blk + 1) * 512],
                       ALU.add, [("bada", None)], PS(7) + [("ada_row", blk)])
                for vi, v in enumerate([0, 1, 3, 4]):
                    for kc in range(8):
                        mm(ps[7][:, vi * 8 + kc:vi * 8 + kc + 1], ada_row[0:1, v * D + kc * 128:v * D + (kc + 1) * 128],
                           ident_f[0:1, 0:1], True, True, [("ada_row", None), ("ident_f", None)], PS(7))
                cp("dve", adacol[:], ps[7][:, 0:32], [], PS(7) + [("adacol", None)])
                tsc("dve", a1[:], adacol[:, 8:16], 1.0, None, ALU.add, None, [("adacol", None)], [("a1", None)])
                tt("dve", a1[:], a1[:], g1c[:], ALU.mult, [("g1c", None)], [("a1", None)])
                cp("dve", b1[:], adacol[:, 0:8], [("adacol", None)], [("b1", None)])
                tsc("dve", a2[:], adacol[:, 24:32], 1.0, None, ALU.add, None, [("adacol", None)], [("a2", None)])
                tt("dve", a2[:], a2[:], g2c[:], ALU.mult, [("g2c", None)], [("a2", None)])
                cp("dve", b2[:], adacol[:, 16:24], [("adacol", None)], [("b2", None)])
                for gi, (v, dst, dname) in enumerate([(2, gate1_bc, "gate1_bc"), (5, gate2_bc, "gate2_bc")]):
                    for hf in range(2):
                        mm(ps[1 + hf][:, 0:512], ones_f[0:1, 0:128], ada_row[0:1, v * D + hf * 512:v * D + (hf + 1) * 512],
                           True, True, [("ones_f", None), ("ada_row", None)], PS(1 + hf))
                        cp("dve", dst[:, hf * 512:(hf + 1) * 512], ps[1 + hf][:, 0:512], [], PS(1 + hf) + [(dname, None)])
                for kc in range(8):
                    P.op("dve", lambda e, kc=kc: e.scalar_tensor_tensor(out=wo_b[:, kc, :], in0=wo_f[:, kc, :], scalar=onc[:, kc:kc + 1],
                                                                          in1=gate1_bc[:], op0=ALU.mult, op1=ALU.mult),
                         reads=[("wo_f", None), ("onc", None), ("gate1_bc", None)], writes=[("wo_b", None)])

            P.barrier()
            dump(a1[:], 0, 8, ("a1", None)); dump(b1[:], 8, 8, ("b1", None)); dump(a2[:], 16, 8, ("a2", None)); dump(b2[:], 24, 8, ("b2", None))
            dump(gate2_bc[:], 32, 1024, ("gate2_bc", None)); dump(wo_b[:, 0, :], 1056, 1024, ("wo_b", None))
            stage(1)
            w_in_b = sbuf(ea, "w_in_b", [128, 8, 2304], BF16)
            wkdup = sbuf(ea, "wkdup", [128, 8, 2, 128], BF16)
            biasA = sbuf(ea, "biasA", [128, 3, 1024], BF16)
            biasBg = sbuf(ea, "biasBg", [128, 5, 1024], BF16)
            biasBs = sbuf(ea, "biasBs", [128, 6, 1024], BF16)
            w_in_v = w_in.rearrange("(kc p) n -> p kc n", p=128)
            for hf in range(2):
                dma("pool", w_in_b[:, :, hf * 1152:(hf + 1) * 1152], w_in_v[:, :, hf * 1152:(hf + 1) * 1152],
                    [], [("w_in_b", None)])
            dma("pool", biasA[:], biasA_d[0].rearrange("c k n -> k c n"), [], [("biasA", None)])
            dma("pool", biasBg[:], biasBg_d.rearrange("c k n -> k c n"), [], [("biasBg", None)])
            for g in range(2):
                for dup in range(2):
                    cp("pool", wkdup[:, :, g, dup * 64:(dup + 1) * 64], w_in_b[:, :, 512 + g * 64:512 + (g + 1) * 64],
                       [("w_in_b", None)], [("wkdup", None)])
            hT = sbuf(ea, "hT", [128, 8, 1024], BF16)
            KA = sbuf(ea, "KA", [128, 2, 1024], BF16)
            KB = sbuf(ea, "KB", [128, 4, 1024], BF16)
            QA = sbuf(ea, "QA", [128, 4, 512], BF16)
            QB = sbuf(ea, "QB", [128, 4, 512], BF16)
            VA = sbuf(ea, "VA", [128, 8, 2, 65], BF16)
            VB = sbuf(ea, "VB", [128, 8, 8, 65], BF16)
            NXT = 3
            xts = [sbuf(ea, "xt%d" % i, [128, D], F32) for i in range(NXT)]
            xrs = [sbuf(ea, "xr%d" % i, [128, D], F32) for i in range(2)]
            nbs = [(sbuf(ea, "ssqA%d" % i, [128, 1], F32), sbuf(ea, "rstdA%d" % i, [128, 1], F32), sbuf(ea, "xnA%d" % i, [128, D], BF16))
                   for i in range(2)]
            Pb = [sbuf(ea, "Pb0", [128, 3, 1024], BF16), sbuf(ea, "Pb1", [128, 6, 1024], BF16),
                  sbuf(ea, "Pb2", [128, 3, 1024], BF16), sbuf(ea, "Pb3", [128, 6, 1024], BF16)]
            den = sbuf(ea, "den", [128, 8], F32)
            rden = sbuf(ea, "rden", [128, 8], F32)
            o_t = sbuf(ea, "o_t", [128, 512], BF16)
            oss = sbuf(ea, "oss", [128, 1], F32)
            orr = sbuf(ea, "orr", [128, 1], F32)
            on_a = sbuf(ea, "on_a", [128, 512], BF16)
            on_b = sbuf(ea, "on_b", [128, 512], BF16)
            mixT = sbuf(ea, "mixT", [128, 8, 128], BF16)
            P.op("dve", lambda e: e.memset(VA[:], 1.0), writes=[("VA", None)])
            P.op("dve", lambda e: e.memset(VB[:], 1.0), writes=[("VB", None)])

            sstate = {"sb": 0, "pb": 0, "x": 0}

            def attn_tile(pb, nch, kT, q, bias, V, sinkt, dst, dst_name, rd, pvb):
                Pt = Pb[pb]
                pname = "Pb%d" % pb
                for c in range(nch):
                    for grp in range(2):
                        bk = 3 + sstate["sb"]
                        sstate["sb"] ^= 1
                        mm(ps[bk][:, 0:512], ident_b[:], bias(c)[:, grp * 512:(grp + 1) * 512], True, False,
                           [("ident_b", None)] + rd, PS(bk))
                        for r in range(4):
                            h = grp + 2 * r
                            mm(ps[bk][:, r * 128:(r + 1) * 128], kT(h, c), q(h), False, r == 3, rd, PS(bk))
                        act(Pt[:, c, grp * 512:(grp + 1) * 512], ps[bk][:, 0:512], AF.Exp, [], PS(bk) + [(pname, None)])

                def pv_phase():
                    for h in range(8):
                        bk = pvb[h // 4]
                        for c in range(nch):
                            pos = (h % 2) * 4 + h // 2
                            mm(ps[bk][:, (h % 4) * 65:(h % 4) * 65 + 65], Pt[:, c, pos * 128:(pos + 1) * 128], V(h, c), c == 0, c == nch - 1,
                               [(pname, None)] + rd, PS(bk))

                def norm_phase():
                    for bnk in range(2):
                        pv = ps[pvb[bnk]][:, 0:260].rearrange("p (h e) -> p h e", e=65)
                        tt("dve", den[:, bnk * 4:(bnk + 1) * 4], pv[:, :, 64], sinkt[:, bnk * 4:(bnk + 1) * 4], ALU.add,
                           [("expsink", None), ("zeros8", None)], PS(pvb[bnk]) + [("den", None)])
                    P.op("dve", lambda e: e.reciprocal(out=rden[:], in_=den[:]), reads=[("den", None)], writes=[("rden", None)])
                    for bnk in range(2):
                        pv = ps[pvb[bnk]][:, 0:260].rearrange("p (h e) -> p h e", e=65)
                        ov = o_t[:, bnk * 256:(bnk + 1) * 256].rearrange("p (h e) -> p h e", e=64)
                        rb = rden[:, bnk * 4:(bnk + 1) * 4].unsqueeze(2).broadcast_to([128, 4, 64])
                        tt("dve", ov, pv[:, :, 0:64], rb, ALU.mult, [("rden", None)], PS(pvb[bnk]) + [("o_t", None)])
                    act(dst[:], o_t[:], AF.Square, [("o_t", None)], [(dst_name, None), ("oss", None)], accum=oss[:])
                    tsc("dve", orr[:], oss[:], 1.0 / 512, EPS, ALU.mult, ALU.add, [("oss", None)], [("orr", None)])
                    act(orr[:], orr[:], AF.Sqrt, [("orr", None)], [("orr", None)])
                    P.op("dve", lambda e: e.reciprocal(out=orr[:], in_=orr[:]), reads=[("orr", None)], writes=[("orr", None)])
                    tsc("dve", dst[:], o_t[:], orr[:, 0:1], None, ALU.mult, None, [("o_t", None), ("orr", None)], [(dst_name, None)])

                return pv_phase, norm_phase

            sp_off = {0: 0, 1: 6, 30: 11, 31: 16}
            for s in range(8):
                if s == 1:
                    stage(6)
                def x_load(te):
                    xs = te % NXT
                    dma("sp", xts[xs][:], x_ext[te * 128:(te + 1) * 128, :], [], [("xt%d" % xs, None)], "xt%d" % xs)

                def nt_stats(te):
                    if te in nstate["stats"]:
                        return
                    nstate["stats"].add(te)
                    if te + NXT - 1 < NEXT:
                        x_load(te + NXT - 1)
                    xs = te % NXT
                    xt = xts[xs]
                    ssq, rstd, xn = nbs[te % 2]
                    tg = "A%d" % (te % 2)
                    xres = ("xt%d" % xs, None)
                    act(xn[:], xt[:], AF.Square, [xres], [("xn" + tg, None), ("ssq" + tg, None)], accum=ssq[:])
                    tsc("dve", rstd[:], ssq[:], 1.0 / D, EPS, ALU.mult, ALU.add, [("ssq" + tg, None)], [("rstd" + tg, None)])
                    act(rstd[:], rstd[:], AF.Sqrt, [("rstd" + tg, None)], [("rstd" + tg, None)])
                    P.op("dve", lambda e: e.reciprocal(out=rstd[:], in_=rstd[:]), reads=[("rstd" + tg, None)], writes=[("rstd" + tg, None)])
                    tsc("dve", xn[:], xt[:], rstd[:, 0:1], None, ALU.mult, None, [xres, ("rstd" + tg, None)], [("xn" + tg, None)])

                def nt_rest(te):
                    tl = te % 8
                    xn = nbs[te % 2][2]
                    tg = "A%d" % (te % 2)
                    psb0 = ps[0][:, :].bitcast(BF16)
                    psb7 = ps[7][:, :].bitcast(BF16)
                    for kc in range(8):
                        pb_, bk_ = (psb0, 0) if kc < 4 else (psb7, 7)
                        tr(pb_[:, (kc % 4) * 128:(kc % 4 + 1) * 128], xn[:, kc * 128:(kc + 1) * 128], ident_b[:],
                           [("xn" + tg, None), ("ident_b", None)], PS(bk_))
                    for kk in range(4):
                        for kc in (kk, kk + 4):
                            dstk = hT[:, kc, tl * 128:(tl + 1) * 128]
                            if kc < 4:
                                tsc("dve", dstk, psb0[:, kc * 128:(kc + 1) * 128], a1[:, kc:kc + 1], b1[:, kc:kc + 1],
                                    ALU.mult, ALU.add, [("a1", None)], PS(0) + [("hT", (tl, kc))])
                            else:
                                act(dstk, psb7[:, (kc - 4) * 128:(kc - 3) * 128], AF.Identity, [("a1", None)], PS(7) + [("hT", (tl, kc))],
                                    bias=b1[:, kc:kc + 1], scale=a1[:, kc:kc + 1])
                    for kc in range(8):
                        mm(ps[1][:, 0:512], hT[:, kc, tl * 128:(tl + 1) * 128], w_in_b[:, kc, 1792:2304], kc == 0, kc == 7,
                           [("hT", (tl, kc)), ("w_in_b", None)], PS(1))
                        mm(ps[2][:, 0:128], hT[:, kc, tl * 128:(tl + 1) * 128], w_in_b[:, kc, 640:768], kc == 0, kc == 7,
                           [("hT", (tl, kc)), ("w_in_b", None)], PS(2))
                    cp("act", VB[:, tl, :, 0:64], ps[1][:, 0:512].rearrange("p (h e) -> p h e", e=64), [], PS(1) + [("VB", tl)])
                    cp("dve", VA[:, tl, :, 0:64], ps[2][:, 0:128].rearrange("p (h e) -> p h e", e=64), [], PS(2) + [("VA", tl)])

                if s == 0:
                    nstate = {"stats": set()}
                    for te0 in range(NXT - 1):
                        x_load(te0)
                newt = list(range(0, 8)) if s == 0 else list(range(4 * s + 4, 4 * s + 8))
                for te in newt:
                    nt_stats(te)
                    if te + 1 < NEXT:
                        nt_stats(te + 1)
                    nt_rest(te)
                stage(2)
                pj = 0
                halves = [0, 1] if s == 0 else [((4 * s + 4) % 8) // 4]
                for hf in halves:
                    for ch in range(6):
                        bk = 1 + pj % 2
                        pj += 1
                        for kc in range(8):
                            lw = wkdup[:, kc, ch, :] if ch < 2 else w_in_b[:, kc, 1280 + (ch - 2) * 128:1280 + (ch - 1) * 128]
                            mm(ps[bk][:, 0:512], lw, hT[:, kc, hf * 512:(hf + 1) * 512], kc == 0, kc == 7,
                               [("hT", None), ("w_in_b", None), ("wkdup", None)], PS(bk))
                        if ch < 2:
                            cp("act" if pj % 2 else "dve", KA[:, ch, hf * 512:(hf + 1) * 512], ps[bk][:, 0:512], [], PS(bk) + [("KA", None)])
                        else:
                            cp("act" if pj % 2 else "dve", KB[:, ch - 2, hf * 512:(hf + 1) * 512], ps[bk][:, 0:512], [], PS(bk) + [("KB", None)])
                q0 = (4 * s + 2) % 8
                qparts = [(q0 * 128, 512, 0)] if q0 == 2 else [(768, 256, 0), (0, 256, 256)]
                for ch in range(8):
                    bk = 1 + pj % 2
                    pj += 1
                    col0 = ch * 128 if ch < 4 else 768 + (ch - 4) * 128
                    first = True
                    for kc in range(8):
                        for (hc, hn_, pc_) in qparts:
                            mm(ps[bk][:, pc_:pc_ + hn_], w_in_b[:, kc, col0:col0 + 128], hT[:, kc, hc:hc + hn_], first,
                               kc == 7 and (hc, hn_, pc_) == qparts[-1], [("hT", None), ("w_in_b", None)], PS(bk))
                            first = False
                    dq = QA[:, ch, :] if ch < 4 else QB[:, ch - 4, :]
                    dn = "QA" if ch < 4 else "QB"
                    act(dq, ps[bk][:, 0:512], AF.Identity, [], PS(bk) + [(dn, None)], scale=0.125)
                stage(3)
                def s_phase(j):
                    i = 4 * s + j
                    tl = j + 2
                    sl_ = lambda p_: (4 * s + p_) % 8
                    pbo = 2 * (j % 2)
                    if 2 <= i < 18:
                        pc = i - 2
                        for (src_d, dst_d, nm) in ((u_lay, uTb, "uTb"), (v_lay, vb, "vb")):
                            dma("pool", dst_d[:, pc * 8192:(pc + 1) * 8192].rearrange("p (a b) -> p a b", b=2048),
                                src_d[:, pc * 8192:(pc + 1) * 8192].rearrange("p (a b) -> p a b", b=2048), [], [(nm, pc)], "cv" + nm)
                    rdA = [("KA", None), ("QA", None), ("VA", None), ("biasA", None), ("biasBs", None)]
                    if i in (0, 31):
                        dma("pool", biasBs[:, 0:3, :], biasA_d[1 if i == 0 else 2].rearrange("c k n -> k c n"),
                            [], [("biasBs", None)], "bs")
                        bfa = lambda c: biasBs[:, c, :]
                    else:
                        bfa = lambda c: biasA[:, c, :]
                    wpv, wnorm = attn_tile(pbo, 3,
                              lambda h, c, tl=tl: KA[(h % 2) * 64:(h % 2) * 64 + 64, h // 4, sl_(tl - 1 + c) * 128:sl_(tl - 1 + c) * 128 + 128],
                              lambda h, j=j: QA[(h % 2) * 64:(h % 2) * 64 + 64, h // 2, j * 128:(j + 1) * 128],
                              bfa,
                              lambda h, c, tl=tl: VA[:, sl_(tl - 1 + c), h // 4, :],
                              expsink, on_a, "on_a", rdA, (5, 6))
                    stage(4)
                    rdB = [("KB", None), ("QB", None), ("VB", None), ("biasBg", None), ("biasBs", None)]
                    if i in sp_off:
                        nchb = 6 if i in (0, 31) else 5
                        dma("pool", biasBs[:, 0:nchb, :], biasBs_d[sp_off[i]:sp_off[i] + nchb].rearrange("c k n -> k c n"),
                            [], [("biasBs", None)], "bs")
                        c0 = tl - 3 if i == 31 else tl - 2
                        bfn = lambda c: biasBs[:, c, :]
                    else:
                        nchb = 5
                        c0 = tl - 2
                        bfn = lambda c: biasBg[:, c, :]
                    bpv, bnorm = attn_tile(pbo + 1, nchb,
                              lambda h, c, c0=c0: KB[(h % 2) * 64:(h % 2) * 64 + 64, h // 2, sl_(c0 + c) * 128:sl_(c0 + c) * 128 + 128],
                              lambda h, j=j: QB[(h % 2) * 64:(h % 2) * 64 + 64, h // 2, j * 128:(j + 1) * 128],
                              bfn,
                              lambda h, c, c0=c0: VB[:, sl_(c0 + c), h, :],
                              zeros8, on_b, "on_b", rdB, (1, 2))

                    def pv():
                        wpv()
                        bpv()

                    def norms():
                        wnorm()
                        bnorm()
                        if i == 0:
                            dump(on_a[:], 2080, 512, ("on_a", None)); dump(on_b[:], 2592, 512, ("on_b", None))
                            dump(hT[:, 0, 256:384], 4128, 128, ("hT", None))

                    def tail():
                        stage(5)
                        psb = ps[0][:, :].bitcast(BF16)
                        for kc in range(8):
                            src_ = on_a if kc < 4 else on_b
                            tr(psb[:, kc * 128:(kc + 1) * 128], src_[:, (kc % 4) * 128:(kc % 4 + 1) * 128], ident_b[:],
                               [("on_a", None), ("on_b", None), ("ident_b", None)], PS(0))
                        cp("act", mixT[:].rearrange("p k t -> p (k t)"), psb[:, 0:1024], [], PS(0) + [("mixT", None)])
                        xr = xrs[i % 2]
                        dma("sp", xr[:], x_ext[(i + 2) * 128:(i + 3) * 128, :], [], [("xr%d" % (i % 2), None)], "xr%d" % (i % 2))
                        for hf in range(2):
                            for kc in range(8):
                                mm(ps[1 + hf][:, 0:512], mixT[:, kc, :], wo_b[:, kc, hf * 512:(hf + 1) * 512], kc == 0, kc == 7,
                                   [("mixT", None), ("wo_b", None)], PS(1 + hf))
                            tt("dve", xr[:, hf * 512:(hf + 1) * 512], ps[1 + hf][:, 0:512], xr[:, hf * 512:(hf + 1) * 512], ALU.add,
                               [("xr%d" % (i % 2), None)], PS(1 + hf) + [("xr%d" % (i % 2), None)])
                        if i == 0:
                            dump(xr[:], 3104, 1024, ("xr0", None))
                        dma("sp", x1s[i * 128:(i + 1) * 128, :], xr[:], [("xr%d" % (i % 2), None)], [("x1s", i)], "x1o%d" % (i % 2))

                    return pv, norms, tail

                prev = s_phase(0)
                prev[0]()
                for j in range(1, 4):
                    cur = s_phase(j)
                    prev[1]()
                    prev[2]()
                    cur[0]()
                    prev = cur
                prev[1]()
                prev[2]()

        P.barrier()
        stage(7)
        with ExitStack() as eb:
            wq = sbuf(eb, "wq", [128, 8, 2048], BF16)
            skT = sbuf(eb, "skT", [128, 2048], BF16)
            dma("pool", wq[:], w_query.rearrange("(kc p) n -> p kc n", p=128), [], [("wq", None)])
            dma("pool", skT[:], skT_d, [], [("skT", None)])
            stage(8)
            Gs = sbuf(eb, "Gs", [128, 256, 128], BF16)
            h2Ts = [sbuf(eb, "h2T%d" % i, [128, 8, 256], BF16) for i in range(2)]
            qhT = sbuf(eb, "qhT", [128, 16, 256], BF16)
            S = sbuf(eb, "S", [128, 16, 128], F32)
            V16 = sbuf(eb, "V16", [128, 16, 16], F32)
            I16 = sbuf(eb, "I16", [128, 16, 16], U32)
            I16f = sbuf(eb, "I16f", [128, 16, 16], F32)
            cand = sbuf(eb, "cand", [128, 8, 256], F32)
            eq = cand[:].rearrange("p h (a b) -> p h a b", b=16)
            VS = sbuf(eb, "VS", [128, 8, 16], F32)
            CI = sbuf(eb, "CI", [128, 8, 16], U32)
            CIf = sbuf(eb, "CIf", [128, 8, 16], F32)
            CAf = sbuf(eb, "CAf", [128, 8, 16], F32)
            CBf = sbuf(eb, "CBf", [128, 8, 16], F32)
            E1 = sbuf(eb, "E1", [128, 128], F32)
            E2 = sbuf(eb, "E2", [128, 128], F32)
            Gt = sbuf(eb, "Gt", [128, 128], F32)
            gsum = sbuf(eb, "gsum", [128, 8], F32)
            E1T = sbuf(eb, "E1T", [128, 256], F32)
            E2T = sbuf(eb, "E2T", [128, 256], F32)
            GT = sbuf(eb, "GT", [128, 256], F32)
            Ag = [sbuf(eb, "Ag%d" % i, [128, 256], BF16) for i in range(4)]
            NOH = 6
            OH1 = [sbuf(eb, "OH1_%d" % i, [128, 128], BF16) for i in range(NOH)]
            OH2 = [sbuf(eb, "OH2_%d" % i, [128, 128], BF16) for i in range(NOH)]
            Uc = [sbuf(eb, "Uc%d" % i, [128, 2, 8, 128], BF16) for i in range(2)]
            Vc = [sbuf(eb, "Vc%d" % i, [128, 2, D], BF16) for i in range(4)]
            At = [sbuf(eb, "At%d" % i, [128, 256], BF16) for i in range(4)]
            x1g = [[sbuf(eb, "x1g%d_%d" % (p_, i), [128, D], F32) for i in range(2)] for p_ in range(2)]
            yt = sbuf(eb, "yt", [128, D], F32)
            nb2 = (sbuf(eb, "ssqB", [128, 1], F32), sbuf(eb, "rstdB", [128, 1], F32), sbuf(eb, "xnB", [128, D], BF16))
            fjunk = sbuf(eb, "fjunk", [128, D], BF16)
            fss = sbuf(eb, "fss", [128, 1], F32)
            frs = sbuf(eb, "frs", [128, 1], F32)
            PB = 7

            def top16_batch(items):
                for (s_, sr, vd, idd, vr, ir) in items:
                    P.op("dve", lambda e, s_=s_, vd=vd: e.max(out=vd[:, 0:8], in_=s_), reads=[sr], writes=[vr])
                yield
                for (s_, sr, vd, idd, vr, ir) in items:
                    P.op("dve", lambda e, s_=s_, vd=vd, idd=idd: e.max_index(out=idd[:, 0:8], in_max=vd[:, 0:8], in_values=s_),
                         reads=[sr, vr], writes=[ir])
                yield
                for (s_, sr, vd, idd, vr, ir) in items:
                    P.op("dve", lambda e, s_=s_, vd=vd: e.match_replace(out=s_, in_to_replace=vd[:, 0:8], in_values=s_, imm_value=-1e30),
                         reads=[vr], writes=[sr])
                yield
                for (s_, sr, vd, idd, vr, ir) in items:
                    P.op("dve", lambda e, s_=s_, vd=vd: e.max(out=vd[:, 8:16], in_=s_), reads=[sr], writes=[vr])
                yield
                for (s_, sr, vd, idd, vr, ir) in items:
                    P.op("dve", lambda e, s_=s_, vd=vd, idd=idd: e.max_index(out=idd[:, 8:16], in_max=vd[:, 8:16], in_values=s_),
                         reads=[sr, vr], writes=[ir])
                yield

            def prep(gi):
                par = gi % 2
                h2T = h2Ts[par]
                hn = "h2T%d" % par
                for tk in range(2):
                    ti = 2 * gi + tk
                    xn_ = "x1g%d_%d" % (par, tk)
                    dma("sp", x1g[par][tk][:], x1s[ti * 128:(ti + 1) * 128, :], [("x1s", ti)], [(xn_, None)], xn_)
                    norm_transpose(nb2, x1g[par][tk][:], (xn_, None), a2, b2, ("a2", None),
                                   lambda kc, tk=tk: h2T[:, kc, tk * 128:(tk + 1) * 128], (hn, None), "B", bank=PB)
                    yield
                stage(9)
                for c in range(16):
                    for kc in range(8):
                        mm(ps[PB][:, 0:256], wq[:, kc, c * 128:(c + 1) * 128], h2T[:, kc, :], kc == 0, kc == 7,
                           [("wq", None), (hn, None)], PS(PB))
                    cp("act", qhT[:, c, :], ps[PB][:, 0:256], [], PS(PB) + [("qhT", None)])
                    yield
                for tk in range(2):
                    for cg in range(4):
                        for cc in range(4):
                            c = cg * 4 + cc
                            mm(ps[PB][:, cc * 128:(cc + 1) * 128], qhT[:, c, tk * 128:(tk + 1) * 128], skT[:, c * 128:(c + 1) * 128],
                               True, True, [("qhT", None), ("skT", None)], PS(PB))
                        cp("act", S[:, cg * 4:(cg + 1) * 4, :].rearrange("p a b -> p (a b)"), ps[PB][:, 0:512],
                           [], PS(PB) + [("S", cg * 4 + q_) for q_ in range(4)])
                        yield
                    if gi == 0 and tk == 0:
                        dump(S[:, 0:2, :].rearrange("p a b -> p (a b)"), 4256, 256, ("S", None))
                        dump(h2T[:, 0, :], 5664, 256, (hn, None))
                    stage(10)
                    for half_ in range(2):
                        yield from top16_batch([(S[:, c, :], ("S", c), V16[:, c, :], I16[:, c, :], ("V16", c), ("I16", c))
                                                for c in range(half_ * 8, half_ * 8 + 8)])
                    V4 = V16[:].rearrange("p (h two) k -> p h two k", two=2)
                    c4 = cand[:].rearrange("p h (a b) -> p h a b", b=16)
                    for hq in range(4):
                        hs = slice(2 * hq, 2 * hq + 2)
                        tt("dve", c4[:, hs], V4[:, hs, 0, :].unsqueeze(3).broadcast_to([128, 2, 16, 16]),
                           V4[:, hs, 1, :].unsqueeze(2).broadcast_to([128, 2, 16, 16]), ALU.add,
                           [("V16", None)], [("cand", 2 * hq), ("cand", 2 * hq + 1)])
                        yield
                    yield from top16_batch([(cand[:, h, :], ("cand", h), VS[:, h, :], CI[:, h, :], ("VS", h), ("CI", h)) for h in range(8)])
                    stage(11)
                    MAGIC = 12582912.0
                    cp("dve", CIf[:], CI[:], [("CI", None)], [("CIf", None)])
                    tsc("dve", CAf[:], CIf[:], 0.0625, -0.46875, ALU.mult, ALU.add, [("CIf", None)], [("CAf", None)])
                    tsc("dve", CAf[:], CAf[:], MAGIC, None, ALU.add, None, [("CAf", None)], [("CAf", None)])
                    tsc("dve", CAf[:], CAf[:], -MAGIC, None, ALU.add, None, [("CAf", None)], [("CAf", None)])
                    P.op("dve", lambda e: e.scalar_tensor_tensor(out=CBf[:], in0=CAf[:], scalar=-16.0, in1=CIf[:], op0=ALU.mult, op1=ALU.add),
                         reads=[("CAf", None), ("CIf", None)], writes=[("CBf", None)])
                    cp("dve", I16f[:], I16[:], [("I16", None)], [("I16f", None)])
                    yield
                    I4 = I16f[:].rearrange("p (h two) k -> p h two k", two=2)
                    io16 = iota_f[:, 0:16].unsqueeze(1).unsqueeze(1).broadcast_to([128, 2, 16, 16])
                    for (cf, cfn, two, Ed, Edn) in ((CAf, "CAf", 0, E1, "E1"), (CBf, "CBf", 1, E2, "E2")):
                        for hq in range(4):
                            hs = slice(2 * hq, 2 * hq + 2)
                            cres = [("cand", 2 * hq), ("cand", 2 * hq + 1)]
                            tt("dve", eq[:, hs], io16, cf[:, hs].unsqueeze(3).broadcast_to([128, 2, 16, 16]), ALU.is_equal,
                               [("iota_f", None), (cfn, None)], cres)
                            yield
                            tt("dve", eq[:, hs], eq[:, hs], I4[:, hs, two, :].unsqueeze(2).broadcast_to([128, 2, 16, 16]), ALU.mult,
                               [("I16f", None)], cres)
                            yield
                            P.op("dve", lambda e, Ed=Ed, hs=hs: e.tensor_reduce(out=Ed[:].rearrange("p (h k) -> p h k", k=16)[:, hs],
                                                                                in_=eq[:, hs], axis=AX.X, op=ALU.add),
                                 reads=cres, writes=[(Edn, hq)])
                            yield
                    G3 = Gt[:].rearrange("p (h k) -> p h k", k=16)
                    tt("dve", G3, VS[:], VS[:, :, 0:1].broadcast_to([128, 8, 16]), ALU.subtract, [("VS", None)], [("Gt", None)])
                    act(Gt[:], Gt[:], AF.Exp, [("Gt", None)], [("Gt", None)])
                    P.op("dve", lambda e: e.tensor_reduce(out=gsum[:], in_=G3, axis=AX.X, op=ALU.add),
                         reads=[("Gt", None)], writes=[("gsum", None)])
                    P.op("dve", lambda e: e.reciprocal(out=gsum[:], in_=gsum[:]), reads=[("gsum", None)], writes=[("gsum", None)])
                    tt("dve", G3, G3, gsum[:].unsqueeze(2).broadcast_to([128, 8, 16]), ALU.mult, [("gsum", None)], [("Gt", None)])
                    yield
                    if gi == 0 and tk == 0:
                        dump(V16[:].rearrange("p a b -> p (a b)"), 4512, 256, ("V16", None))
                        dump(I16f[:].rearrange("p a b -> p (a b)"), 4768, 256, ("I16f", None))
                        dump(VS[:].rearrange("p a b -> p (a b)"), 5024, 128, ("VS", None))
                        dump(CIf[:].rearrange("p a b -> p (a b)"), 5152, 128, ("CIf", None))
                        dump(E1[:], 5280, 128, ("E1", None)); dump(E2[:], 5408, 128, ("E2", None)); dump(Gt[:], 5536, 128, ("Gt", None))
                    for qi, (src_, sn, dstT, dn, sc_) in enumerate(((E1, "E1", E1T, "E1T", 1.0), (E2, "E2", E2T, "E2T", 1.0),
                                                                    (Gt, "Gt", GT, "GT", 1.0))):
                        tr(ps[PB][:, 0:128], src_[:], ident_f[:], [(sn, None), ("ident_f", None)], PS(PB))
                        act(dstT[:, tk * 128:(tk + 1) * 128], ps[PB][:, 0:128], AF.Identity, [], PS(PB) + [(dn, None)], scale=sc_)
                        yield

            def gbuild(gi):
                for t in range(256):
                    sl = t % NOH
                    tsc("dve", OH2[sl][:], iota_b[:], E2T[:, t:t + 1], GT[:, t:t + 1], ALU.is_equal, ALU.mult,
                        [("iota_b", None), ("E2T", None), ("GT", None)], [("OH2_%d" % sl, None)])
                    tsc("dve", OH1[sl][:], iota_b[:], E1T[:, t:t + 1], None, ALU.is_equal, None,
                        [("iota_b", None), ("E1T", None)], [("OH1_%d" % sl, None)])
                    bk = 4 + (t // 4) % 4
                    mm(ps[bk][:, (t % 4) * 128:(t % 4 + 1) * 128], OH2[sl][:], OH1[sl][:], True, True,
                       [("OH2_%d" % sl, None), ("OH1_%d" % sl, None)], PS(bk))
                    if t % 4 == 3:
                        cp("act", Gs[:, t - 3:t + 1, :].rearrange("p a b -> p (a b)"), ps[bk][:, 0:512],
                           [], PS(bk) + [("Gs", None)])
                if gi == 0:
                    dump(Gs[:, 0, :], 6048, 128, ("Gs", None)); dump(Gs[:, 200, :], 6176, 128, ("Gs", None))

            LA = 2
            NZ = 3

            def sweep(gi, gen):
                par = gi % 2
                h2T = h2Ts[par]
                hn = "h2T%d" % par

                def zstage(c):
                    sc, cl = c // 2, c % 2
                    us = sc % 2
                    if cl == 0:
                        dma("sp", Uc[us][:].rearrange("p a b c -> p (a b c)"), uTb[:, sc * 2048:(sc + 1) * 2048],
                            [("uTb", None)], [("Uc%d" % us, None)], "Uc%d" % us)
                        vs_ = sc % 4
                        dma("sp", Vc[vs_][:].rearrange("p a b -> p (a b)"), vb[:, sc * 2048:(sc + 1) * 2048],
                            [("vb", None)], [("Vc%d" % vs_, None)], "Vc%d" % vs_)
                    bk = 4 + c % NZ
                    for kc in range(8):
                        mm(ps[bk][:, 0:256], Uc[us][:, cl, kc, :], h2T[:, kc, :], kc == 0, kc == 7,
                           [("Uc%d" % us, None), (hn, None)], PS(bk))
                    if SWEEP_MULT_ENG == "pool":
                        act(Ag[c % 4][:], ps[bk][:, 0:256], AF.Gelu_apprx_tanh, [], PS(bk) + [("Ag%d" % (c % 4), None)])
                        tt("pool", At[c % 4][:], Ag[c % 4][:], Gs[:, :, c], ALU.mult, [("Gs", None), ("Ag%d" % (c % 4), None)],
                           [("At%d" % (c % 4), None)])
                    else:
                        act(ps[bk][:, 0:256], ps[bk][:, 0:256], AF.Gelu_apprx_tanh, [], PS(bk))
                        tt("dve", At[c % 4][:], ps[bk][:, 0:256], Gs[:, :, c], ALU.mult, [("Gs", None)], PS(bk) + [("At%d" % (c % 4), None)])

                def ostage(c):
                    sc, cl = c // 2, c % 2
                    vs_ = sc % 4
                    for tk in range(2):
                        for hf in range(2):
                            mm(ps[tk * 2 + hf][:, 0:512], At[c % 4][:, tk * 128:(tk + 1) * 128], Vc[vs_][:, cl, hf * 512:(hf + 1) * 512],
                               c == 0, c == 127, [("At%d" % (c % 4), None), ("Vc%d" % vs_, None)], PS(tk * 2 + hf))

                for step in range(128 + LA):
                    if step < 128:
                        zstage(step)
                    if step >= LA:
                        ostage(step - LA)
                    if gen is not None and step >= 4:
                        next(gen, None)

            def finalize(gi):
                par = gi % 2
                for tk in range(2):
                    ti = 2 * gi + tk
                    xn_ = "x1g%d_%d" % (par, tk)
                    for hf in range(2):
                        tt("dve", yt[:, hf * 512:(hf + 1) * 512], ps[tk * 2 + hf][:, 0:512], gate2_bc[:, hf * 512:(hf + 1) * 512], ALU.mult,
                           [("gate2_bc", None)], PS(tk * 2 + hf) + [("yt", None)])
                    if ti == 0:
                        dump(yt[:], 6304, 1024, ("yt", None))
                    tt("pool", yt[:], yt[:], x1g[par][tk][:], ALU.add, [(xn_, None)], [("yt", None)])
                    act(fjunk[:], yt[:], AF.Square, [("yt", None)], [("fjunk", None), ("fss", None)], accum=fss[:])
                    tsc("dve", frs[:], fss[:], 1.0 / D, EPS, ALU.mult, ALU.add, [("fss", None)], [("frs", None)])
                    act(frs[:], frs[:], AF.Sqrt, [("frs", None)], [("frs", None)])
                    P.op("dve", lambda e: e.reciprocal(out=frs[:], in_=frs[:]), reads=[("frs", None)], writes=[("frs", None)])
                    o_ = x1g[par][tk]
                    P.op("dve", lambda e, o_=o_: e.scalar_tensor_tensor(out=o_[:], in0=yt[:], scalar=frs[:, 0:1], in1=fg_bc[:],
                                                                          op0=ALU.mult, op1=ALU.mult),
                         reads=[("yt", None), ("frs", None), ("fg_bc", None)], writes=[(xn_, None)])
                    dma("sp", out[ti * 128:(ti + 1) * 128, :], o_[:], [(xn_, None)], [("out", ti)], "oo" + xn_)

            for _ in prep(0):
                pass
            for gi in range(16):
                stage(12)
                gbuild(gi)
                stage(13)
                gen = prep(gi + 1) if gi + 1 < 16 else None
                sweep(gi, gen)
                if gen is not None:
                    for _ in gen:
                        pass
                stage(14)
                finalize(gi)
      except _Stop:
        pass
      P.finish("sp")
      build_program.stats = dict(nops=len(P.ops), nwaits=P.nwaits, nsems=len(P.sems))
    return nc


def _t5_bucket_np(rel):
    import jax
    import jax.numpy as jnp
    import math
    cpu = jax.devices("cpu")[0]
    with jax.default_device(cpu):
        rel = jnp.asarray(rel, dtype=jnp.int32)
        half = 16
        max_exact = 8
        ret = jnp.where(rel > 0, half, 0)
        n = jnp.abs(rel)
        nf = jnp.maximum(n, 1).astype(jnp.float32)
        large = max_exact + (jnp.log(nf / max_exact) / math.log(128 / max_exact) * (half - max_exact)).astype(jnp.int32)
        large = jnp.minimum(large, half - 1)
        return np.asarray(ret + jnp.where(n < max_exact, n, large))


def _col(v):
    return np.ascontiguousarray(np.asarray(v, np.float32).reshape(8, 128).T)


def _bias_a(t5_table):
    k = np.arange(128)[:, None]
    q = np.arange(128)[None, :]
    outp = np.full((3, 3, 128, 8, 128), NEGM, np.float32)
    for c in range(3):
        rel = (c * 128 + k - 128) - q
        band = np.abs(rel) <= 128
        bk = _t5_bucket_np(rel)
        vals = t5_table[bk]
        vals = np.transpose(vals, (0, 2, 1))[:, _HORDER, :]
        m = np.broadcast_to(band[:, None, :], vals.shape)
        blk = np.where(m, vals, np.float32(NEGM)).astype(np.float32)
        outp[0, c] = blk
        outp[1, c] = blk if c != 0 else NEGM
        outp[2, c] = blk if c != 2 else NEGM
    return outp.reshape(3, 3, 128, 1024)


def _bias_b_chunk(rpb, r0, kp):
    outp = np.full((128, 8, 128), NEGM, np.float32)
    kk = np.arange(128)
    kr, kcol = kk // 64, kk % 64
    y = 2 * kp + kr
    qq = np.arange(128)
    qr, c = qq // 64, qq % 64
    r = r0 + qr
    rs = np.clip(r - 4, 0, 120)
    cs = np.clip(c - 8, 0, 48)
    Y = y[:, None]
    KC = kcol[:, None]
    valid = (Y >= rs[None, :]) & (Y < rs[None, :] + 8) & (KC >= cs[None, :]) & (KC < cs[None, :] + 16) & (Y >= 0) & (Y < 128)
    ro = np.clip(Y - r[None, :] + 7, 0, 14)
    co = np.clip(KC - c[None, :] + 15, 0, 30)
    vals = rpb[:, ro, co]
    vals = np.transpose(vals, (1, 0, 2))[:, _HORDER, :]
    m = np.broadcast_to(valid[:, None, :], vals.shape)
    return np.where(m, vals, np.float32(NEGM)).astype(np.float32)


_PREP_ONLY = False
_HORDER = [0, 2, 4, 6, 1, 3, 5, 7]
_SP_LIST = [(0, list(range(-2, 4))), (1, list(range(-1, 4))), (30, list(range(28, 33))), (31, list(range(28, 34)))]


def kernel(x, c, w_ada, b_ada, norm1_g, w_in, sink_a, t5_table, rpb_b, out_norm_a, out_norm_b,
           w_out, norm2_g, w_query, sub_keys, u_experts, v_experts, final_g):
    f = lambda a: np.ascontiguousarray(np.asarray(a, dtype=np.float32))
    x = f(x); c = f(c); w_ada = f(w_ada)[0]; b_ada = f(b_ada); w_in = f(w_in)[0]
    t5 = f(t5_table); rpb = f(rpb_b)[0]; w_out = f(w_out)[0]; w_query = f(w_query)[0]
    sk = f(sub_keys)[0]; u = f(u_experts)[0]; v = f(v_experts)[0]
    shared = {
        "w_ada": w_ada, "b_ada": b_ada.reshape(1, 6 * D), "g1c": _col(f(norm1_g)[0]), "g2c": _col(f(norm2_g)[0]),
        "onc": _col(np.concatenate([f(out_norm_a)[0], f(out_norm_b)[0]])), "w_in": w_in, "sink": f(sink_a).reshape(1, 8),
        "biasA": _bias_a(t5), "w_out": w_out, "w_query": w_query,
        "skT": np.ascontiguousarray(np.transpose(sk, (3, 0, 1, 2)).reshape(128, 2048)),
        "u_lay": np.ascontiguousarray(np.transpose(u.reshape(128, 128, 8, 128), (3, 0, 2, 1)).reshape(128, 131072)),
        "v_lay": np.ascontiguousarray(np.transpose(v.reshape(128, 128, D), (1, 0, 2)).reshape(128, 131072)),
        "final_g": f(final_g).reshape(1, D), "ident": np.eye(128, dtype=np.float32),
        "iota": np.ascontiguousarray(np.broadcast_to(np.arange(128, dtype=np.float32)[None, :], (128, 128))),
        "biasBg": np.stack([_bias_b_chunk(rpb, 20, 10 - 2 + j) for j in range(5)]).reshape(5, 128, 1024),
    }
    in_maps = []
    for core in range(NCORES):
        b, half = core // 2, core % 2
        start = half * TOK - HALO
        xe = np.zeros((NEXT * 128, D), np.float32)
        lo, hi = max(start, 0), min(start + NEXT * 128, SEQ)
        xe[lo - start:hi - start] = x[b, lo:hi]
        sp = []
        for (li, kps) in _SP_LIST:
            gp = half * 32 + li
            for kp in kps:
                sp.append(_bias_b_chunk(rpb, 2 * gp, half * 32 + kp))
        bA = shared["biasA"].copy()
        if half != 0:
            bA[1] = bA[0]
        if half != 1:
            bA[2] = bA[0]
        m = dict(shared)
        m.update({"x_ext": xe, "c_col": _col(c[b]), "biasA": bA, "biasBs": np.stack(sp).reshape(22, 128, 1024)})
        in_maps.append(m)
    if _PREP_ONLY:
        return in_maps
    nc = build_program()
    res = run_bass_kernel_spmd(nc, in_maps, core_ids=list(range(NCORES)))
    outp = np.zeros((4, SEQ, D), np.float32)
    for core in range(NCORES):
        b, half = core // 2, core % 2
        outp[b, half * TOK:(half + 1) * TOK] = res.results[core]["out"]
    return outp


if __name__ == "__main__":
    import time
    t0 = time.time()
    build_program()
    print("built", time.time() - t0, build_program.stats)
```

```python
import numpy as np
from contextlib import ExitStack
import concourse.bass as bass
import concourse.mybir as mybir
from concourse.bass_utils import run_bass_kernel_spmd

F32 = mybir.dt.float32
BF16 = mybir.dt.bfloat16
U32 = mybir.dt.uint32
AF = mybir.ActivationFunctionType
ALU = mybir.AluOpType
AX = mybir.AxisListType

SAME_ENGINE_SYNC = True
EPS = 1e-6
NEGM = -1.0e4
NCORES = 8
SEQ = 8192
TOK = 4096
HALO = 256
NEXT = 36
D = 1024


class Prog:
    EPOCH = 20000

    def __init__(self, nc, es):
        self.nc = nc
        self.es = es
        self.engs = {"pe": nc.tensor, "act": nc.scalar, "dve": nc.vector, "pool": nc.gpsimd, "sp": nc.sync}
        self.sems = {}
        self.cnt = {}
        self.waited = {}
        self.state = {}
        self.ops = []
        self.engcount = {k: 0 for k in self.engs}
        self.nwaits = 0
        self.stopped = False

    def _sem(self, k):
        if k not in self.sems:
            self.sems[k] = self.es.enter_context(self.nc.semaphore("s%d" % len(self.sems)))
        return self.sems[k]

    def _entries(self, name, key):
        d = self.state.setdefault(name, {})
        if key is None:
            if None not in d:
                d[None] = [None, {}, []]
            return list(d.values())
        if key not in d:
            if None in d:
                b = d[None]
                d[key] = [b[0], dict(b[1]), list(b[2])]
            else:
                d[key] = [None, {}, []]
        return [d[key]]

    def op(self, eng, fn, reads=(), writes=(), dma=False, semkey=None):
        if self.stopped:
            return None
        idx = len(self.ops)
        deps = set()
        psdeps = set()
        for (name, key) in reads:
            for ent in self._entries(name, key):
                if ent[0] is not None:
                    deps.add(ent[0])
        for (name, key) in writes:
            tgt = psdeps if name.startswith("ps") else deps
            for ent in self._entries(name, key):
                if ent[0] is not None:
                    tgt.add(ent[0])
                tgt.update(ent[1].values())
                tgt.update(ent[2])
        e = self.engs[eng]
        for di in sorted(deps | psdeps):
            deng, dk, dval, ddma = self.ops[di]
            if (not ddma) and deng == eng and (eng == "pe" or not SAME_ENGINE_SYNC or di not in deps):
                continue
            if self.waited.get((eng, dk), 0) >= dval:
                continue
            e.wait_ge(self._sem(dk), dval)
            self.waited[(eng, dk)] = dval
            self.nwaits += 1
        ins = fn(e)
        if dma:
            k = ("dma", semkey)
            inc = 16
        else:
            self.engcount[eng] += 1
            k = ("eng", eng, self.engcount[eng] // self.EPOCH)
            inc = 1
        self.cnt[k] = self.cnt.get(k, 0) + inc
        ins.then_inc(self._sem(k), inc)
        self.ops.append((eng, k, self.cnt[k], dma))
        for (name, key) in reads:
            for ent in self._entries(name, key):
                if dma:
                    ent[2].append(idx)
                else:
                    ent[1][eng] = idx
        for (name, key) in writes:
            d = self.state[name]
            if key is None:
                for kk in list(d.keys()):
                    d[kk] = [idx, {}, []]
            else:
                d[key] = [idx, {}, []]
        return idx

    def barrier(self):
        if self.stopped:
            return
        targets = list(self.cnt.items())
        for eng, e in self.engs.items():
            for k, v in targets:
                if self.waited.get((eng, k), 0) >= v:
                    continue
                e.wait_ge(self._sem(k), v)
                self.waited[(eng, k)] = v
                self.nwaits += 1

    def finish(self, eng="sp"):
        e = self.engs[eng]
        for k, v in self.cnt.items():
            if k[0] == "dma" and self.waited.get((eng, k), 0) < v:
                e.wait_ge(self._sem(k), v)


class _Stop(Exception):
    pass


STAGE = 99
DEBUG = False
OH1_ENG = "dve"
SWEEP_MULT_ENG = "pool"
ACT_OH_EVERY = 1000000


def build_program():
    nc = bass.Bass("TRN2", target_bir_lowering=False)

    def stage(n):
        if STAGE <= n:
            P.stopped = True

    def din(name, shape, dt=F32):
        return nc.dram_tensor(name, shape, dt, kind="ExternalInput").ap()

    x_ext = din("x_ext", [NEXT * 128, D])
    c_col = din("c_col", [128, 8])
    w_ada = din("w_ada", [D, 6 * D])
    b_ada = din("b_ada", [1, 6 * D])
    g1c_d = din("g1c", [128, 8])
    g2c_d = din("g2c", [128, 8])
    onc_d = din("onc", [128, 8])
    w_in = din("w_in", [D, 2304])
    sink_d = din("sink", [1, 8])
    biasA_d = din("biasA", [3, 3, 128, 1024])
    biasBg_d = din("biasBg", [5, 128, 1024])
    biasBs_d = din("biasBs", [22, 128, 1024])
    w_out = din("w_out", [D, D])
    w_query = din("w_query", [D, 2048])
    skT_d = din("skT", [128, 2048])
    u_lay = din("u_lay", [128, 131072])
    v_lay = din("v_lay", [128, 131072])
    fg_d = din("final_g", [1, D])
    ident_d = din("ident", [128, 128])
    iota_d = din("iota", [128, 128])
    out = nc.dram_tensor("out", [TOK, D], F32, kind="ExternalOutput").ap()
    dbg = nc.dram_tensor("dbg", [128, 8192], F32, kind="ExternalOutput").ap() if DEBUG else None
    x1s = nc.dram_tensor("x1s", [TOK, D], F32, kind="Internal").ap()
    uTb = nc.dram_tensor("uTb", [128, 131072], BF16, kind="Internal").ap()
    vb = nc.dram_tensor("vb", [128, 131072], BF16, kind="Internal").ap()

    with ExitStack() as es:
      P = Prog(nc, es)
      try:

        def sbuf(stack, name, shape, dt):
            return stack.enter_context(nc.sbuf_tensor("sb_" + name, shape, dt))

        ps = [es.enter_context(nc.psum_tensor("ps%d" % b, [128, 512], F32)) for b in range(8)]

        def PS(b):
            return [("ps%d" % b, None)]

        ukey = [0]

        def dma(eng, out_ap, in_ap, reads, writes, semkey=None):
            if semkey is None:
                ukey[0] += 1
                semkey = "u%d" % ukey[0]
            return P.op(eng, lambda e: e.dma_start(out=out_ap, in_=in_ap), reads=reads, writes=writes, dma=True, semkey=semkey)

        def dump(ap, col0, width, res):
            if DEBUG:
                dma("pool", dbg[:, col0:col0 + width], ap, [res], [("dbg", col0)])

        def mm(o, lhsT, rhs, start, stop, reads, writes):
            return P.op("pe", lambda e: e.matmul(o, lhsT=lhsT, rhs=rhs, start=start, stop=stop), reads=reads, writes=writes)

        def tr(o, in_, ident, reads, writes):
            return P.op("pe", lambda e: e.transpose(out=o, in_=in_, identity=ident), reads=reads, writes=writes)

        def act(o, in_, func, reads, writes, bias=None, scale=None, accum=None):
            kw = {}
            if bias is not None:
                kw["bias"] = bias
            if scale is not None:
                kw["scale"] = scale
            if accum is not None:
                kw["accum_out"] = accum
            return P.op("act", lambda e: e.activation(out=o, in_=in_, func=func, **kw), reads=reads, writes=writes)

        def tt(eng, o, a, b, op, reads, writes):
            return P.op(eng, lambda e: e.tensor_tensor(out=o, in0=a, in1=b, op=op), reads=reads, writes=writes)

        def tsc(eng, o, a, s1, s2, op0, op1, reads, writes):
            if op1 is None:
                return P.op(eng, lambda e: e.tensor_scalar(out=o, in0=a, scalar1=s1, scalar2=None, op0=op0), reads=reads, writes=writes)
            return P.op(eng, lambda e: e.tensor_scalar(out=o, in0=a, scalar1=s1, scalar2=s2, op0=op0, op1=op1), reads=reads, writes=writes)

        def cp(eng, o, a, reads, writes):
            if eng == "act":
                return P.op("act", lambda e: e.copy(out=o, in_=a), reads=reads, writes=writes)
            return P.op(eng, lambda e: e.tensor_copy(out=o, in_=a), reads=reads, writes=writes)

        ident_f = sbuf(es, "ident_f", [128, 128], F32)
        ident_b = sbuf(es, "ident_b", [128, 128], BF16)
        iota_f = sbuf(es, "iota_f", [128, 128], F32)
        iota_b = sbuf(es, "iota_b", [128, 128], BF16)
        ones_f = sbuf(es, "ones_f", [128, 128], F32)
        g2c = sbuf(es, "g2c", [128, 8], F32)
        a2 = sbuf(es, "a2", [128, 8], F32)
        b2 = sbuf(es, "b2", [128, 8], F32)
        gate2_bc = sbuf(es, "gate2_bc", [128, D], F32)
        fg_bc = sbuf(es, "fg_bc", [128, D], F32)

        dma("sp", ident_f[:], ident_d, [], [("ident_f", None)])
        dma("sp", iota_f[:], iota_d, [], [("iota_f", None)])
        dma("sp", g2c[:], g2c_d, [], [("g2c", None)])
        dma("sp", fg_bc[:], fg_d[0].partition_broadcast(128), [], [("fg_bc", None)])
        cp("dve", ident_b[:], ident_f[:], [("ident_f", None)], [("ident_b", None)])
        cp("dve", iota_b[:], iota_f[:], [("iota_f", None)], [("iota_b", None)])
        P.op("dve", lambda e: e.memset(ones_f[:], 1.0), writes=[("ones_f", None)])

        def norm_transpose(stack_bufs, xt_ap, xt_res, acol, bcol, acol_res, dst_fn, dst_res, tag, bank=0):
            ssq, rstd, xn = stack_bufs
            act(xn[:], xt_ap, AF.Square, [xt_res], [("xn" + tag, None), ("ssq" + tag, None)], accum=ssq[:])
            tsc("dve", rstd[:], ssq[:], 1.0 / D, EPS, ALU.mult, ALU.add, [("ssq" + tag, None)], [("rstd" + tag, None)])
            act(rstd[:], rstd[:], AF.Sqrt, [("rstd" + tag, None)], [("rstd" + tag, None)])
            P.op("dve", lambda e: e.reciprocal(out=rstd[:], in_=rstd[:]), reads=[("rstd" + tag, None)], writes=[("rstd" + tag, None)])
            tsc("dve", xn[:], xt_ap, rstd[:, 0:1], None, ALU.mult, None, [xt_res, ("rstd" + tag, None)], [("xn" + tag, None)])
            psb = ps[bank][:, :].bitcast(BF16)
            for kc in range(8):
                tr(psb[:, kc * 128:(kc + 1) * 128], xn[:, kc * 128:(kc + 1) * 128], ident_b[:],
                   [("xn" + tag, None), ("ident_b", None)], PS(bank))
            for kc in range(8):
                eng = "dve" if kc % 2 == 0 else "act"
                if eng == "dve":
                    tsc("dve", dst_fn(kc), psb[:, kc * 128:(kc + 1) * 128], acol[:, kc:kc + 1], bcol[:, kc:kc + 1],
                        ALU.mult, ALU.add, [acol_res], PS(bank) + [dst_res])
                else:
                    act(dst_fn(kc), psb[:, kc * 128:(kc + 1) * 128], AF.Identity, [acol_res], PS(bank) + [dst_res],
                        bias=bcol[:, kc:kc + 1], scale=acol[:, kc:kc + 1])

        with ExitStack() as ea:
            c_sb = sbuf(ea, "c_sb", [128, 8], F32)
            c_act = sbuf(ea, "c_act", [128, 8], F32)
            g1c = sbuf(ea, "g1c", [128, 8], F32)
            onc = sbuf(ea, "onc", [128, 8], F32)
            a1 = sbuf(ea, "a1", [128, 8], F32)
            b1 = sbuf(ea, "b1", [128, 8], F32)
            adacol = sbuf(ea, "adacol", [128, 32], F32)
            sink_bc = sbuf(ea, "sink_bc", [128, 8], F32)
            expsink = sbuf(ea, "expsink", [128, 8], F32)
            zeros8 = sbuf(ea, "zeros8", [128, 8], F32)
            wo_b = sbuf(ea, "wo_b", [128, 8, D], BF16)

            dma("sp", c_sb[:], c_col, [], [("c_sb", None)])
            dma("sp", g1c[:], g1c_d, [], [("g1c", None)])
            dma("sp", onc[:], onc_d, [], [("onc", None)])
            dma("sp", sink_bc[:], sink_d[0].partition_broadcast(128), [], [("sink_bc", None)])
            P.op("dve", lambda e: e.memset(zeros8[:], 0.0), writes=[("zeros8", None)])
            act(expsink[:], sink_bc[:], AF.Exp, [("sink_bc", None)], [("expsink", None)])
            act(c_act[:], c_sb[:], AF.Silu, [("c_sb", None)], [("c_act", None)])

            with ExitStack() as e0:
                wa = [sbuf(e0, "wa%d" % i, [128, 8, 512], F32) for i in range(2)]
                ada_row = sbuf(e0, "ada_row", [1, 6 * D], F32)
                bada = sbuf(e0, "bada", [1, 6 * D], F32)
                wo_f = sbuf(e0, "wo_f", [128, 8, D], F32)
                gate1_bc = sbuf(e0, "gate1_bc", [128, D], F32)
                dma("sp", bada[:], b_ada, [], [("bada", None)])
                dma("sp", wo_f[:], w_out.rearrange("(kc p) n -> p kc n", p=128), [], [("wo_f", None)])
                w_ada_v = w_ada.rearrange("(kc p) n -> p kc n", p=128)
                for blk in range(12):
                    sl = blk % 2
                    dma("sp", wa[sl][:], w_ada_v[:, :, blk * 512:(blk + 1) * 512], [], [("wa%d" % sl, None)], "wa%d" % sl)
                    for kc in range(8):
                        mm(ps[7][0:1, 0:512], c_act[:, kc:kc + 1], wa[sl][:, kc, :], kc == 0, kc == 7,
                           [("c_act", None), ("wa%d" % sl, None)], PS(7))
                    tt("dve", ada_row[0:1, blk * 512:(blk + 1) * 512], ps[7][0:1, 0:512], bada[0:1, blk * 512:(blk + 1) * 512],
                       ALU.add, [("bada", None)], PS(7) + [("ada_row", blk)])
                for vi, v in enumerate([0, 1, 3, 4]):
                    for kc in range(8):
                        mm(ps[7][:, vi * 8 + kc:vi * 8 + kc + 1], ada_row[0:1, v * D + kc * 128:v * D + (kc + 1) * 128],
                           ident_f[0:1, 0:1], True, True, [("ada_row", None), ("ident_f", None)], PS(7))
                cp("dve", adacol[:], ps[7][:, 0:32], [], PS(7) + [("adacol", None)])
                tsc("dve", a1[:], adacol[:, 8:16], 1.0, None, ALU.add, None, [("adacol", None)], [("a1", None)])
                tt("dve", a1[:], a1[:], g1c[:], ALU.mult, [("g1c", None)], [("a1", None)])
                cp("dve", b1[:], adacol[:, 0:8], [("adacol", None)], [("b1", None)])
                tsc("dve", a2[:], adacol[:, 24:32], 1.0, None, ALU.add, None, [("adacol", None)], [("a2", None)])
                tt("dve", a2[:], a2[:], g2c[:], ALU.mult, [("g2c", None)], [("a2", None)])
                cp("dve", b2[:], adacol[:, 16:24], [("adacol", None)], [("b2", None)])
                for gi, (v, dst, dname) in enumerate([(2, gate1_bc, "gate1_bc"), (5, gate2_bc, "gate2_bc")]):
                    for hf in range(2):
                        mm(ps[1 + hf][:, 0:512], ones_f[0:1, 0:128], ada_row[0:1, v * D + hf * 512:v * D + (hf + 1) * 512],
                           True, True, [("ones_f", None), ("ada_row", None)], PS(1 + hf))
                        cp("dve", dst[:, hf * 512:(hf + 1) * 512], ps[1 + hf][:, 0:512], [], PS(1 + hf) + [(dname, None)])
                for kc in range(8):
                    P.op("dve", lambda e, kc=kc: e.scalar_tensor_tensor(out=wo_b[:, kc, :], in0=wo_f[:, kc, :], scalar=onc[:, kc:kc + 1],
                                                                          in1=gate1_bc[:], op0=ALU.mult, op1=ALU.mult),
                         reads=[("wo_f", None), ("onc", None), ("gate1_bc", None)], writes=[("wo_b", None)])

            P.barrier()
            dump(a1[:], 0, 8, ("a1", None)); dump(b1[:], 8, 8, ("b1", None)); dump(a2[:], 16, 8, ("a2", None)); dump(b2[:], 24, 8, ("b2", None))
            dump(gate2_bc[:], 32, 1024, ("gate2_bc", None)); dump(wo_b[:, 0, :], 1056, 1024, ("wo_b", None))
            stage(1)
            w_in_b = sbuf(ea, "w_in_b", [128, 8, 2304], BF16)
            wkdup = sbuf(ea, "wkdup", [128, 8, 2, 128], BF16)
            biasA = sbuf(ea, "biasA", [128, 3, 1024], BF16)
            biasBg = sbuf(ea, "biasBg", [128, 5, 1024], BF16)
            biasBs = sbuf(ea, "biasBs", [128, 6, 1024], BF16)
            w_in_v = w_in.rearrange("(kc p) n -> p kc n", p=128)
            for hf in range(2):
                dma("pool", w_in_b[:, :, hf * 1152:(hf + 1) * 1152], w_in_v[:, :, hf * 1152:(hf + 1) * 1152],
                    [], [("w_in_b", None)])
            dma("pool", biasA[:], biasA_d[0].rearrange("c k n -> k c n"), [], [("biasA", None)])
            dma("pool", biasBg[:], biasBg_d.rearrange("c k n -> k c n"), [], [("biasBg", None)])
            for g in range(2):
                for dup in range(2):
                    cp("pool", wkdup[:, :, g, dup * 64:(dup + 1) * 64], w_in_b[:, :, 512 + g * 64:512 + (g + 1) * 64],
                       [("w_in_b", None)], [("wkdup", None)])
            hT = sbuf(ea, "hT", [128, 8, 1024], BF16)
            KA = sbuf(ea, "KA", [128, 2, 1024], BF16)
            KB = sbuf(ea, "KB", [128, 4, 1024], BF16)
            QA = sbuf(ea, "QA", [128, 4, 512], BF16)
            QB = sbuf(ea, "QB", [128, 4, 512], BF16)
            VA = sbuf(ea, "VA", [128, 8, 2, 65], BF16)
            VB = sbuf(ea, "VB", [128, 8, 8, 65], BF16)
            NXT = 3
            xts = [sbuf(ea, "xt%d" % i, [128, D], F32) for i in range(NXT)]
            xrs = [sbuf(ea, "xr%d" % i, [128, D], F32) for i in range(2)]
            nbs = [(sbuf(ea, "ssqA%d" % i, [128, 1], F32), sbuf(ea, "rstdA%d" % i, [128, 1], F32), sbuf(ea, "xnA%d" % i, [128, D], BF16))
                   for i in range(2)]
            Pb = [sbuf(ea, "Pb0", [128, 3, 1024], BF16), sbuf(ea, "Pb1", [128, 6, 1024], BF16),
                  sbuf(ea, "Pb2", [128, 3, 1024], BF16), sbuf(ea, "Pb3", [128, 6, 1024], BF16)]
            den = sbuf(ea, "den", [128, 8], F32)
            rden = sbuf(ea, "rden", [128, 8], F32)
            o_t = sbuf(ea, "o_t", [128, 512], BF16)
            oss = sbuf(ea, "oss", [128, 1], F32)
            orr = sbuf(ea, "orr", [128, 1], F32)
            on_a = sbuf(ea, "on_a", [128, 512], BF16)
            on_b = sbuf(ea, "on_b", [128, 512], BF16)
            mixT = sbuf(ea, "mixT", [128, 8, 128], BF16)
            P.op("dve", lambda e: e.memset(VA[:], 1.0), writes=[("VA", None)])
            P.op("dve", lambda e: e.memset(VB[:], 1.0), writes=[("VB", None)])

            sstate = {"sb": 0, "pb": 0, "x": 0}

            def attn_tile(pb, nch, kT, q, bias, V, sinkt, dst, dst_name, rd, pvb):
                Pt = Pb[pb]
                pname = "Pb%d" % pb
                for c in range(nch):
                    for grp in range(2):
                        bk = 3 + sstate["sb"]
                        sstate["sb"] ^= 1
                        mm(ps[bk][:, 0:512], ident_b[:], bias(c)[:, grp * 512:(grp + 1) * 512], True, False,
                           [("ident_b", None)] + rd, PS(bk))
                        for r in range(4):
                            h = grp + 2 * r
                            mm(ps[bk][:, r * 128:(r + 1) * 128], kT(h, c), q(h), False, r == 3, rd, PS(bk))
                        act(Pt[:, c, grp * 512:(grp + 1) * 512], ps[bk][:, 0:512], AF.Exp, [], PS(bk) + [(pname, None)])

                def pv_phase():
                    for h in range(8):
                        bk = pvb[h // 4]
                        for c in range(nch):
                            pos = (h % 2) * 4 + h // 2
                            mm(ps[bk][:, (h % 4) * 65:(h % 4) * 65 + 65], Pt[:, c, pos * 128:(pos + 1) * 128], V(h, c), c == 0, c == nch - 1,
                               [(pname, None)] + rd, PS(bk))

                def norm_phase():
                    for bnk in range(2):
                        pv = ps[pvb[bnk]][:, 0:260].rearrange("p (h e) -> p h e", e=65)
                        tt("dve", den[:, bnk * 4:(bnk + 1) * 4], pv[:, :, 64], sinkt[:, bnk * 4:(bnk + 1) * 4], ALU.add,
                           [("expsink", None), ("zeros8", None)], PS(pvb[bnk]) + [("den", None)])
                    P.op("dve", lambda e: e.reciprocal(out=rden[:], in_=den[:]), reads=[("den", None)], writes=[("rden", None)])
                    for bnk in range(2):
                        pv = ps[pvb[bnk]][:, 0:260].rearrange("p (h e) -> p h e", e=65)
                        ov = o_t[:, bnk * 256:(bnk + 1) * 256].rearrange("p (h e) -> p h e", e=64)
                        rb = rden[:, bnk * 4:(bnk + 1) * 4].unsqueeze(2).broadcast_to([128, 4, 64])
                        tt("dve", ov, pv[:, :, 0:64], rb, ALU.mult, [("rden", None)], PS(pvb[bnk]) + [("o_t", None)])
                    act(dst[:], o_t[:], AF.Square, [("o_t", None)], [(dst_name, None), ("oss", None)], accum=oss[:])
                    tsc("dve", orr[:], oss[:], 1.0 / 512, EPS, ALU.mult, ALU.add, [("oss", None)], [("orr", None)])
                    act(orr[:], orr[:], AF.Sqrt, [("orr", None)], [("orr", None)])
                    P.op("dve", lambda e: e.reciprocal(out=orr[:], in_=orr[:]), reads=[("orr", None)], writes=[("orr", None)])
                    tsc("dve", dst[:], o_t[:], orr[:, 0:1], None, ALU.mult, None, [("o_t", None), ("orr", None)], [(dst_name, None)])

                return pv_phase, norm_phase

            sp_off = {0: 0, 1: 6, 30: 11, 31: 16}
            for s in range(8):
                if s == 1:
                    stage(6)
                def x_load(te):
                    xs = te % NXT
                    dma("sp", xts[xs][:], x_ext[te * 128:(te + 1) * 128, :], [], [("xt%d" % xs, None)], "xt%d" % xs)

                def nt_stats(te):
                    if te in nstate["stats"]:
                        return
                    nstate["stats"].add(te)
                    if te + NXT - 1 < NEXT:
                        x_load(te + NXT - 1)
                    xs = te % NXT
                    xt = xts[xs]
                    ssq, rstd, xn = nbs[te % 2]
                    tg = "A%d" % (te % 2)
                    xres = ("xt%d" % xs, None)
                    act(xn[:], xt[:], AF.Square, [xres], [("xn" + tg, None), ("ssq" + tg, None)], accum=ssq[:])
                    tsc("dve", rstd[:], ssq[:], 1.0 / D, EPS, ALU.mult, ALU.add, [("ssq" + tg, None)], [("rstd" + tg, None)])
                    act(rstd[:], rstd[:], AF.Sqrt, [("rstd" + tg, None)], [("rstd" + tg, None)])
                    P.op("dve", lambda e: e.reciprocal(out=rstd[:], in_=rstd[:]), reads=[("rstd" + tg, None)], writes=[("rstd" + tg, None)])
                    tsc("dve", xn[:], xt[:], rstd[:, 0:1], None, ALU.mult, None, [xres, ("rstd" + tg, None)], [("xn" + tg, None)])

                def nt_rest(te):
                    tl = te % 8
                    xn = nbs[te % 2][2]
                    tg = "A%d" % (te % 2)
                    psb0 = ps[0][:, :].bitcast(BF16)
                    psb7 = ps[7][:, :].bitcast(BF16)
                    for kc in range(8):
                        pb_, bk_ = (psb0, 0) if kc < 4 else (psb7, 7)
                        tr(pb_[:, (kc % 4) * 128:(kc % 4 + 1) * 128], xn[:, kc * 128:(kc + 1) * 128], ident_b[:],
                           [("xn" + tg, None), ("ident_b", None)], PS(bk_))
                    for kk in range(4):
                        for kc in (kk, kk + 4):
                            dstk = hT[:, kc, tl * 128:(tl + 1) * 128]
                            if kc < 4:
                                tsc("dve", dstk, psb0[:, kc * 128:(kc + 1) * 128], a1[:, kc:kc + 1], b1[:, kc:kc + 1],
                                    ALU.mult, ALU.add, [("a1", None)], PS(0) + [("hT", (tl, kc))])
                            else:
                                act(dstk, psb7[:, (kc - 4) * 128:(kc - 3) * 128], AF.Identity, [("a1", None)], PS(7) + [("hT", (tl, kc))],
                                    bias=b1[:, kc:kc + 1], scale=a1[:, kc:kc + 1])
                    for kc in range(8):
                        mm(ps[1][:, 0:512], hT[:, kc, tl * 128:(tl + 1) * 128], w_in_b[:, kc, 1792:2304], kc == 0, kc == 7,
                           [("hT", (tl, kc)), ("w_in_b", None)], PS(1))
                        mm(ps[2][:, 0:128], hT[:, kc, tl * 128:(tl + 1) * 128], w_in_b[:, kc, 640:768], kc == 0, kc == 7,
                           [("hT", (tl, kc)), ("w_in_b", None)], PS(2))
                    cp("act", VB[:, tl, :, 0:64], ps[1][:, 0:512].rearrange("p (h e) -> p h e", e=64), [], PS(1) + [("VB", tl)])
                    cp("dve", VA[:, tl, :, 0:64], ps[2][:, 0:128].rearrange("p (h e) -> p h e", e=64), [], PS(2) + [("VA", tl)])

                if s == 0:
                    nstate = {"stats": set()}
                    for te0 in range(NXT - 1):
                        x_load(te0)
                newt = list(range(0, 8)) if s == 0 else list(range(4 * s + 4, 4 * s + 8))
                for te in newt:
                    nt_stats(te)
                    if te + 1 < NEXT:
                        nt_stats(te + 1)
                    nt_rest(te)
                stage(2)
                pj = 0
                halves = [0, 1] if s == 0 else [((4 * s + 4) % 8) // 4]
                for hf in halves:
                    for ch in range(6):
                        bk = 1 + pj % 2
                        pj += 1
                        for kc in range(8):
                            lw = wkdup[:, kc, ch, :] if ch < 2 else w_in_b[:, kc, 1280 + (ch - 2) * 128:1280 + (ch - 1) * 128]
                            mm(ps[bk][:, 0:512], lw, hT[:, kc, hf * 512:(hf + 1) * 512], kc == 0, kc == 7,
                               [("hT", None), ("w_in_b", None), ("wkdup", None)], PS(bk))
                        if ch < 2:
                            cp("act" if pj % 2 else "dve", KA[:, ch, hf * 512:(hf + 1) * 512], ps[bk][:, 0:512], [], PS(bk) + [("KA", None)])
                        else:
                            cp("act" if pj % 2 else "dve", KB[:, ch - 2, hf * 512:(hf + 1) * 512], ps[bk][:, 0:512], [], PS(bk) + [("KB", None)])
                q0 = (4 * s + 2) % 8
                qparts = [(q0 * 128, 512, 0)] if q0 == 2 else [(768, 256, 0), (0, 256, 256)]
                for ch in range(8):
                    bk = 1 + pj % 2
                    pj += 1
                    col0 = ch * 128 if ch < 4 else 768 + (ch - 4) * 128
                    first = True
                    for kc in range(8):
                        for (hc, hn_, pc_) in qparts:
                            mm(ps[bk][:, pc_:pc_ + hn_], w_in_b[:, kc, col0:col0 + 128], hT[:, kc, hc:hc + hn_], first,
                               kc == 7 and (hc, hn_, pc_) == qparts[-1], [("hT", None), ("w_in_b", None)], PS(bk))
                            first = False
                    dq = QA[:, ch, :] if ch < 4 else QB[:, ch - 4, :]
                    dn = "QA" if ch < 4 else "QB"
                    act(dq, ps[bk][:, 0:512], AF.Identity, [], PS(bk) + [(dn, None)], scale=0.125)
                stage(3)
                def s_phase(j):
                    i = 4 * s + j
                    tl = j + 2
                    sl_ = lambda p_: (4 * s + p_) % 8
                    pbo = 2 * (j % 2)
                    if 2 <= i < 18:
                        pc = i - 2
                        for (src_d, dst_d, nm) in ((u_lay, uTb, "uTb"), (v_lay, vb, "vb")):
                            dma("pool", dst_d[:, pc * 8192:(pc + 1) * 8192].rearrange("p (a b) -> p a b", b=2048),
                                src_d[:, pc * 8192:(pc + 1) * 8192].rearrange("p (a b) -> p a b", b=2048), [], [(nm, pc)], "cv" + nm)
                    rdA = [("KA", None), ("QA", None), ("VA", None), ("biasA", None), ("biasBs", None)]
                    if i in (0, 31):
                        dma("pool", biasBs[:, 0:3, :], biasA_d[1 if i == 0 else 2].rearrange("c k n -> k c n"),
                            [], [("biasBs", None)], "bs")
                        bfa = lambda c: biasBs[:, c, :]
                    else:
                        bfa = lambda c: biasA[:, c, :]
                    wpv, wnorm = attn_tile(pbo, 3,
                              lambda h, c, tl=tl: KA[(h % 2) * 64:(h % 2) * 64 + 64, h // 4, sl_(tl - 1 + c) * 128:sl_(tl - 1 + c) * 128 + 128],
                              lambda h, j=j: QA[(h % 2) * 64:(h % 2) * 64 + 64, h // 2, j * 128:(j + 1) * 128],
                              bfa,
                              lambda h, c, tl=tl: VA[:, sl_(tl - 1 + c), h // 4, :],
                              expsink, on_a, "on_a", rdA, (5, 6))
                    stage(4)
                    rdB = [("KB", None), ("QB", None), ("VB", None), ("biasBg", None), ("biasBs", None)]
                    if i in sp_off:
                        nchb = 6 if i in (0, 31) else 5
                        dma("pool", biasBs[:, 0:nchb, :], biasBs_d[sp_off[i]:sp_off[i] + nchb].rearrange("c k n -> k c n"),
                            [], [("biasBs", None)], "bs")
                        c0 = tl - 3 if i == 31 else tl - 2
                        bfn = lambda c: biasBs[:, c, :]
                    else:
                        nchb = 5
                        c0 = tl - 2
                        bfn = lambda c: biasBg[:, c, :]
                    bpv, bnorm = attn_tile(pbo + 1, nchb,
                              lambda h, c, c0=c0: KB[(h % 2) * 64:(h % 2) * 64 + 64, h // 2, sl_(c0 + c) * 128:sl_(c0 + c) * 128 + 128],
                              lambda h, j=j: QB[(h % 2) * 64:(h % 2) * 64 + 64, h // 2, j * 128:(j + 1) * 128],
                              bfn,
                              lambda h, c, c0=c0: VB[:, sl_(c0 + c), h, :],
                              zeros8, on_b, "on_b", rdB, (1, 2))

                    def pv():
                        wpv()
                        bpv()

                    def norms():
                        wnorm()
                        bnorm()
                        if i == 0:
                            dump(on_a[:], 2080, 512, ("on_a", None)); dump(on_b[:], 2592, 512, ("on_b", None))
                            dump(hT[:, 0, 256:384], 4128, 128, ("hT", None))

                    def tail():
                        stage(5)
                        psb = ps[0][:, :].bitcast(BF16)
                        for kc in range(8):
                            src_ = on_a if kc < 4 else on_b
                            tr(psb[:, kc * 128:(kc + 1) * 128], src_[:, (kc % 4) * 128:(kc % 4 + 1) * 128], ident_b[:],
                               [("on_a", None), ("on_b", None), ("ident_b", None)], PS(0))
                        cp("act", mixT[:].rearrange("p k t -> p (k t)"), psb[:, 0:1024], [], PS(0) + [("mixT", None)])
                        xr = xrs[i % 2]
                        dma("sp", xr[:], x_ext[(i + 2) * 128:(i + 3) * 128, :], [], [("xr%d" % (i % 2), None)], "xr%d" % (i % 2))
                        for hf in range(2):
                            for kc in range(8):
                                mm(ps[1 + hf][:, 0:512], mixT[:, kc, :], wo_b[:, kc, hf * 512:(hf + 1) * 512], kc == 0, kc == 7,
                                   [("mixT", None), ("wo_b", None)], PS(1 + hf))
                            tt("dve", xr[:, hf * 512:(hf + 1) * 512], ps[1 + hf][:, 0:512], xr[:, hf * 512:(hf + 1) * 512], ALU.add,
                               [("xr%d" % (i % 2), None)], PS(1 + hf) + [("xr%d" % (i % 2), None)])
                        if i == 0:
                            dump(xr[:], 3104, 1024, ("xr0", None))
                        dma("sp", x1s[i * 128:(i + 1) * 128, :], xr[:], [("xr%d" % (i % 2), None)], [("x1s", i)], "x1o%d" % (i % 2))

                    return pv, norms, tail

                prev = s_phase(0)
                prev[0]()
                for j in range(1, 4):
                    cur = s_phase(j)
                    prev[1]()
                    prev[2]()
                    cur[0]()
                    prev = cur
                prev[1]()
                prev[2]()

        P.barrier()
        stage(7)
        with ExitStack() as eb:
            wq = sbuf(eb, "wq", [128, 8, 2048], BF16)
            skT = sbuf(eb, "skT", [128, 2048], BF16)
            dma("pool", wq[:], w_query.rearrange("(kc p) n -> p kc n", p=128), [], [("wq", None)])
            dma("pool", skT[:], skT_d, [], [("skT", None)])
            stage(8)
            Gs = sbuf(eb, "Gs", [128, 256, 128], BF16)
            h2Ts = [sbuf(eb, "h2T%d" % i, [128, 8, 256], BF16) for i in range(2)]
            qhT = sbuf(eb, "qhT", [128, 16, 256], BF16)
            S = sbuf(eb, "S", [128, 16, 128], F32)
            V16 = sbuf(eb, "V16", [128, 16, 16], F32)
            I16 = sbuf(eb, "I16", [128, 16, 16], U32)
            I16f = sbuf(eb, "I16f", [128, 16, 16], F32)
            cand = sbuf(eb, "cand", [128, 8, 256], F32)
            eq = cand[:].rearrange("p h (a b) -> p h a b", b=16)
            VS = sbuf(eb, "VS", [128, 8, 16], F32)
            CI = sbuf(eb, "CI", [128, 8, 16], U32)
            CIf = sbuf(eb, "CIf", [128, 8, 16], F32)
            CAf = sbuf(eb, "CAf", [128, 8, 16], F32)
            CBf = sbuf(eb, "CBf", [128, 8, 16], F32)
            E1 = sbuf(eb, "E1", [128, 128], F32)
            E2 = sbuf(eb, "E2", [128, 128], F32)
            Gt = sbuf(eb, "Gt", [128, 128], F32)
            gsum = sbuf(eb, "gsum", [128, 8], F32)
            E1T = sbuf(eb, "E1T", [128, 256], F32)
            E2T = sbuf(eb, "E2T", [128, 256], F32)
            GT = sbuf(eb, "GT", [128, 256], F32)
            Ag = [sbuf(eb, "Ag%d" % i, [128, 256], BF16) for i in range(4)]
            NOH = 6
            OH1 = [sbuf(eb, "OH1_%d" % i, [128, 128], BF16) for i in range(NOH)]
            OH2 = [sbuf(eb, "OH2_%d" % i, [128, 128], BF16) for i in range(NOH)]
            Uc = [sbuf(eb, "Uc%d" % i, [128, 2, 8, 128], BF16) for i in range(2)]
            Vc = [sbuf(eb, "Vc%d" % i, [128, 2, D], BF16) for i in range(4)]
            At = [sbuf(eb, "At%d" % i, [128, 256], BF16) for i in range(4)]
            x1g = [[sbuf(eb, "x1g%d_%d" % (p_, i), [128, D], F32) for i in range(2)] for p_ in range(2)]
            yt = sbuf(eb, "yt", [128, D], F32)
            nb2 = (sbuf(eb, "ssqB", [128, 1], F32), sbuf(eb, "rstdB", [128, 1], F32), sbuf(eb, "xnB", [128, D], BF16))
            fjunk = sbuf(eb, "fjunk", [128, D], BF16)
            fss = sbuf(eb, "fss", [128, 1], F32)
            frs = sbuf(eb, "frs", [128, 1], F32)
            PB = 7

            def top16_batch(items):
                for (s_, sr, vd, idd, vr, ir) in items:
                    P.op("dve", lambda e, s_=s_, vd=vd: e.max(out=vd[:, 0:8], in_=s_), reads=[sr], writes=[vr])
                yield
                for (s_, sr, vd, idd, vr, ir) in items:
                    P.op("dve", lambda e, s_=s_, vd=vd, idd=idd: e.max_index(out=idd[:, 0:8], in_max=vd[:, 0:8], in_values=s_),
                         reads=[sr, vr], writes=[ir])
                yield
                for (s_, sr, vd, idd, vr, ir) in items:
                    P.op("dve", lambda e, s_=s_, vd=vd: e.match_replace(out=s_, in_to_replace=vd[:, 0:8], in_values=s_, imm_value=-1e30),
                         reads=[vr], writes=[sr])
                yield
                for (s_, sr, vd, idd, vr, ir) in items:
                    P.op("dve", lambda e, s_=s_, vd=vd: e.max(out=vd[:, 8:16], in_=s_), reads=[sr], writes=[vr])
                yield
                for (s_, sr, vd, idd, vr, ir) in items:
                    P.op("dve", lambda e, s_=s_, vd=vd, idd=idd: e.max_index(out=idd[:, 8:16], in_max=vd[:, 8:16], in_values=s_),
                         reads=[sr, vr], writes=[ir])
                yield

            def prep(gi):
                par = gi % 2
                h2T = h2Ts[par]
                hn = "h2T%d" % par
                for tk in range(2):
                    ti = 2 * gi + tk
                    xn_ = "x1g%d_%d" % (par, tk)
                    dma("sp", x1g[par][tk][:], x1s[ti * 128:(ti + 1) * 128, :], [("x1s", ti)], [(xn_, None)], xn_)
                    norm_transpose(nb2, x1g[par][tk][:], (xn_, None), a2, b2, ("a2", None),
                                   lambda kc, tk=tk: h2T[:, kc, tk * 128:(tk + 1) * 128], (hn, None), "B", bank=PB)
                    yield
                stage(9)
                for c in range(16):
                    for kc in range(8):
                        mm(ps[PB][:, 0:256], wq[:, kc, c * 128:(c + 1) * 128], h2T[:, kc, :], kc == 0, kc == 7,
                           [("wq", None), (hn, None)], PS(PB))
                    cp("act", qhT[:, c, :], ps[PB][:, 0:256], [], PS(PB) + [("qhT", None)])
                    yield
                for tk in range(2):
                    for cg in range(4):
                        for cc in range(4):
                            c = cg * 4 + cc
                            mm(ps[PB][:, cc * 128:(cc + 1) * 128], qhT[:, c, tk * 128:(tk + 1) * 128], skT[:, c * 128:(c + 1) * 128],
                               True, True, [("qhT", None), ("skT", None)], PS(PB))
                        cp("act", S[:, cg * 4:(cg + 1) * 4, :].rearrange("p a b -> p (a b)"), ps[PB][:, 0:512],
                           [], PS(PB) + [("S", cg * 4 + q_) for q_ in range(4)])
                        yield
                    if gi == 0 and tk == 0:
                        dump(S[:, 0:2, :].rearrange("p a b -> p (a b)"), 4256, 256, ("S", None))
                        dump(h2T[:, 0, :], 5664, 256, (hn, None))
                    stage(10)
                    for half_ in range(2):
                        yield from top16_batch([(S[:, c, :], ("S", c), V16[:, c, :], I16[:, c, :], ("V16", c), ("I16", c))
                                                for c in range(half_ * 8, half_ * 8 + 8)])
                    V4 = V16[:].rearrange("p (h two) k -> p h two k", two=2)
                    c4 = cand[:].rearrange("p h (a b) -> p h a b", b=16)
                    for hq in range(4):
                        hs = slice(2 * hq, 2 * hq + 2)
                        tt("dve", c4[:, hs], V4[:, hs, 0, :].unsqueeze(3).broadcast_to([128, 2, 16, 16]),
                           V4[:, hs, 1, :].unsqueeze(2).broadcast_to([128, 2, 16, 16]), ALU.add,
                           [("V16", None)], [("cand", 2 * hq), ("cand", 2 * hq + 1)])
                        yield
                    yield from top16_batch([(cand[:, h, :], ("cand", h), VS[:, h, :], CI[:, h, :], ("VS", h), ("CI", h)) for h in range(8)])
                    stage(11)
                    MAGIC = 12582912.0
                    cp("dve", CIf[:], CI[:], [("CI", None)], [("CIf", None)])
                    tsc("dve", CAf[:], CIf[:], 0.0625, -0.46875, ALU.mult, ALU.add, [("CIf", None)], [("CAf", None)])
                    tsc("dve", CAf[:], CAf[:], MAGIC, None, ALU.add, None, [("CAf", None)], [("CAf", None)])
                    tsc("dve", CAf[:], CAf[:], -MAGIC, None, ALU.add, None, [("CAf", None)], [("CAf", None)])
                    P.op("dve", lambda e: e.scalar_tensor_tensor(out=CBf[:], in0=CAf[:], scalar=-16.0, in1=CIf[:], op0=ALU.mult, op1=ALU.add),
                         reads=[("CAf", None), ("CIf", None)], writes=[("CBf", None)])
                    cp("dve", I16f[:], I16[:], [("I16", None)], [("I16f", None)])
                    yield
                    I4 = I16f[:].rearrange("p (h two) k -> p h two k", two=2)
                    io16 = iota_f[:, 0:16].unsqueeze(1).unsqueeze(1).broadcast_to([128, 2, 16, 16])
                    for (cf, cfn, two, Ed, Edn) in ((CAf, "CAf", 0, E1, "E1"), (CBf, "CBf", 1, E2, "E2")):
                        for hq in range(4):
                            hs = slice(2 * hq, 2 * hq + 2)
                            cres = [("cand", 2 * hq), ("cand", 2 * hq + 1)]
                            tt("dve", eq[:, hs], io16, cf[:, hs].unsqueeze(3).broadcast_to([128, 2, 16, 16]), ALU.is_equal,
                               [("iota_f", None), (cfn, None)], cres)
                            yield
                            tt("dve", eq[:, hs], eq[:, hs], I4[:, hs, two, :].unsqueeze(2).broadcast_to([128, 2, 16, 16]), ALU.mult,
                               [("I16f", None)], cres)
                            yield
                            P.op("dve", lambda e, Ed=Ed, hs=hs: e.tensor_reduce(out=Ed[:].rearrange("p (h k) -> p h k", k=16)[:, hs],
                                                                                in_=eq[:, hs], axis=AX.X, op=ALU.add),
                                 reads=cres, writes=[(Edn, hq)])
                            yield
                    G3 = Gt[:].rearrange("p (h k) -> p h k", k=16)
                    tt("dve", G3, VS[:], VS[:, :, 0:1].broadcast_to([128, 8, 16]), ALU.subtract, [("VS", None)], [("Gt", None)])
                    act(Gt[:], Gt[:], AF.Exp, [("Gt", None)], [("Gt", None)])
                    P.op("dve", lambda e: e.tensor_reduce(out=gsum[:], in_=G3, axis=AX.X, op=ALU.add),
                         reads=[("Gt", None)], writes=[("gsum", None)])
                    P.op("dve", lambda e: e.reciprocal(out=gsum[:], in_=gsum[:]), reads=[("gsum", None)], writes=[("gsum", None)])
                    tt("dve", G3, G3, gsum[:].unsqueeze(2).broadcast_to([128, 8, 16]), ALU.mult, [("gsum", None)], [("Gt", None)])
                    yield
                    if gi == 0 and tk == 0:
                        dump(V16[:].rearrange("p a b -> p (a b)"), 4512, 256, ("V16", None))
                        dump(I16f[:].rearrange("p a b -> p (a b)"), 4768, 256, ("I16f", None))
                        dump(VS[:].rearrange("p a b -> p (a b)"), 5024, 128, ("VS", None))
                        dump(CIf[:].rearrange("p a b -> p (a b)"), 5152, 128, ("CIf", None))
                        dump(E1[:], 5280, 128, ("E1", None)); dump(E2[:], 5408, 128, ("E2", None)); dump(Gt[:], 5536, 128, ("Gt", None))
                    for qi, (src_, sn, dstT, dn, sc_) in enumerate(((E1, "E1", E1T, "E1T", 1.0), (E2, "E2", E2T, "E2T", 1.0),
                                                                    (Gt, "Gt", GT, "GT", 1.0))):
                        tr(ps[PB][:, 0:128], src_[:], ident_f[:], [(sn, None), ("ident_f", None)], PS(PB))
                        act(dstT[:, tk * 128:(tk + 1) * 128], ps[PB][:, 0:128], AF.Identity, [], PS(PB) + [(dn, None)], scale=sc_)
                        yield

            def gbuild(gi):
                for t in range(256):
                    sl = t % NOH
                    tsc("dve", OH2[sl][:], iota_b[:], E2T[:, t:t + 1], GT[:, t:t + 1], ALU.is_equal, ALU.mult,
                        [("iota_b", None), ("E2T", None), ("GT", None)], [("OH2_%d" % sl, None)])
                    tsc("dve", OH1[sl][:], iota_b[:], E1T[:, t:t + 1], None, ALU.is_equal, None,
                        [("iota_b", None), ("E1T", None)], [("OH1_%d" % sl, None)])
                    bk = 4 + (t // 4) % 4
                    mm(ps[bk][:, (t % 4) * 128:(t % 4 + 1) * 128], OH2[sl][:], OH1[sl][:], True, True,
                       [("OH2_%d" % sl, None), ("OH1_%d" % sl, None)], PS(bk))
                    if t % 4 == 3:
                        cp("act", Gs[:, t - 3:t + 1, :].rearrange("p a b -> p (a b)"), ps[bk][:, 0:512],
                           [], PS(bk) + [("Gs", None)])
                if gi == 0:
                    dump(Gs[:, 0, :], 6048, 128, ("Gs", None)); dump(Gs[:, 200, :], 6176, 128, ("Gs", None))

            LA = 2
            NZ = 3

            def wload(sc):
                us = sc % 2
                dma("sp", Uc[us][:].rearrange("p a b c -> p (a b c)"), uTb[:, sc * 2048:(sc + 1) * 2048],
                    [("uTb", None)], [("Uc%d" % us, None)], "Uc%d" % us)
                vs_ = sc % 4
                dma("sp", Vc[vs_][:].rearrange("p a b -> p (a b)"), vb[:, sc * 2048:(sc + 1) * 2048],
                    [("vb", None)], [("Vc%d" % vs_, None)], "Vc%d" % vs_)

            def sweep(gi, gen):
                par = gi % 2
                h2T = h2Ts[par]
                hn = "h2T%d" % par

                def zstage(c):
                    sc, cl = c // 2, c % 2
                    us = sc % 2
                    if cl == 0 and sc >= 2:
                        wload(sc)
                    bk = 4 + c % NZ
                    for kc in range(8):
                        mm(ps[bk][:, 0:256], Uc[us][:, cl, kc, :], h2T[:, kc, :], kc == 0, kc == 7,
                           [("Uc%d" % us, None), (hn, None)], PS(bk))
                    if SWEEP_MULT_ENG == "pool":
                        act(Ag[c % 4][:], ps[bk][:, 0:256], AF.Gelu_apprx_tanh, [], PS(bk) + [("Ag%d" % (c % 4), None)])
                        tt("pool", At[c % 4][:], Ag[c % 4][:], Gs[:, :, c], ALU.mult, [("Gs", None), ("Ag%d" % (c % 4), None)],
                           [("At%d" % (c % 4), None)])
                    else:
                        act(ps[bk][:, 0:256], ps[bk][:, 0:256], AF.Gelu_apprx_tanh, [], PS(bk))
                        tt("dve", At[c % 4][:], ps[bk][:, 0:256], Gs[:, :, c], ALU.mult, [("Gs", None)], PS(bk) + [("At%d" % (c % 4), None)])

                def ostage(c):
                    sc, cl = c // 2, c % 2
                    vs_ = sc % 4
                    for tk in range(2):
                        for hf in range(2):
                            mm(ps[tk * 2 + hf][:, 0:512], At[c % 4][:, tk * 128:(tk + 1) * 128], Vc[vs_][:, cl, hf * 512:(hf + 1) * 512],
                               c == 0, c == 127, [("At%d" % (c % 4), None), ("Vc%d" % vs_, None)], PS(tk * 2 + hf))

                for step in range(128 + LA):
                    if step < 128:
                        zstage(step)
                    if step >= LA:
                        ostage(step - LA)
                    if gen is not None and step >= 4:
                        next(gen, None)

            def finalize(gi):
                par = gi % 2
                for tk in range(2):
                    ti = 2 * gi + tk
                    xn_ = "x1g%d_%d" % (par, tk)
                    for hf in range(2):
                        tt("dve", yt[:, hf * 512:(hf + 1) * 512], ps[tk * 2 + hf][:, 0:512], gate2_bc[:, hf * 512:(hf + 1) * 512], ALU.mult,
                           [("gate2_bc", None)], PS(tk * 2 + hf) + [("yt", None)])
                    if ti == 0:
                        dump(yt[:], 6304, 1024, ("yt", None))
                    tt("pool", yt[:], yt[:], x1g[par][tk][:], ALU.add, [(xn_, None)], [("yt", None)])
                    act(fjunk[:], yt[:], AF.Square, [("yt", None)], [("fjunk", None), ("fss", None)], accum=fss[:])
                    tsc("dve", frs[:], fss[:], 1.0 / D, EPS, ALU.mult, ALU.add, [("fss", None)], [("frs", None)])
                    act(frs[:], frs[:], AF.Sqrt, [("frs", None)], [("frs", None)])
                    P.op("dve", lambda e: e.reciprocal(out=frs[:], in_=frs[:]), reads=[("frs", None)], writes=[("frs", None)])
                    o_ = x1g[par][tk]
                    P.op("dve", lambda e, o_=o_: e.scalar_tensor_tensor(out=o_[:], in0=yt[:], scalar=frs[:, 0:1], in1=fg_bc[:],
                                                                          op0=ALU.mult, op1=ALU.mult),
                         reads=[("yt", None), ("frs", None), ("fg_bc", None)], writes=[(xn_, None)])
                    dma("sp", out[ti * 128:(ti + 1) * 128, :], o_[:], [(xn_, None)], [("out", ti)], "oo" + xn_)

            for _ in prep(0):
                pass
            for gi in range(16):
                stage(12)
                wload(0)
                wload(1)
                gbuild(gi)
                stage(13)
                gen = prep(gi + 1) if gi + 1 < 16 else None
                sweep(gi, gen)
                if gen is not None:
                    for _ in gen:
                        pass
                stage(14)
                finalize(gi)
      except _Stop:
        pass
      P.finish("sp")
      build_program.stats = dict(nops=len(P.ops), nwaits=P.nwaits, nsems=len(P.sems))
    return nc


def _t5_bucket_np(rel):
    import jax
    import jax.numpy as jnp
    import math
    cpu = jax.devices("cpu")[0]
    with jax.default_device(cpu):
        rel = jnp.asarray(rel, dtype=jnp.int32)
        half = 16
        max_exact = 8
        ret = jnp.where(rel > 0, half, 0)
        n = jnp.abs(rel)
        nf = jnp.maximum(n, 1).astype(jnp.float32)
        large = max_exact + (jnp.log(nf / max_exact) / math.log(128 / max_exact) * (half - max_exact)).astype(jnp.int32)
        large = jnp.minimum(large, half - 1)
        return np.asarray(ret + jnp.where(n < max_exact, n, large))


def _col(v):
    return np.ascontiguousarray(np.asarray(v, np.float32).reshape(8, 128).T)


def _bias_a(t5_table):
    k = np.arange(128)[:, None]
    q = np.arange(128)[None, :]
    outp = np.full((3, 3, 128, 8, 128), NEGM, np.float32)
    for c in range(3):
        rel = (c * 128 + k - 128) - q
        band = np.abs(rel) <= 128
        bk = _t5_bucket_np(rel)
        vals = t5_table[bk]
        vals = np.transpose(vals, (0, 2, 1))[:, _HORDER, :]
        m = np.broadcast_to(band[:, None, :], vals.shape)
        blk = np.where(m, vals, np.float32(NEGM)).astype(np.float32)
        outp[0, c] = blk
        outp[1, c] = blk if c != 0 else NEGM
        outp[2, c] = blk if c != 2 else NEGM
    return outp.reshape(3, 3, 128, 1024)


def _bias_b_chunk(rpb, r0, kp):
    outp = np.full((128, 8, 128), NEGM, np.float32)
    kk = np.arange(128)
    kr, kcol = kk // 64, kk % 64
    y = 2 * kp + kr
    qq = np.arange(128)
    qr, c = qq // 64, qq % 64
    r = r0 + qr
    rs = np.clip(r - 4, 0, 120)
    cs = np.clip(c - 8, 0, 48)
    Y = y[:, None]
    KC = kcol[:, None]
    valid = (Y >= rs[None, :]) & (Y < rs[None, :] + 8) & (KC >= cs[None, :]) & (KC < cs[None, :] + 16) & (Y >= 0) & (Y < 128)
    ro = np.clip(Y - r[None, :] + 7, 0, 14)
    co = np.clip(KC - c[None, :] + 15, 0, 30)
    vals = rpb[:, ro, co]
    vals = np.transpose(vals, (1, 0, 2))[:, _HORDER, :]
    m = np.broadcast_to(valid[:, None, :], vals.shape)
    return np.where(m, vals, np.float32(NEGM)).astype(np.float32)


_PREP_ONLY = False
_HORDER = [0, 2, 4, 6, 1, 3, 5, 7]
_SP_LIST = [(0, list(range(-2, 4))), (1, list(range(-1, 4))), (30, list(range(28, 33))), (31, list(range(28, 34)))]


def kernel(x, c, w_ada, b_ada, norm1_g, w_in, sink_a, t5_table, rpb_b, out_norm_a, out_norm_b,
           w_out, norm2_g, w_query, sub_keys, u_experts, v_experts, final_g):
    f = lambda a: np.ascontiguousarray(np.asarray(a, dtype=np.float32))
    x = f(x); c = f(c); w_ada = f(w_ada)[0]; b_ada = f(b_ada); w_in = f(w_in)[0]
    t5 = f(t5_table); rpb = f(rpb_b)[0]; w_out = f(w_out)[0]; w_query = f(w_query)[0]
    sk = f(sub_keys)[0]; u = f(u_experts)[0]; v = f(v_experts)[0]
    shared = {
        "w_ada": w_ada, "b_ada": b_ada.reshape(1, 6 * D), "g1c": _col(f(norm1_g)[0]), "g2c": _col(f(norm2_g)[0]),
        "onc": _col(np.concatenate([f(out_norm_a)[0], f(out_norm_b)[0]])), "w_in": w_in, "sink": f(sink_a).reshape(1, 8),
        "biasA": _bias_a(t5), "w_out": w_out, "w_query": w_query,
        "skT": np.ascontiguousarray(np.transpose(sk, (3, 0, 1, 2)).reshape(128, 2048)),
        "u_lay": np.ascontiguousarray(np.transpose(u.reshape(128, 128, 8, 128), (3, 0, 2, 1)).reshape(128, 131072)),
        "v_lay": np.ascontiguousarray(np.transpose(v.reshape(128, 128, D), (1, 0, 2)).reshape(128, 131072)),
        "final_g": f(final_g).reshape(1, D), "ident": np.eye(128, dtype=np.float32),
        "iota": np.ascontiguousarray(np.broadcast_to(np.arange(128, dtype=np.float32)[None, :], (128, 128))),
        "biasBg": np.stack([_bias_b_chunk(rpb, 20, 10 - 2 + j) for j in range(5)]).reshape(5, 128, 1024),
    }
    in_maps = []
    for core in range(NCORES):
        b, half = core // 2, core % 2
        start = half * TOK - HALO
        xe = np.zeros((NEXT * 128, D), np.float32)
        lo, hi = max(start, 0), min(start + NEXT * 128, SEQ)
        xe[lo - start:hi - start] = x[b, lo:hi]
        sp = []
        for (li, kps) in _SP_LIST:
            gp = half * 32 + li
            for kp in kps:
                sp.append(_bias_b_chunk(rpb, 2 * gp, half * 32 + kp))
        bA = shared["biasA"].copy()
        if half != 0:
            bA[1] = bA[0]
        if half != 1:
            bA[2] = bA[0]
        m = dict(shared)
        m.update({"x_ext": xe, "c_col": _col(c[b]), "biasA": bA, "biasBs": np.stack(sp).reshape(22, 128, 1024)})
        in_maps.append(m)
    if _PREP_ONLY:
        return in_maps
    nc = build_program()
    res = run_bass_kernel_spmd(nc, in_maps, core_ids=list(range(NCORES)))
    outp = np.zeros((4, SEQ, D), np.float32)
    for core in range(NCORES):
        b, half = core // 2, core % 2
        outp[b, half * TOK:(half + 1) * TOK] = res.results[core]["out"]
    return outp


if __name__ == "__main__":
    import time
    t0 = time.time()
    build_program()
    print("built", time.time() - t0, build_program.stats)
```
